# Optimizing a Trainium2 kernel written in Bass

```python
import math
import jax, jax.numpy as jnp
from jax import lax
import numpy as np

D_MODEL = 4096
BATCH = 2
SEQ = 4096
DEPTH = 2

CTX_LEN = 256
GRID_W = 64
HEAD_DIM = 128
MIX_WIDTH = D_MODEL
GROUP_WIDTH = MIX_WIDTH // 4
GLA_HEADS = GROUP_WIDTH // HEAD_DIM
GLA_DK = HEAD_DIM // 2
GLA_DV = HEAD_DIM
GLA_GATE_RANK = 16
GLA_TAU = 16.0
FNET_GROUPS = GROUP_WIDTH // HEAD_DIM
FNET_GDIM = HEAD_DIM
GDN_HEADS = GROUP_WIDTH // HEAD_DIM
GDN_DK = HEAD_DIM
GDN_DV = HEAD_DIM
CONV_W = 3
CHUNK = 64
N_EXPERTS = 16
EC_CAPACITY = 2
D_FF = D_MODEL // 4
N_MOD = 6
EPS = 1e-6
IN_SIZES = (
    GLA_HEADS * GLA_DK,
    GLA_HEADS * GLA_DK,
    GLA_HEADS * GLA_DV,
    GLA_HEADS * GLA_DV,
    2 * GLA_GATE_RANK,
    FNET_GROUPS * FNET_GDIM,
    3 * GDN_HEADS * GDN_DK,
    GDN_HEADS * GDN_DV,
    2 * GDN_HEADS,
    2 * GDN_HEADS,
    GROUP_WIDTH,
    GROUP_WIDTH,
    GROUP_WIDTH,
)
IN_WIDTH = sum(IN_SIZES)

kernel_name = "hybrid_parallel_gla_fnet_gdn_shortconv_ecmoe"


def rmsnorm(x, w):
    x32 = x.astype(jnp.float32)
    y = x32 * lax.rsqrt(jnp.mean(x32 * x32, axis=-1, keepdims=True) + EPS)
    return (y * w.astype(jnp.float32)).astype(x.dtype)


def l2norm(t):
    t32 = t.astype(jnp.float32)
    return t32 * lax.rsqrt(jnp.sum(t32 * t32, axis=-1, keepdims=True) + EPS)


def modulate(h, shift, scale):
    return h * (1 + scale) + shift


def conv3(u, w, axis):
    n = u.shape[axis]
    pad = [(0, 0)] * u.ndim
    pad[axis] = (1, 1)
    up = jnp.pad(u, pad)
    return (lax.slice_in_dim(up, 0, n, axis=axis) * w[0]
            + lax.slice_in_dim(up, 1, n + 1, axis=axis) * w[1]
            + lax.slice_in_dim(up, 2, n + 2, axis=axis) * w[2])


def grid_conv3(u, w, axis):
    b, t, c = u.shape
    rows = t // GRID_W
    return conv3(u.reshape(b, rows, GRID_W, c), w, axis).reshape(b, t, c)


def gla_chunked(q, k, v, log_a, s0, need_out):
    b_, t_, h_, _ = q.shape
    n_ = t_ // CHUNK
    q, k, v, log_a = [a.astype(jnp.float32).reshape(b_, n_, CHUNK, h_, a.shape[-1]) for a in (q, k, v, log_a)]
    cum = jnp.cumsum(log_a, axis=2)
    cum_last = cum[:, :, -1]
    k_end = k * jnp.exp(cum_last[:, :, None] - cum)
    incr = jnp.einsum('bnchk,bnchv->nbhkv', k_end, v)
    decay = jnp.moveaxis(jnp.exp(cum_last), 1, 0)

    def step(s, inp):
        d, u = inp
        return d[..., None] * s + u, s

    s_final, s_in = lax.scan(step, s0, (decay, incr))
    if not need_out:
        return None, s_final
    qd = q * jnp.exp(cum)
    kd = k * jnp.exp(-cum)
    mask = jnp.tril(jnp.ones((CHUNK, CHUNK), dtype=bool))
    att = jnp.where(mask, jnp.einsum('bnchk,bnshk->bnhcs', qd, kd), 0.0)
    o = (jnp.einsum('bnhcs,bnshv->bnchv', att, v)
         + jnp.einsum('bnchk,nbhkv->bnchv', qd, s_in))
    return o.reshape(b_, t_, h_, v.shape[-1]), s_final


def gdn_chunked(q, k, v, beta, log_a, s0, need_out):
    b_, t_, h_, _ = q.shape
    n_ = t_ // CHUNK

    def chunks(a):
        return a.astype(jnp.float32).reshape(b_, n_, CHUNK, h_, a.shape[-1]).transpose(0, 3, 1, 2, 4)

    def chunks_s(a):
        return a.astype(jnp.float32).reshape(b_, n_, CHUNK, h_).transpose(0, 3, 1, 2)

    q, k, v = chunks(q), chunks(k), chunks(v)
    beta, gc = chunks_s(beta), jnp.cumsum(chunks_s(log_a), axis=-1)
    incl = jnp.tril(jnp.ones((CHUNK, CHUNK), dtype=bool))
    strict = jnp.tril(jnp.ones((CHUNK, CHUNK), dtype=bool), -1)
    diff = gc[..., :, None] - gc[..., None, :]
    lmat = jnp.exp(jnp.where(incl, diff, -jnp.inf))
    kb = k * beta[..., None]
    a_strict = jnp.where(strict, jnp.einsum('bhncd,bhnsd->bhncs', kb, k) * lmat, 0.0)
    m = a_strict + jnp.eye(CHUNK, dtype=jnp.float32)
    u = lax.linalg.triangular_solve(m, v * beta[..., None], left_side=True, lower=True, unit_diagonal=True)
    w = lax.linalg.triangular_solve(m, kb * jnp.exp(gc)[..., None], left_side=True, lower=True, unit_diagonal=True)
    g_last = gc[..., -1]
    k_end = k * jnp.exp(g_last[..., None] - gc)[..., None]
    xs = (jnp.moveaxis(u, 2, 0), jnp.moveaxis(w, 2, 0), jnp.moveaxis(k_end, 2, 0), jnp.moveaxis(jnp.exp(g_last), 2, 0))

    def step(s, inp):
        u_n, w_n, ke_n, d_n = inp
        v_new = u_n - jnp.einsum('bhck,bhkv->bhcv', w_n, s)
        s_next = d_n[..., None, None] * s + jnp.einsum('bhck,bhcv->bhkv', ke_n, v_new)
        return s_next, (s, v_new)

    s_final, (s_in, v_new) = lax.scan(step, s0, xs)
    if not need_out:
        return None, s_final
    v_new = jnp.moveaxis(v_new, 0, 2)
    qk = jnp.einsum('bhnck,bhnsk->bhncs', q, k) * lmat
    o = (jnp.einsum('bhnck,nbhkv->bhncv', q * jnp.exp(gc)[..., None], s_in)
         + jnp.einsum('bhncs,bhnsv->bhncv', qk, v_new))
    return o.transpose(0, 2, 3, 1, 4).reshape(b_, t_, h_, v.shape[-1]), s_final


def bidir(scan_fn, fwd_args, bwd_args, s0_f, s0_b, need_out):
    o_f, s_f = scan_fn(*fwd_args, s0_f, need_out)
    o_b, s_b = scan_fn(*[jnp.flip(a, axis=1) for a in bwd_args], s0_b, need_out)
    o = o_f + jnp.flip(o_b, axis=1) if need_out else None
    return o, s_f, s_b


def recurrent_inputs(parts, gla_gate_up, gla_gate_b, gdn_conv_w, gdn_A_log, gdn_dt_bias, conv_fn):
    gq, gk, gv, glr, dqkv, dbeta, da = parts[0], parts[1], parts[2], parts[4], parts[6], parts[8], parts[9]
    b_, t_, _ = gq.shape
    q = gq.reshape(b_, t_, GLA_HEADS, GLA_DK) * (GLA_DK ** -0.5)
    k = gk.reshape(b_, t_, GLA_HEADS, GLA_DK)
    v = gv.reshape(b_, t_, GLA_HEADS, GLA_DV)
    lr = jnp.split(glr, 2, axis=-1)
    la = [jax.nn.log_sigmoid((lr[d] @ gla_gate_up[d] + gla_gate_b[d]).astype(jnp.float32)).reshape(
        b_, t_, GLA_HEADS, GLA_DK) / GLA_TAU for d in range(2)]
    gla = ((q, k, v, la[0]), (q, k, v, la[1]))
    qkv = jax.nn.silu(conv_fn(dqkv, gdn_conv_w))
    dq, dk, dv = jnp.split(qkv, 3, axis=-1)
    dq = l2norm(dq.reshape(b_, t_, GDN_HEADS, GDN_DK)) * (GDN_DK ** -0.5)
    dk = l2norm(dk.reshape(b_, t_, GDN_HEADS, GDN_DK))
    dv = dv.reshape(b_, t_, GDN_HEADS, GDN_DV)
    betas = jnp.split(jax.nn.sigmoid(dbeta.astype(jnp.float32)), 2, axis=-1)
    a = jnp.split(da.astype(jnp.float32), 2, axis=-1)
    lg = [-jnp.exp(gdn_A_log[d].astype(jnp.float32)) * jax.nn.softplus(a[d] + gdn_dt_bias[d].astype(jnp.float32))
          for d in range(2)]
    gdn = ((dq, dk, dv, betas[0], lg[0]), (dq, dk, dv, betas[1], lg[1]))
    return gla, gdn


def fourier(u):
    b_, t_, _ = u.shape
    g = u.astype(jnp.float32).reshape(b_, t_, FNET_GROUPS, FNET_GDIM)
    y = jnp.fft.fft2(g, axes=(1, 3), norm='ortho').real
    return y.reshape(b_, t_, -1).astype(u.dtype)


def mix_out(parts, gla_o, gdn_o, gla_norm_w, gdn_norm_w, sc_conv_w, conv_fn):
    gg, fin, z, sh, sb, scg = parts[3], parts[5], parts[7], parts[10], parts[11], parts[12]
    b_, t_, _ = gg.shape
    dt = gg.dtype
    out_a = rmsnorm(gla_o, gla_norm_w).reshape(b_, t_, -1).astype(dt) * jax.nn.silu(gg)
    out_b = fourier(fin)
    out_c = rmsnorm(gdn_o, gdn_norm_w).reshape(b_, t_, -1).astype(dt) * jax.nn.silu(z)
    out_d = sb * conv_fn(scg * sh, sc_conv_w)
    return jnp.concatenate([out_a, out_b, out_c, out_d], axis=-1)


def ec_moe(h, w_router, w_gate, w_up, w_down):
    b_, t_, d_ = h.shape
    cap = EC_CAPACITY * t_ // N_EXPERTS
    aff = jax.nn.softmax((h @ w_router).astype(jnp.float32), axis=-1)
    g, idx = lax.top_k(jnp.swapaxes(aff, 1, 2), cap)
    xg = jax.vmap(lambda hb, ib: hb[ib])(h, idx)
    hid = jax.nn.silu(jnp.einsum('becd,edf->becf', xg, w_gate)) * jnp.einsum('becd,edf->becf', xg, w_up)
    out = jnp.einsum('becf,efd->becd', hid, w_down) * g[..., None].astype(h.dtype)
    return jax.vmap(lambda ob, ib: jnp.zeros((t_, d_), ob.dtype).at[ib.reshape(-1)].add(ob.reshape(-1, d_)))(out, idx)


def layer(x, xc, mod, mod_c, norm1_w, norm2_w, w_in, w_out, gla_gate_up, gla_gate_b, gla_norm_w,
          gdn_conv_w, gdn_A_log, gdn_dt_bias, gdn_norm_w, sc_conv_w, w_router, w_gate, w_up, w_down, last):
    b_ = x.shape[0]
    offsets = np.cumsum(IN_SIZES)[:-1].tolist()
    sh1, sc1, g1, sh2, sc2, g2 = jnp.split(mod, N_MOD, axis=-1)
    csh1, csc1, cg1, csh2, csc2, cg2 = jnp.split(mod_c, N_MOD, axis=-1)
    lat_parts = jnp.split(modulate(rmsnorm(x, norm1_w), sh1, sc1) @ w_in, offsets, axis=-1)
    ctx_parts = jnp.split(modulate(rmsnorm(xc, norm1_w), csh1, csc1) @ w_in, offsets, axis=-1)

    lat_conv_v = lambda u, w: grid_conv3(u, w, 1)
    lat_conv_h = lambda u, w: grid_conv3(u, w, 2)
    seq_conv = lambda u, w: conv3(u, w, 1)
    lat_gla, lat_gdn = recurrent_inputs(lat_parts, gla_gate_up, gla_gate_b, gdn_conv_w, gdn_A_log, gdn_dt_bias, lat_conv_v)
    ctx_gla, ctx_gdn = recurrent_inputs(ctx_parts, gla_gate_up, gla_gate_b, gdn_conv_w, gdn_A_log, gdn_dt_bias, seq_conv)

    need_ctx = not last
    z_gla = jnp.zeros((b_, GLA_HEADS, GLA_DK, GLA_DV), jnp.float32)
    z_gdn = jnp.zeros((b_, GDN_HEADS, GDN_DK, GDN_DV), jnp.float32)
    cgla_o, gla_sf, gla_sb = bidir(gla_chunked, ctx_gla[0], ctx_gla[1], z_gla, z_gla, need_ctx)
    gla_o, _, _ = bidir(gla_chunked, lat_gla[0], lat_gla[1], gla_sf, gla_sb, True)
    cgdn_o, gdn_sf, gdn_sb = bidir(gdn_chunked, ctx_gdn[0], ctx_gdn[1], z_gdn, z_gdn, need_ctx)
    gdn_o, _, _ = bidir(gdn_chunked, lat_gdn[0], lat_gdn[1], gdn_sf, gdn_sb, True)

    x = x + g1 * (mix_out(lat_parts, gla_o, gdn_o, gla_norm_w, gdn_norm_w, sc_conv_w, lat_conv_h) @ w_out)
    x = x + g2 * ec_moe(modulate(rmsnorm(x, norm2_w), sh2, sc2), w_router, w_gate, w_up, w_down)
    if need_ctx:
        xc = xc + cg1 * (mix_out(ctx_parts, cgla_o, cgdn_o, gla_norm_w, gdn_norm_w, sc_conv_w, seq_conv) @ w_out)
        xc = xc + cg2 * ec_moe(modulate(rmsnorm(xc, norm2_w), csh2, csc2), w_router, w_gate, w_up, w_down)
    return x, xc


def setup_inputs(seed: int = 0) -> dict:
    key = jax.random.key(seed)
    ks = jax.random.split(key, 24)
    f32 = jnp.float32
    L, D = DEPTH, D_MODEL

    def nrm(k, shape, scale):
        return jax.random.normal(k, shape, f32) * scale

    dt = jnp.exp(jax.random.uniform(ks[12], (L, 2, GDN_HEADS), f32, math.log(1e-3), math.log(1e-1)))
    dt_bias = dt + jnp.log(-jnp.expm1(-dt))
    a_log = jnp.log(jax.random.uniform(ks[13], (L, 2, GDN_HEADS), f32, 1.0, 16.0))
    return {
        "x": nrm(ks[0], (BATCH, SEQ, D), 1.0),
        "c": nrm(ks[1], (BATCH, D), 1.0),
        "ctx": nrm(ks[2], (BATCH, CTX_LEN, D), 1.0),
        "c_ctx": nrm(ks[3], (D,), 1.0),
        "w_ada": nrm(ks[4], (L, D, N_MOD * D), 0.5 * D ** -0.5),
        "b_ada": nrm(ks[5], (L, N_MOD * D), 0.02),
        "norm1_w": 1.0 + nrm(ks[6], (L, D), 0.02),
        "norm2_w": 1.0 + nrm(ks[7], (L, D), 0.02),
        "w_in": nrm(ks[8], (L, D, IN_WIDTH), D ** -0.5),
        "w_out": nrm(ks[9], (L, MIX_WIDTH, D), MIX_WIDTH ** -0.5),
        "gla_gate_up": nrm(ks[10], (L, 2, GLA_GATE_RANK, GLA_HEADS * GLA_DK), GLA_GATE_RANK ** -0.5),
        "gla_gate_b": nrm(ks[11], (L, 2, GLA_HEADS * GLA_DK), 0.1),
        "gla_norm_w": 1.0 + nrm(ks[14], (L, GLA_DV), 0.02),
        "gdn_conv_w": nrm(ks[15], (L, CONV_W, 3 * GDN_HEADS * GDN_DK), CONV_W ** -0.5),
        "gdn_A_log": a_log,
        "gdn_dt_bias": dt_bias,
        "gdn_norm_w": 1.0 + nrm(ks[16], (L, GDN_DV), 0.02),
        "sc_conv_w": nrm(ks[17], (L, CONV_W, GROUP_WIDTH), CONV_W ** -0.5),
        "w_router": nrm(ks[18], (L, D, N_EXPERTS), D ** -0.5),
        "w_gate": nrm(ks[19], (L, N_EXPERTS, D, D_FF), D ** -0.5),
        "w_up": nrm(ks[20], (L, N_EXPERTS, D, D_FF), D ** -0.5),
        "w_down": nrm(ks[21], (L, N_EXPERTS, D_FF, D), D_FF ** -0.5),
        "final_norm_w": 1.0 + nrm(ks[22], (D,), 0.02),
    }


def reference(x, c, ctx, c_ctx, w_ada, b_ada, norm1_w, norm2_w, w_in, w_out, gla_gate_up, gla_gate_b,
              gla_norm_w, gdn_conv_w, gdn_A_log, gdn_dt_bias, gdn_norm_w, sc_conv_w, w_router, w_gate,
              w_up, w_down, final_norm_w):
    silu_c = jax.nn.silu(c)
    silu_cc = jax.nn.silu(c_ctx)
    xc = ctx
    for l in range(DEPTH):
        mod = (silu_c @ w_ada[l] + b_ada[l])[:, None, :]
        mod_c = (silu_cc @ w_ada[l] + b_ada[l])[None, None, :]
        x, xc = layer(x, xc, mod, mod_c, norm1_w[l], norm2_w[l], w_in[l], w_out[l], gla_gate_up[l],
                      gla_gate_b[l], gla_norm_w[l], gdn_conv_w[l], gdn_A_log[l], gdn_dt_bias[l],
                      gdn_norm_w[l], sc_conv_w[l], w_router[l], w_gate[l], w_up[l], w_down[l],
                      l == DEPTH - 1)
    return rmsnorm(x, final_norm_w)
```

```python
import numpy as np
import concourse.bass as bass
import concourse.mybir as mybir
from concourse.bass_utils import run_bass_kernel_spmd

F32 = mybir.dt.float32
BF16 = mybir.dt.bfloat16
I32 = mybir.dt.int32
U32 = mybir.dt.uint32
AF = mybir.ActivationFunctionType
ALU = mybir.AluOpType
AX = mybir.AxisListType

SEM_WRAP = 1 << 30


class P:
    def __init__(self, n_dma_sems=24):
        self.nc = bass.Bass("TRN2", target_bir_lowering=False)
        nc = self.nc
        self.eng = {"pe": nc.tensor, "dve": nc.vector, "act": nc.scalar, "pool": nc.gpsimd, "sp": nc.sync}
        self.sem = {k: nc.alloc_semaphore("s_" + k) for k in self.eng}
        self.cnt = {k: 0 for k in self.eng}
        self.know = {k: {} for k in self.eng}
        self.dsem = [nc.alloc_semaphore("d%d" % i) for i in range(n_dma_sems)]
        self.dval = [0] * n_dma_sems
        self.dnext = 0
        self.state = {}
        self.n_ins = 0
        self.n_wait = 0

    def dram(self, name, shape, dt, kind):
        return self.nc.dram_tensor(name, list(shape), dt, kind=kind).ap()

    def sb(self, name, shape, dt=F32):
        return self.nc.alloc_sbuf_tensor("sb_" + name, list(shape), dt).ap()

    def ps(self, name, shape, dt=F32):
        return self.nc.alloc_psum_tensor("ps_" + name, list(shape), dt).ap()

    def _wait(self, e, tok):
        if tok is None:
            return
        if tok[0] == "e":
            src, pos = tok[1], tok[2]
            if src == e and e == "pe":
                return
            kn = self.know[e].get(src, 0)
            if kn >= pos:
                return
            self.eng[e].wait_ge(self.sem[src], pos)
            self.know[e][src] = pos
            self.n_wait += 1
        else:
            i, val = tok[1], tok[2]
            key = ("d", i)
            if self.know[e].get(key, 0) >= val:
                return
            self.eng[e].wait_ge(self.dsem[i], val)
            self.know[e][key] = val
            self.n_wait += 1

    def _name(self, ref):
        ap = ref[0] if isinstance(ref, tuple) else ref
        return ap if isinstance(ap, str) else ap.tensor.name

    def _st(self, ref):
        if isinstance(ref, tuple):
            ap, key = ref
        else:
            ap, key = ref, None
        name = ap if isinstance(ap, str) else ap.tensor.name
        d = self.state.setdefault(name, {})
        return d, key

    def _deps(self, e, reads, writes):
        need = {}

        def add(tok):
            if tok is None:
                return
            k = (tok[0], tok[1])
            if need.get(k, 0) < tok[2]:
                need[k] = tok[2]
        for r in reads:
            d, key = self._st(r)
            keys = list(d.keys()) if key is None else [k for k in d if k is None or k == key]
            for k in keys:
                add(d[k][0])
        for w in writes:
            d, key = self._st(w)
            keys = list(d.keys()) if key is None else [k for k in d if k is None or k == key]
            for k in keys:
                add(d[k][0])
                for t in d[k][1]:
                    add(t)
        for (ty, src), v in need.items():
            self._wait(e, (ty, src, v))

    @staticmethod
    def _addreader(lst, tok):
        for i, t in enumerate(lst):
            if t[0] == tok[0] and t[1] == tok[1]:
                if t[2] < tok[2]:
                    lst[i] = tok
                return
        lst.append(tok)

    def _commit(self, tok, reads, writes):
        for r in reads:
            d, key = self._st(r)
            if key is None:
                if not d:
                    d[None] = [None, []]
                for k in d:
                    self._addreader(d[k][1], tok)
            else:
                if key not in d:
                    base = d.get(None, [None, []])
                    d[key] = [base[0], list(base[1])]
                self._addreader(d[key][1], tok)
        for w in writes:
            d, key = self._st(w)
            if key is None:
                d.clear()
                d[None] = [tok, []]
            else:
                if None in d and key not in d:
                    pass
                d[key] = [tok, []]

    def op(self, e, fn, reads, writes):
        pr = [r for r in reads if self._name(r).startswith("ps_")]
        if pr:
            reads = [r for r in reads if not self._name(r).startswith("ps_")]
            writes = list(writes) + pr
        self._deps(e, reads, writes)
        ins = fn()
        self.cnt[e] += 1
        ins.then_inc(self.sem[e], 1)
        tok = ("e", e, self.cnt[e])
        self.know[e][e] = self.know[e].get(e, 0)
        self._commit(tok, reads, writes)
        self.n_ins += 1
        return ins

    def dma(self, q, out, in_, reads=None, writes=None, **kw):
        reads = [in_] if reads is None else reads
        writes = [out] if writes is None else writes
        out = self._a(out); in_ = self._a(in_)
        i = self.dnext
        self.dnext = (self.dnext + 1) % len(self.dsem)
        if self.dval[i] > 0:
            self._wait(q, ("d", i, self.dval[i]))
        self._deps(q, reads, writes)
        ins = self.eng[q].dma_start(out=out, in_=in_, **kw)
        self.dval[i] += 16
        ins.then_inc(self.dsem[i], 16)
        tok = ("d", i, self.dval[i])
        self._commit(tok, reads, writes)
        self.n_ins += 1
        return tok

    def finish(self, toks_or_refs):
        for r in toks_or_refs:
            d, key = self._st(r)
            for k in d:
                self._wait("sp", d[k][0])
        for e in self.eng:
            if e != "sp" and self.cnt[e] > 0:
                self._wait("sp", ("e", e, self.cnt[e]))

    @staticmethod
    def _a(x):
        return x[0] if isinstance(x, tuple) else x

    def mm(self, out, lhsT, rhs, start=True, stop=True, okey=None, rkeys=None):
        rd = [lhsT, rhs] if rkeys is None else rkeys
        wr = [out if okey is None else (self._a(out), okey)]
        o_, l_, r_ = self._a(out), self._a(lhsT), self._a(rhs)
        return self.op("pe", lambda: self.nc.tensor.matmul(o_, l_, r_, start=start, stop=stop), rd, wr)

    def tr(self, out, in_, ident):
        o_, i_, d_ = self._a(out), self._a(in_), self._a(ident)
        return self.op("pe", lambda: self.nc.tensor.transpose(o_, i_, d_), [in_, ident], [out])

    def act(self, out, in_, func, bias=None, scale=None, accum_out=None, extra_reads=()):
        kw = {}
        rd = [in_] + list(extra_reads)
        if bias is not None:
            kw["bias"] = self._a(bias)
            if not isinstance(bias, (int, float)):
                rd.append(bias)
        if scale is not None:
            kw["scale"] = self._a(scale)
            if not isinstance(scale, (int, float)):
                rd.append(scale)
        wr = [out]
        if accum_out is not None:
            kw["accum_out"] = self._a(accum_out)
            wr.append(accum_out)
        o_, i_ = self._a(out), self._a(in_)
        return self.op("act", lambda: self.nc.scalar.activation(o_, i_, func, **kw), rd, wr)

    def _veng(self, e):
        return self.nc.vector if e == "dve" else self.nc.gpsimd

    def copy(self, out, in_, e="dve"):
        o_, i_ = self._a(out), self._a(in_)
        if e == "act":
            return self.op("act", lambda: self.nc.scalar.copy(o_, i_), [in_], [out])
        return self.op(e, lambda: self._veng(e).tensor_copy(o_, i_), [in_], [out])

    def tt(self, out, in0, in1, op, e="dve"):
        o_, a_, b_ = self._a(out), self._a(in0), self._a(in1)
        return self.op(e, lambda: self._veng(e).tensor_tensor(o_, a_, b_, op), [in0, in1], [out])

    def ts(self, out, in0, s1, op0, s2=None, op1=None, accum_out=None, e="dve"):
        rd = [in0]
        if not isinstance(s1, (int, float)):
            rd.append(s1)
        if s2 is not None and not isinstance(s2, (int, float)):
            rd.append(s2)
        wr = [out]
        kw = {}
        if op1 is not None:
            kw["op1"] = op1
        if accum_out is not None:
            kw["accum_out"] = self._a(accum_out)
            wr.append(accum_out)
        o_, a_, s1_, s2_ = self._a(out), self._a(in0), self._a(s1), self._a(s2)
        return self.op(e, lambda: self._veng(e).tensor_scalar(o_, a_, s1_, s2_, op0, **kw), rd, wr)

    def stt(self, out, in0, scalar, in1, op0, op1, e="dve"):
        rd = [in0, in1]
        if not isinstance(scalar, (int, float)):
            rd.append(scalar)
        o_, a_, s_, b_ = self._a(out), self._a(in0), self._a(scalar), self._a(in1)
        return self.op(e, lambda: self._veng(e).scalar_tensor_tensor(o_, a_, s_, b_, op0, op1), rd, [out])

    def memset(self, out, val, e="dve"):
        o_ = self._a(out)
        return self.op(e, lambda: self._veng(e).memset(o_, val), [], [out])

    def reduce(self, out, in_, op=ALU.add, axis=AX.X, e="dve"):
        o_, i_ = self._a(out), self._a(in_)
        return self.op(e, lambda: self._veng(e).tensor_reduce(o_, i_, axis, op), [in_], [out])

    def recip(self, out, in_):
        o_, i_ = self._a(out), self._a(in_)
        return self.op("dve", lambda: self.nc.vector.reciprocal(o_, i_), [in_], [out])


def run(p, in_maps, n=8):
    return run_bass_kernel_spmd(p.nc, in_maps, core_ids=list(range(n)))


D = 4096
NCORES = 8


def EPS_AP(p, val=1e-6):
    key = "_eps%g" % val
    if not hasattr(p, key):
        t = p.sb("eps%d" % len([k for k in p.__dict__ if k.startswith("_eps")]), [128, 1])
        p.memset(t, val)
        setattr(p, key, t)
    return getattr(p, key)
_CACHE = {}


def build_norm(out_bf16=True, modulate=True, router=False):
    p = P()
    xl = p.dram("xl", [1024, D], F32, "ExternalInput")
    xc = p.dram("xc", [64, D], F32, "ExternalInput")
    nw = p.dram("nw", [128, D], F32, "ExternalInput")
    odt = BF16 if out_bf16 else F32
    ol = p.dram("ol", [1024, D], odt, "ExternalOutput")
    oc = p.dram("oc", [64, D], odt, "ExternalOutput")
    nw_t = p.sb("nw_t", [128, D])
    p.dma("sp", nw_t, nw)
    wv = {}; sh = {}
    if modulate:
        for ty in ("l", "c"):
            sc_d = p.dram("sc_" + ty, [128, D], F32, "ExternalInput")
            sh_d = p.dram("sh_" + ty, [128, D], F32, "ExternalInput")
            wv[ty] = p.sb("wv_" + ty, [128, D]); sh[ty] = p.sb("sh_" + ty, [128, D])
            p.dma("sp", wv[ty], sc_d); p.dma("sp", sh[ty], sh_d)
            p.stt(wv[ty], wv[ty], 1.0, nw_t, ALU.add, ALU.mult)
    else:
        wv = {"l": nw_t, "c": nw_t}
    if router:
        wr = p.dram("wr", [128, 32, 16], F32, "ExternalInput")
        idn = p.dram("idn", [128, 128], F32, "ExternalInput")
        al = p.dram("al", [1024, 16], F32, "ExternalOutput"); ac = p.dram("ac", [64, 16], F32, "ExternalOutput")
        wrt = p.sb("wrt", [128, 32, 16]); idt = p.sb("idt", [128, 128])
        p.dma("sp", wrt, wr); p.dma("sp", idt, idn)
        hT = p.sb("hT", [128, 32, 128])
        ptr = [p.ps("ptr%d" % i, [128, 512]) for i in range(3)]
        plg = p.ps("plg", [128, 16])
        lg = p.sb("lg", [128, 16]); mx = p.sb("mx", [128, 1]); sm = p.sb("sm", [128, 1]); af = [p.sb("af%d" % i, [128, 16]) for i in range(2)]
    xt = [p.sb("xt%d" % i, [128, D]) for i in range(2)]
    sq = p.sb("sq", [128, D])
    ot = [p.sb("ot%d" % i, [128, D], odt) for i in range(2)]
    ss = [p.sb("ss%d" % i, [128, 1]) for i in range(2)]
    rs = [p.sb("rs%d" % i, [128, 1]) for i in range(2)]
    for t in range(9):
        n = 128 if t < 8 else 64
        ty = "l" if t < 8 else "c"
        src = xl[t * 128:(t + 1) * 128, :] if t < 8 else xc
        dst = ol[t * 128:(t + 1) * 128, :] if t < 8 else oc
        X = xt[t % 2]; O = ot[t % 2]; S = ss[t % 2]; Rr = rs[t % 2]
        p.dma("sp", X[:n], src)
        p.act(sq[:n], X[:n], AF.Square, accum_out=S[:n])
        p.act(Rr[:n], S[:n], AF.Ln, scale=1.0 / D, bias=EPS_AP(p)[:n])
        p.act(Rr[:n], Rr[:n], AF.Exp, scale=-0.5)
        if modulate:
            p.stt(sq[:n], X[:n], Rr[:n], wv[ty][:n], ALU.mult, ALU.mult, e="dve")
            if router:
                p.tt(sq[:n], sq[:n], sh[ty][:n], ALU.add, e="pool")
                p.copy(O[:n], sq[:n], e="act")
            else:
                p.tt(O[:n], sq[:n], sh[ty][:n], ALU.add, e="pool")
        else:
            p.stt(O[:n], X[:n], Rr[:n], wv[ty][:n], ALU.mult, ALU.mult, e="dve")
        p.dma("sp", dst, O[:n])
        if router:
            for g in range(8):
                pt = ptr[g % 3]
                for i in range(4):
                    kc = g * 4 + i
                    p.tr(pt[:, i * 128:i * 128 + n], sq[:n, kc * 128:(kc + 1) * 128], idt[:n, :n])
                src_v = pt.rearrange("p (a t) -> p a t", a=4)[:, :, :n]
                p.copy(hT[:, g * 4:(g + 1) * 4, :n], src_v, e=("dve", "act")[g % 2])
            for kc in range(32):
                p.mm(plg[:n], hT[:, kc, :n], wrt[:, kc, :], start=(kc == 0), stop=(kc == 31))
            A_ = af[t % 2]
            p.copy(lg[:n], plg[:n], e="dve")
            p.reduce(mx[:n], lg[:n], op=ALU.max, axis=AX.X, e="dve")
            p.ts(mx[:n], mx[:n], -1.0, ALU.mult)
            p.act(A_[:n], lg[:n], AF.Exp, bias=mx[:n], accum_out=sm[:n])
            p.recip(sm[:n], sm[:n])
            p.ts(A_[:n], A_[:n], sm[:n], ALU.mult)
            p.dma("sp", al[t * 128:(t + 1) * 128, :] if t < 8 else ac, A_[:n])
    p.finish([ol, oc] + ([al, ac] if router else []))
    return p


NPROJ = 1444
NTOK = 8704


def build_proj():
    p = P()
    NT = NTOK // 128
    hT = p.dram("hT", [NT, 128, 32, 128], BF16, "ExternalInput")
    w = p.dram("w", [128, 32, NPROJ], F32, "ExternalInput")
    o = p.dram("o", [NTOK, NPROJ], F32, "ExternalOutput")
    wb = p.sb("wb", [128, 32, NPROJ], BF16)
    stg = [p.sb("stg%d" % i, [128, 2, NPROJ]) for i in range(2)]
    for c in range(16):
        s = stg[c % 2]
        p.dma("sp", s, w[:, 2 * c:2 * c + 2, :])
        p.copy((wb[:, 2 * c:2 * c + 2, :]), s, e=("dve", "act", "pool")[c % 3]) if False else \
            p.op(("dve", "pool")[c % 2], lambda s=s, c=c: (p.nc.vector if c % 2 == 0 else p.nc.gpsimd).tensor_copy(wb[:, 2 * c:2 * c + 2, :], s), [s], [(wb, c)])
    ht = [p.sb("ht%d" % i, [128, 32, 128], BF16) for i in range(3)]
    ot = [p.sb("ot%d" % i, [128, NPROJ]) for i in range(2)]
    pst = [p.ps("pp%d" % i, [128, 512]) for i in range(6)]
    ntl = [(0, 512), (512, 512), (1024, NPROJ - 1024)]
    for t in range(NT):
        H = ht[t % 3]; O = ot[t % 2]
        p.dma("sp", H, hT[t])
        for j, (n0, nn) in enumerate(ntl):
            ps = pst[(t % 2) * 3 + j]
            for kc in range(32):
                p.mm(ps[:, :nn], H[:, kc, :], wb[:, kc, n0:n0 + nn], start=(kc == 0), stop=(kc == 31),
                     rkeys=[H, (wb, kc // 2)])
            if j == 1:
                p.copy(O[:, n0:n0 + nn], ps[:, :nn], e="act")
            else:
                p.copy(O[:, n0:n0 + nn], ps[:, :nn], e="dve")
        p.dma("sp", o[t * 128:(t + 1) * 128, :], O)
    p.finish([o])
    return p


def build_mod():
    p = P()
    NCOL = 3072
    cT = p.dram("cT", [128, 32, 3], F32, "ExternalInput")
    w = p.dram("w", [2, 128, 32, NCOL], F32, "ExternalInput")
    b = p.dram("b", [2, 1, NCOL], F32, "ExternalInput")
    o = p.dram("o", [2, 3, NCOL], F32, "ExternalOutput")
    ct = p.sb("ct", [128, 32, 3]); st = p.sb("st", [128, 32, 3])
    ones = p.sb("ones", [1, 3]); bt = p.sb("bt", [1, 2, NCOL])
    p.dma("sp", ct, cT)
    p.dma("sp", bt[:, 0, :], b[0]); p.dma("sp", bt[:, 1, :], b[1])
    p.act(st, ct, AF.Silu)
    p.memset(ones, 1.0)
    wt = [p.sb("wt%d" % i, [128, 8, 512]) for i in range(3)]
    pst = [p.ps("pm%d" % i, [3, 512]) for i in range(2)]
    ot = p.sb("ot", [3, 2, NCOL])
    i = 0
    for l in range(2):
        for nt in range(6):
            ps = pst[(l * 6 + nt) % 2]
            for kg in range(4):
                W = wt[i % 3]; i += 1
                p.dma("sp", W, w[l, :, kg * 8:(kg + 1) * 8, nt * 512:(nt + 1) * 512])
                for k in range(8):
                    p.mm(ps, st[:, kg * 8 + k, :], W[:, k, :], start=(kg == 0 and k == 0), stop=False)
            p.mm(ps, ones, bt[:, l, nt * 512:(nt + 1) * 512], start=False, stop=True)
            p.copy(ot[:, l, nt * 512:(nt + 1) * 512], ps, e="dve")
    p.dma("sp", o[0], ot[:, 0, :]); p.dma("sp", o[1], ot[:, 1, :])
    p.finish([o])
    return p


IN_SIZES = (512, 512, 1024, 1024, 32, 1024, 3072, 1024, 16, 16, 1024, 1024, 1024)
IN_OFF = np.concatenate([[0], np.cumsum(IN_SIZES)]).astype(int)


def head_cols(h):
    o = IN_OFF
    segs = [("gq", o[0] + h * 64, 64), ("gk", o[1] + h * 64, 64), ("gv", o[2] + h * 128, 128), ("gg", o[3] + h * 128, 128),
            ("glr", o[4], 32), ("fin", o[5] + h * 128, 128),
            ("dq", o[6] + h * 128, 128), ("dk", o[6] + 1024 + h * 128, 128), ("dv", o[6] + 2048 + h * 128, 128),
            ("dz", o[7] + h * 128, 128), ("bf", o[8] + h, 1), ("bb", o[8] + 8 + h, 1), ("af", o[9] + h, 1), ("ab", o[9] + 8 + h, 1),
            ("sh", o[10] + h * 128, 128), ("sb", o[11] + h * 128, 128), ("sg", o[12] + h * 128, 128)]
    idx = np.concatenate([np.arange(s, s + n) for _, s, n in segs])
    sl = {}
    pos = 0
    for nm, s, n in segs:
        sl[nm] = slice(pos, pos + n); pos += n
    assert pos == NPROJ
    return idx, sl


def tok_order(lat, ctx):
    return np.concatenate([np.concatenate([ctx[b], lat[b]], 0) for b in range(2)], 0)


TB = 4352
NCH = 68


def build_fnet_sconv():
    p = P()
    finT = p.dram("finT", [2, 128, TB], F32, "ExternalInput")
    shT = p.dram("shT", [2, 128, TB], F32, "ExternalInput")
    sbT = p.dram("sbT", [2, 128, TB], F32, "ExternalInput")
    sgT = p.dram("sgT", [2, 128, TB], F32, "ExternalInput")
    cw = p.dram("cw", [128, 3], F32, "ExternalInput")
    cs_l = p.dram("cs_l", [128, 256], F32, "ExternalInput")
    cs_c = p.dram("cs_c", [128, 256], F32, "ExternalInput")
    tab = p.dram("tab", [8, 4, 128, 2, 8, 512], BF16, "ExternalInput")
    tabc = p.dram("tabc", [128, 2, 2, 256], BF16, "ExternalInput")
    ob = p.dram("ob", [2, 128, TB], BF16, "ExternalOutput")
    od = p.dram("od", [2, 128, TB], BF16, "ExternalOutput")
    A = [p.sb("A%d" % i, [128, TB]) for i in range(4)]
    cwt = p.sb("cwt", [128, 3]); csl = p.sb("csl", [128, 256]); csc = p.sb("csc", [128, 256])
    tct = p.sb("tct", [128, 2, 2, 256], BF16)
    p.dma("sp", cwt, cw); p.dma("sp", csl, cs_l); p.dma("sp", csc, cs_c); p.dma("sp", tct, tabc)
    gcs = [p.sb("gcs%d" % b, [128, 34, 256], BF16) for b in range(2)]
    ps1 = [p.ps("f1_%d" % i, [128, 512]) for i in range(2)]
    ps2 = [p.ps("f2_%d" % i, [128, 512]) for i in range(4)]
    for b in range(2):
        p.dma("sp", A[b], finT[b])
    k = 0
    for b in range(2):
        for g in range(17):
            ps = ps1[k % 2]; k += 1
            for j in range(2):
                tcn = 2 * g + j
                p.mm(ps[:, j * 256:(j + 1) * 256], A[b][:, tcn * 128:(tcn + 1) * 128], csc if tcn < 2 else csl)
            p.copy(gcs[b][:, 2 * g:2 * g + 2, :], ps.rearrange("p (a n) -> p a n", a=2), e=("dve", "act")[k % 2])
    obt = [p.sb("obt%d" % b, [128, TB], BF16) for b in range(2)]
    for b in range(2):
        ps = ps2[b]
        i = 0
        for cs in range(2):
            for tcn in range(2):
                p.mm(ps[:, :256], gcs[b][:, tcn, cs * 128:(cs + 1) * 128], tct[:, cs, tcn, :], start=(i == 0), stop=(i == 3)); i += 1
        p.copy(obt[b][:, 0:256], ps[:, :256], e="dve")
    tb_ = [p.sb("tb%d" % i, [128, 2, 8, 512], BF16) for i in range(4)]
    k = 0
    for j in range(8):
        for tg in range(4):
            T_ = tb_[k % 4]; k += 1
            p.dma("sp", T_, tab[j, tg])
            for b in range(2):
                ps = ps2[(j % 2) * 2 + b]
                for cs in range(2):
                    for t8 in range(8):
                        tcn = 2 + tg * 8 + t8
                        p.mm(ps, gcs[b][:, tcn, cs * 128:(cs + 1) * 128], T_[:, cs, t8, :],
                             start=(tg == 0 and cs == 0 and t8 == 0), stop=(tg == 3 and cs == 1 and t8 == 7))
        for b in range(2):
            p.copy(obt[b][:, 256 + j * 512:256 + (j + 1) * 512], ps2[(j % 2) * 2 + b], e=("dve", "act")[b])
    for b in range(2):
        p.dma("sp", ob[b], obt[b])
    odt = [p.sb("odt%d" % b, [128, TB], BF16) for b in range(2)]
    for b in range(2):
        SH, SB_, SG, ACC = A[0], A[1], A[2], A[3]
        p.dma("sp", SH, shT[b]); p.dma("sp", SB_, sbT[b]); p.dma("sp", SG, sgT[b])
        p.tt(SG, SG, SH, ALU.mult, e="pool")
        p.ts(ACC, SG, cwt[:, 1:2], ALU.mult, e="dve")
        for (lo, n, rows) in ((0, 256, 1), (256, 4096, 64)):
            w_ = n // rows
            u3 = SG[:, lo:lo + n].rearrange("p (r w) -> p r w", r=rows)
            a3 = ACC[:, lo:lo + n].rearrange("p (r w) -> p r w", r=rows)
            p.stt(a3[:, :, 1:], u3[:, :, :w_ - 1], cwt[:, 0:1], a3[:, :, 1:], ALU.mult, ALU.add, e="dve")
            p.stt(a3[:, :, :w_ - 1], u3[:, :, 1:], cwt[:, 2:3], a3[:, :, :w_ - 1], ALU.mult, ALU.add, e="dve")
        p.tt(odt[b], ACC, SB_, ALU.mult, e="pool")
        p.dma("sp", od[b], odt[b])
    p.finish([ob, od])
    return p


def fnet_tables():
    key = "fnet_tables"
    if key in _CACHE:
        return _CACHE[key]
    import ml_dtypes
    bf = ml_dtypes.bfloat16
    out = {}
    c = np.arange(128)
    ang = 2 * np.pi * np.outer(c, c) / 128.0
    for nm, T in (("cs_l", 4096), ("cs_c", 256)):
        s = 1.0 / np.sqrt(T * 128.0)
        out[nm] = np.concatenate([np.cos(ang) * s, np.sin(ang) * s], 1).astype(np.float32)
    t = np.arange(4096)
    m = (np.outer(t, t) % 4096).astype(np.int64)
    base = 2 * np.pi * np.arange(4096) / 4096.0
    cosv = np.cos(base).astype(np.float32); sinv = (-np.sin(base)).astype(np.float32)
    C = cosv[m].astype(bf); S = sinv[m].astype(bf)
    def lay(M):
        return M.reshape(4, 8, 128, 8, 512).transpose(3, 0, 2, 1, 4)
    out["tab"] = np.ascontiguousarray(np.stack([lay(C), lay(S)], axis=3))
    tc = np.arange(256)
    mc = (np.outer(tc, tc) % 256)
    bc = 2 * np.pi * np.arange(256) / 256.0
    Cc = np.cos(bc)[mc].astype(bf); Sc = (-np.sin(bc))[mc].astype(bf)
    def layc(M):
        return M.reshape(2, 128, 256).transpose(1, 0, 2)
    out["tabc"] = np.ascontiguousarray(np.stack([layc(Cc), layc(Sc)], axis=1))
    _CACHE[key] = out
    return out


def const_tile(p, val, parts=128):
    key = "_c_%g" % val
    if not hasattr(p, key):
        t = p.sb("cst%d" % len([k for k in p.__dict__ if k.startswith("_c_")]), [128, 1])
        p.memset(t, val)
        setattr(p, key, t)
    return getattr(p, key)


CH_FWD = list(range(NCH))
CH_BWD = [3, 2, 1, 0] + list(range(NCH - 1, 3, -1))


def hillis(p, A, B, nparts, bwd, C=64):
    src, dst = A, B
    j = 1
    while j < C:
        s3 = src.rearrange("p (n c) -> p n c", c=C); d3 = dst.rearrange("p (n c) -> p n c", c=C)
        if not bwd:
            p.tt(d3[:nparts, :, j:], s3[:nparts, :, j:], s3[:nparts, :, :C - j], ALU.add, e="dve")
            p.copy(d3[:nparts, :, :j], s3[:nparts, :, :j], e="pool")
        else:
            p.tt(d3[:nparts, :, :C - j], s3[:nparts, :, :C - j], s3[:nparts, :, j:], ALU.add, e="dve")
            p.copy(d3[:nparts, :, C - j:], s3[:nparts, :, C - j:], e="pool")
        src, dst = dst, src
        j *= 2
    return src


def build_gla():
    p = P()
    qT = p.dram("qT", [2, 64, TB], F32, "ExternalInput")
    kT = p.dram("kT", [2, 64, TB], F32, "ExternalInput")
    vtok = p.dram("vtok", [2, 64, NCH, 128], F32, "ExternalInput")
    ggtok = p.dram("ggtok", [2, 64, NCH * 128], F32, "ExternalInput")
    lrT = p.dram("lrT", [2, 2, 16, TB], F32, "ExternalInput")
    up = p.dram("up", [2, 16, 64], F32, "ExternalInput")
    gb = p.dram("gb", [64, 2], F32, "ExternalInput")
    nwr = p.dram("nwr", [64, 128], F32, "ExternalInput")
    msk = p.dram("msk", [2, 64, 64], F32, "ExternalInput")
    idn = p.dram("idn", [64, 64], F32, "ExternalInput")
    oa = p.dram("oa", [2, 64, NCH * 128], BF16, "ExternalOutput")

    WAB = p.sb("WAB", [64, 2 * TB]); LA = WAB[:, :TB]; LB = WAB[:, TB:]
    Q_ = p.sb("Q_", [64, TB]); K_ = p.sb("K_", [64, TB]); KT = p.sb("KT", [64, TB]); AT = p.sb("AT", [64, TB])
    V = p.sb("V", [64, NCH, 128]); O = p.sb("O", [64, NCH, 128])
    upt = p.sb("upt", [16, 2, 64]); gbt = p.sb("gbt", [64, 2]); nwt = p.sb("nwt", [64, 128])
    mk = p.sb("mk", [64, 2, 64]); idt = p.sb("idt", [64, 64])
    tot = p.sb("tot", [64, NCH]); dT = p.sb("dT", [64, NCH])
    S = [p.sb("S%d" % i, [64, 128]) for i in range(3)]
    ss = p.sb("ssq", [64, NCH]); rst = p.sb("rst", [64, NCH])
    one = const_tile(p, 1.0); eps = const_tile(p, 1e-6)
    for d in range(2):
        p.dma("sp", upt[:, d, :], up[d]); p.dma("sp", mk[:, d, :], msk[d])
    p.dma("sp", gbt, gb); p.dma("sp", nwt, nwr); p.dma("sp", idt, idn)
    p.ts(gbt, gbt, -1.0, ALU.mult)
    psA = [p.ps("ga%d" % i, [64, 512]) for i in range(2)]
    psT = [p.ps("gt%d" % i, [64, 512]) for i in range(2)]
    psO = [p.ps("go%d" % i, [64, 512]) for i in range(2)]
    psI = [p.ps("gi%d" % i, [64, 128]) for i in range(2)]
    LR = AT[:16, :]
    tiles = [(i * 512, 512) for i in range(8)] + [(4096, 256)]
    for b in range(2):
        p.dma("sp", V, vtok[b])
        for d in range(2):
            bwd = d == 1
            p.dma("sp", LR, lrT[b, d])
            for i, (t0, tn) in enumerate(tiles):
                ps = psA[i % 2]
                p.mm(ps[:, :tn], upt[:, d, :], LR[:, t0:t0 + tn])
                p.act(LA[:, t0:t0 + tn], ps[:, :tn], AF.Exp, scale=-1.0, bias=gbt[:, d:d + 1])
            p.act(LA, LA, AF.Ln, bias=one[:64])
            cum = hillis(p, LA, LB, 64, bwd)
            assert cum is LA
            c3 = LA.rearrange("p (n c) -> p n c", c=64)
            p.copy(tot, c3[:, :, 0] if bwd else c3[:, :, 63], e="dve")
            p.act(dT, tot, AF.Exp, scale=-1.0 / 16)
            p.act(AT, LA, AF.Exp, scale=-1.0 / 16)
            p.dma("sp", Q_, qT[b])
            p.stt(Q_, Q_, 0.125, AT, ALU.mult, ALU.mult, e="dve")
            p.act(AT, LA, AF.Exp, scale=1.0 / 16)
            p.dma("sp", K_, kT[b])
            p.tt(K_, K_, AT, ALU.mult, e="pool")
            b3 = LB.rearrange("p (n c) -> p n c", c=64)
            p.tt(b3, c3, tot.unsqueeze(2).to_broadcast([64, NCH, 64]), ALU.subtract, e="dve")
            p.act(LB, LB, AF.Exp, scale=1.0 / 16)
            p.dma("sp", AT, kT[b])
            p.tt(LB, LB, AT, ALU.mult, e="dve")
            kt3 = KT.rearrange("p (n c) -> p n c", c=64); at3 = AT.rearrange("p (n c) -> p n c", c=64)
            for g in range(9):
                ng = min(8, NCH - g * 8)
                pt = psT[g % 2]; pa = psA[g % 2]
                for i in range(ng):
                    n = g * 8 + i
                    p.tr(pt[:, i * 64:(i + 1) * 64], LB[:, n * 64:(n + 1) * 64], idt)
                p.copy(kt3[:, g * 8:g * 8 + ng, :], pt[:, :ng * 64].rearrange("p (n c) -> p n c", c=64), e="act")
            for g in range(9):
                ng = min(8, NCH - g * 8)
                pa = psA[g % 2]
                for i in range(ng):
                    n = g * 8 + i
                    p.mm(pa[:, i * 64:(i + 1) * 64], K_[:, n * 64:(n + 1) * 64], Q_[:, n * 64:(n + 1) * 64])
                p.tt(at3[:, g * 8:g * 8 + ng, :], pa[:, :ng * 64].rearrange("p (n c) -> p n c", c=64),
                     mk[:, d:d + 1, :].to_broadcast([64, ng, 64]), ALU.mult, e="dve")
            order = CH_BWD if bwd else CH_FWD
            p.memset(S[0], 0.0)
            si = 0
            for step, n in enumerate(order):
                grp = n // 4
                po = psO[grp % 2]
                sl = po[:, (n % 4) * 128:(n % 4 + 1) * 128]
                p.mm(sl, at3[:, n, :], V[:, n, :], start=True, stop=False)
                p.mm(sl, Q_[:, n * 64:(n + 1) * 64], S[si], start=False, stop=True)
                pi = psI[step % 2]
                p.mm(pi, kt3[:, n, :], V[:, n, :])
                sn = (si + 1) % 3
                p.stt(S[sn], S[si], dT[:, n:n + 1], pi, ALU.mult, ALU.add, e="dve")
                si = sn
                last_in_grp = (n % 4 == 0) if bwd else (n % 4 == 3)
                if last_in_grp:
                    og = O[:, grp * 4:grp * 4 + 4, :]
                    pv = po.rearrange("p (n v) -> p n v", v=128)
                    if not bwd:
                        p.copy(og, pv, e="act")
                    else:
                        p.tt(og, og, pv, ALU.add, e="pool" if False else "dve")
        GG = WAB.rearrange("p (n v) -> p n v", v=128)
        p.dma("sp", WAB, ggtok[b])
        sq = Q_.rearrange("p (n c) -> p n c", c=64)
        SQ = p_view_sq = None
        h = NCH // 2
        for half, buf in ((0, Q_), (1, K_)):
            o_h = O[:, half * h:(half + 1) * h, :]
            b_h = buf.rearrange("p (n v) -> p n v", v=128)
            p.tt(b_h, o_h, o_h, ALU.mult, e="dve")
            p.reduce(ss[:, half * h:(half + 1) * h], b_h, op=ALU.add, axis=AX.X, e="dve")
        p.act(rst, ss, AF.Ln, scale=1.0 / 128, bias=eps[:64])
        p.act(rst, rst, AF.Exp, scale=-0.5)
        p.act(WAB, WAB, AF.Silu)
        p.tt(O, O, rst.unsqueeze(2).to_broadcast([64, NCH, 128]), ALU.mult, e="dve")
        p.tt(O, O, nwt.unsqueeze(1).to_broadcast([64, NCH, 128]), ALU.mult, e="pool")
        OUT = KT.bitcast(BF16).rearrange("p (n v) -> p n v", v=128)
        p.tt(OUT, O, GG, ALU.mult, e="dve")
        p.dma("sp", oa[b], KT.bitcast(BF16))
    p.finish([oa])
    return p


def build_gdn_pre():
    p = P()
    raw = p.dram("raw", [2, 3, 64, NCH, 128], F32, "ExternalInput")
    rawc = p.dram("rawc", [2, 3, 256, 128], F32, "ExternalInput")
    cwr = p.dram("cwr", [3, 64, 3, 128], F32, "ExternalInput")
    o = p.dram("o", [2, 3, 64, NCH * 128], F32, "ExternalOutput")
    RAW = [p.sb("RAW%d" % i, [64, NCH, 128]) for i in range(2)]
    ACC = [p.sb("ACC%d" % i, [64, NCH, 128]) for i in range(2)]
    TMP = p.sb("TMP", [64, NCH, 128])
    M1 = p.sb("M1", [64, 4, 128]); P1 = p.sb("P1", [64, 4, 128]); TC = p.sb("TC", [64, 4, 128])
    cw = p.sb("cw", [64, 3, 3, 128])
    ss = p.sb("ss", [64, NCH]); rn = p.sb("rn", [64, NCH])
    eps = const_tile(p, 1e-6)
    for c in range(3):
        p.dma("sp", cw[:, c], cwr[c])
    k = 0
    for b in range(2):
        for c in range(3):
            R_ = RAW[k % 2]; A_ = ACC[k % 2]; k += 1
            p.dma("sp", R_, raw[b, c])
            p.memset(M1, 0.0, e="pool"); p.memset(P1, 0.0, e="pool")
            src = rawc[b, c]
            p.dma("sp", M1[1:64, 0, :], src[0:63, :])
            p.dma("sp", M1[:, 1:4, :], src[63:255, :].rearrange("(n s) c -> s n c", s=64))
            p.dma("sp", P1[:, 0:3, :], src[1:193, :].rearrange("(n s) c -> s n c", s=64))
            p.dma("sp", P1[0:63, 3, :], src[193:256, :])
            w0 = cw[:, c, 0:1, :]; w1 = cw[:, c, 1:2, :]; w2 = cw[:, c, 2:3, :]
            p.tt(A_, R_, w1.to_broadcast([64, NCH, 128]), ALU.mult, e="dve")
            p.tt(TMP[:, 5:68], R_[:, 4:67], w0.to_broadcast([64, 63, 128]), ALU.mult, e="pool")
            p.tt(A_[:, 5:68], A_[:, 5:68], TMP[:, 5:68], ALU.add, e="dve")
            p.tt(TMP[:, 4:67], R_[:, 5:68], w2.to_broadcast([64, 63, 128]), ALU.mult, e="pool")
            p.tt(A_[:, 4:67], A_[:, 4:67], TMP[:, 4:67], ALU.add, e="dve")
            p.tt(TC, M1, w0.to_broadcast([64, 4, 128]), ALU.mult, e="pool")
            p.tt(A_[:, 0:4], A_[:, 0:4], TC, ALU.add, e="dve")
            p.tt(TC, P1, w2.to_broadcast([64, 4, 128]), ALU.mult, e="pool")
            p.tt(A_[:, 0:4], A_[:, 0:4], TC, ALU.add, e="dve")
            p.act(A_, A_, AF.Silu)
            if c < 2:
                p.tt(TMP, A_, A_, ALU.mult, e="pool")
                p.reduce(ss, TMP, op=ALU.add, axis=AX.X, e="dve")
                p.act(rn, ss, AF.Ln, bias=eps[:64])
                p.act(rn, rn, AF.Exp, scale=-0.5)
                if c == 0:
                    p.ts(rn, rn, float(128 ** -0.5), ALU.mult)
                p.tt(A_, A_, rn.unsqueeze(2).to_broadcast([64, NCH, 128]), ALU.mult, e="dve")
            p.dma("sp", o[b, c], A_.rearrange("p n c -> p (n c)"))
    p.finish([o])
    return p


GROUPS = [(0, 4)] + [(4 + 8 * i, 8) for i in range(8)]


def build_gdn_main():
    p = P()
    qT = p.dram("qT", [2, 128, TB], F32, "ExternalInput")
    kT = p.dram("kT", [2, 128, TB], F32, "ExternalInput")
    ktok = p.dram("ktok", [2, 64, NCH, 128], F32, "ExternalInput")
    vtok = p.dram("vtok", [2, 64, NCH, 128], F32, "ExternalInput")
    atok = p.dram("atok", [2, 2, 64, NCH], F32, "ExternalInput")
    btok = p.dram("btok", [2, 2, 64, NCH], F32, "ExternalInput")
    ztok = p.dram("ztok", [2, 64, NCH * 128], F32, "ExternalInput")
    prm = p.dram("prm", [128, 4], F32, "ExternalInput")
    nwr = p.dram("nwr", [64, 128], F32, "ExternalInput")
    msk = p.dram("msk", [4, 64, 64], F32, "ExternalInput")
    idn = p.dram("idn", [64, 64], F32, "ExternalInput")
    oc = p.dram("oc", [2, 64, NCH * 128], BF16, "ExternalOutput")

    QT = p.sb("QT", [128, TB]); KTs = p.sb("KTs", [128, TB])
    O = p.sb("O", [64, NCH, 128]); Z = p.sb("Z", [64, NCH * 128]); SQ = p.sb("SQ", [64, TB])
    prt = p.sb("prt", [128, 4]); nwt = p.sb("nwt", [64, 128]); mk = p.sb("mk", [64, 4, 64]); idt = p.sb("idt", [64, 64])
    ones = p.sb("ones", [64, 128])
    one = const_tile(p, 1.0); eps = const_tile(p, 1e-6)
    p.dma("sp", prt, prm); p.dma("sp", nwt, nwr); p.dma("sp", idt, idn)
    for i in range(4):
        p.dma("sp", mk[:, i, :], msk[i])
    p.memset(ones, 1.0)
    expA = p.sb("expA", [128, 2])
    p.act(expA, prt[:, 2:4], AF.Exp)
    At = p.sb("At", [64, NCH]); Bt = p.sb("Bt", [64, NCH]); L = p.sb("L", [64, NCH])
    beta = p.sb("beta", [64, NCH]); nbeta = p.sb("nbeta", [64, NCH]); eg = p.sb("eg", [64, NCH])
    tot = p.sb("tot", [64, NCH]); kes = p.sb("kes", [64, NCH]); bw = p.sb("bw", [64, NCH]); dS = p.sb("dS", [128, NCH])
    ss = p.sb("ssq", [64, NCH]); rst = p.sb("rst", [64, NCH])
    def g64(nm):
        return p.sb(nm, [64, 8, 64])
    laU = g64("laU"); laV = g64("laV"); LM = g64("LM"); LMT = g64("LMT"); TMPg = g64("TMPg")
    Xa = g64("Xa"); XTa = g64("XTa"); Xb = g64("Xb"); XTb = g64("XTb"); QKM = g64("QKM")
    R = p.sb("R", [64, 8, 256])
    KG = p.sb("KG", [64, 8, 128]); VG = p.sb("VG", [64, 8, 128]); KE = p.sb("KE", [64, 8, 128]); VN = p.sb("VN", [64, 8, 128])
    WT = p.sb("WT", [128, 8, 64]); QG = p.sb("QG", [128, 512]); EGR = p.sb("EGR", [128, 512])
    S = [p.sb("S%d" % i, [128, 128]) for i in range(3)]
    PA = [p.ps("PA%d" % i, [64, 1024]) for i in range(2)]; PB = [p.ps("PB%d" % i, [128, 512]) for i in range(2)]
    PC = p.ps("PC", [128, 512]); PD = p.ps("PD", [64, 512])
    PCv = PC[:64, 0:128]; PCs = PC[:, 128:256]

    def fl(t, ng):
        return t[:, :ng, :].rearrange("p n c -> p (n c)")

    for b in range(2):
        p.dma("sp", QT, qT[b]); p.dma("sp", KTs, kT[b])
        for d in range(2):
            bwd = d == 1
            Um = mk[:, 2 * d, :]; Vm = mk[:, 2 * d + 1, :]
            p.dma("sp", At, atok[b, d]); p.dma("sp", Bt, btok[b, d])
            p.act(L, At, AF.Exp, bias=prt[:64, d:d + 1])
            p.act(L, L, AF.Ln, bias=one[:64])
            p.ts(L, L, expA[:64, d:d + 1], ALU.mult)
            p.act(beta, Bt, AF.Exp, scale=-1.0)
            p.ts(beta, beta, 1.0, ALU.add)
            p.recip(beta, beta)
            p.ts(nbeta, beta, -1.0, ALU.mult)
            p.mm(PB[0][:64, :NCH], Um, L)
            p.mm(PB[1][:, :NCH], ones, L)
            p.act(eg, PB[0][:64, :NCH], AF.Exp, scale=-1.0)
            p.act(dS, PB[1][:, :NCH], AF.Exp, scale=-1.0)
            p.copy(tot, PB[1][:64, :NCH], e="dve")
            p.tt(kes, tot, PB[0][:64, :NCH], ALU.subtract)
            p.act(kes, kes, AF.Exp, scale=-1.0)
            p.tt(bw, beta, eg, ALU.mult)
            p.memset(S[0], 0.0)
            si = 0
            gorder = ([GROUPS[0]] + GROUPS[:0:-1]) if bwd else GROUPS
            for (n0, ng) in gorder:
                t0 = n0 * 64; tn = ng * 64
                p.dma("sp", KG[:, :ng], ktok[b, :, n0:n0 + ng, :]); p.dma("sp", VG[:, :ng], vtok[b, :, n0:n0 + ng, :])
                Lg = L[:, n0:n0 + ng].unsqueeze(2).to_broadcast([64, ng, 64])
                p.tt(laU[:, :ng], Um.unsqueeze(1).to_broadcast([64, ng, 64]), Lg, ALU.mult, e="pool")
                p.tt(laV[:, :ng], Vm.unsqueeze(1).to_broadcast([64, ng, 64]), Lg, ALU.mult, e="pool")
                p.mm(PB[0][:64, :tn], Um, fl(laV, ng))
                p.act(fl(LM, ng), PB[0][:64, :tn], AF.Exp, scale=-1.0)
                p.tt(LM[:, :ng], LM[:, :ng], Vm.unsqueeze(1).to_broadcast([64, ng, 64]), ALU.mult, e="pool")
                p.mm(PB[1][:64, :tn], Vm, fl(laU, ng))
                p.act(fl(LMT, ng), PB[1][:64, :tn], AF.Exp, scale=-1.0)
                p.tt(LMT[:, :ng], LMT[:, :ng], Um.unsqueeze(1).to_broadcast([64, ng, 64]), ALU.mult, e="pool")
                for i in range(ng):
                    ks = KTs[:, t0 + i * 64:t0 + (i + 1) * 64]
                    p.mm(PB[0][:64, i * 64:(i + 1) * 64], ks, ks)
                p.tt(fl(TMPg, ng), PB[0][:64, :tn], fl(LM, ng), ALU.mult, e="dve")
                p.tt(Xa[:, :ng], TMPg[:, :ng], nbeta[:, n0:n0 + ng].unsqueeze(2).to_broadcast([64, ng, 64]), ALU.mult, e="dve")
                for i in range(ng):
                    p.tr(PB[1][:64, i * 64:(i + 1) * 64], Xa[:, i, :], idt)
                p.copy(fl(XTa, ng), PB[1][:64, :tn], e="act")
                p.tt(R[:, :ng, 0:128], VG[:, :ng], beta[:, n0:n0 + ng].unsqueeze(2).to_broadcast([64, ng, 128]), ALU.mult, e="pool")
                p.tt(R[:, :ng, 128:256], KG[:, :ng], bw[:, n0:n0 + ng].unsqueeze(2).to_broadcast([64, ng, 128]), ALU.mult, e="pool")
                p.tt(KE[:, :ng], KG[:, :ng], kes[:, n0:n0 + ng].unsqueeze(2).to_broadcast([64, ng, 128]), ALU.mult, e="pool")
                X, XT, Xn, XTn = Xa, XTa, Xb, XTb
                for lev in range(6):
                    for hf in range((ng + 3) // 4):
                        Rh = R[:, hf * 4:hf * 4 + 4, :]
                        for i in range(4):
                            p.mm(PA[hf][:, i * 256:(i + 1) * 256], XT[:, hf * 4 + i, :], (R[:, hf * 4 + i, :], hf))
                        Rf = Rh.rearrange("p n c -> p (n c)")
                        p.tt((Rf, hf), (Rf, hf), PA[hf], ALU.add, e="dve")
                    if lev < 5:
                        for i in range(ng):
                            p.mm(PB[0][:64, i * 64:(i + 1) * 64], XT[:, i, :], X[:, i, :])
                        for i in range(ng):
                            p.mm(PB[1][:64, i * 64:(i + 1) * 64], X[:, i, :], XT[:, i, :])
                        p.copy(fl(Xn, ng), PB[0][:64, :tn], e="act")
                        p.copy(fl(XTn, ng), PB[1][:64, :tn], e="pool" if False else "act")
                        X, XT, Xn, XTn = Xn, XTn, X, XT
                for i in range(ng):
                    p.tr(PB[0][:, i * 64:(i + 1) * 64], R[:, i, 128:256], idt)
                p.copy(fl(WT, ng), PB[0][:, :tn], e="act")
                for i in range(ng):
                    sl_ = slice(t0 + i * 64, t0 + (i + 1) * 64)
                    p.mm(PB[1][:64, i * 64:(i + 1) * 64], KTs[:, sl_], QT[:, sl_])
                p.tt(fl(QKM, ng), PB[1][:64, :tn], fl(LMT, ng), ALU.mult, e="dve")
                p.mm(PB[0][:, :tn], ones, fl(laU, ng))
                p.act(EGR[:, :tn], PB[0][:, :tn], AF.Exp, scale=-1.0)
                p.tt(QG[:, :tn], QT[:, t0:t0 + tn], EGR[:, :tn], ALU.mult, e="dve")
                corder = range(ng - 1, -1, -1) if bwd else range(ng)
                for i in corder:
                    n = n0 + i
                    p.mm(PCv, WT[:, i, :], S[si])
                    p.tt(VN[:, i, :], R[:, i, 0:128], PCv, ALU.subtract, e="dve")
                    slot = n % 4
                    osl = PD[:, slot * 128:(slot + 1) * 128]
                    p.mm(osl, QG[:, i * 64:(i + 1) * 64], S[si], start=True, stop=False)
                    p.mm(osl, QKM[:, i, :], VN[:, i, :], start=False, stop=True)
                    p.mm(PCs, KE[:, i, :], VN[:, i, :])
                    sn = (si + 1) % 3
                    p.stt(S[sn], S[si], dS[:, n:n + 1], PCs, ALU.mult, ALU.add, e="dve")
                    si = sn
                    if (slot == 0) if bwd else (slot == 3):
                        grp = n // 4
                        og = O[:, grp * 4:grp * 4 + 4, :]
                        pv = PD.rearrange("p (n v) -> p n v", v=128)
                        if not bwd:
                            p.copy(og, pv, e="act")
                        else:
                            p.tt(og, og, pv, ALU.add, e="pool" if False else "dve")
        p.dma("sp", Z, ztok[b])
        h = NCH // 2
        for half in range(2):
            o_h = O[:, half * h:(half + 1) * h, :]
            b_h = SQ.rearrange("p (n v) -> p n v", v=128)
            p.tt(b_h, o_h, o_h, ALU.mult, e="dve")
            p.reduce(ss[:, half * h:(half + 1) * h], b_h, op=ALU.add, axis=AX.X, e="dve")
        p.act(rst, ss, AF.Ln, scale=1.0 / 128, bias=eps[:64])
        p.act(rst, rst, AF.Exp, scale=-0.5)
        p.act(Z, Z, AF.Silu)
        p.tt(O, O, rst.unsqueeze(2).to_broadcast([64, NCH, 128]), ALU.mult, e="dve")
        p.tt(O, O, nwt.unsqueeze(1).to_broadcast([64, NCH, 128]), ALU.mult, e="pool")
        OUT = SQ.bitcast(BF16).rearrange("p (n v) -> p n v", v=128)
        p.tt(OUT, O, Z.rearrange("p (n v) -> p n v", v=128), ALU.mult, e="dve")
        p.dma("sp", oc[b], SQ.bitcast(BF16))
    p.finish([oc])
    return p


def build_wout():
    p = P()
    NTK = 1088
    mixT = p.dram("mixT", [128, 32, NTK], BF16, "ExternalInput")
    w = p.dram("w", [128, 32, D], F32, "ExternalInput")
    xl = p.dram("xl", [1024, D], F32, "ExternalInput"); xc = p.dram("xc", [64, D], F32, "ExternalInput")
    g_l = p.dram("g_l", [128, D], F32, "ExternalInput"); g_c = p.dram("g_c", [128, D], F32, "ExternalInput")
    ol = p.dram("ol", [1024, D], F32, "ExternalOutput"); oc = p.dram("oc", [64, D], F32, "ExternalOutput")
    MT = p.sb("MT", [128, 32, NTK], BF16)
    for q in range(4):
        p.dma("sp", (MT[:, q * 8:(q + 1) * 8, :], q), mixT[:, q * 8:(q + 1) * 8, :])
    WB = [p.sb("WB%d" % i, [128, 32, 512], BF16) for i in range(2)]
    stg = [p.sb("stg%d" % i, [128, 8, 512]) for i in range(2)]
    gl = [p.sb("gl%d" % i, [128, 512]) for i in range(2)]; gc = [p.sb("gc%d" % i, [128, 512]) for i in range(2)]
    xt = [p.sb("xt%d" % i, [128, 512]) for i in range(3)]; tm = [p.sb("tm%d" % i, [128, 512]) for i in range(2)]
    ot = [p.sb("ot%d" % i, [128, 512]) for i in range(3)]
    pst = [p.ps("pw%d" % i, [128, 512]) for i in range(4)]
    k = 0; u = 0
    for j in range(8):
        cs = slice(j * 512, (j + 1) * 512)
        Wb = WB[j % 2]
        for q in range(4):
            s_ = stg[k % 2]; k += 1
            p.dma("sp", s_, w[:, q * 8:(q + 1) * 8, cs])
            e = ("dve", "pool")[q % 2]
            p.copy((Wb[:, q * 8:(q + 1) * 8, :], q), s_, e=e)
        GL = gl[j % 2]; GC = gc[j % 2]
        p.dma("sp", GL, g_l[:, cs]); p.dma("sp", GC, g_c[:, cs])
        for t in range(9):
            n = 128 if t < 8 else 64
            X = xt[u % 3]; O_ = ot[u % 3]; T_ = tm[u % 2]; ps = pst[u % 4]; u += 1
            src = xl[t * 128:(t + 1) * 128, cs] if t < 8 else xc[:, cs]
            dst = ol[t * 128:(t + 1) * 128, cs] if t < 8 else oc[:, cs]
            p.dma("sp", X[:n], src)
            for kc in range(32):
                p.mm(ps[:n], (MT[:, kc, t * 128:t * 128 + n], kc // 8), (Wb[:, kc, :], kc // 8), start=(kc == 0), stop=(kc == 31))
            p.tt(T_[:n], ps[:n], (GL if t < 8 else GC)[:n], ALU.mult, e="dve")
            p.tt(O_[:n], T_[:n], X[:n], ALU.add, e="pool")
            p.dma("sp", dst, O_[:n])
    p.finish([ol, oc])
    return p


def _idma(p, out, in_dram, idx_ap, bounds=None):
    q = "pool"
    i = p.dnext
    p.dnext = (p.dnext + 1) % len(p.dsem)
    if p.dval[i] > 0:
        p._wait(q, ("d", i, p.dval[i]))
    reads = [in_dram, idx_ap]; writes = [out]
    p._deps(q, reads, writes)
    kw = {}
    if bounds is not None:
        rk_ = "_breg_%d" % int(bounds)
        if not hasattr(p, rk_):
            setattr(p, rk_, p.nc.gpsimd.to_reg(int(bounds)))
        kw = {"bounds_check": getattr(p, rk_), "oob_is_err": False}
    ins = p.nc.gpsimd.indirect_dma_start(out=p._a(out), out_offset=None, in_=p._a(in_dram),
                                         in_offset=bass.IndirectOffsetOnAxis(ap=p._a(idx_ap), axis=0), **kw)
    p.dval[i] += 16
    ins.then_inc(p.dsem[i], 16)
    tok = ("d", i, p.dval[i])
    p._commit(tok, reads, writes)
    p.n_ins += 1
    return tok


def build_route():
    p = P()
    arow = p.dram("arow", [4, 128, 4096], F32, "ExternalInput"); acol = p.dram("acol", [4, 128, 32], F32, "ExternalInput")
    arowc = p.dram("arowc", [4, 128, 256], F32, "ExternalInput"); acolc = p.dram("acolc", [4, 128, 2], F32, "ExternalInput")
    iota = p.dram("iota", [128, 512], F32, "ExternalInput"); tv = p.dram("tv", [128, 32], F32, "ExternalInput")
    idx = p.dram("idx", [4, 128, 4], I32, "ExternalOutput"); gate = p.dram("gate", [4, 128, 4], F32, "ExternalOutput")
    rank = p.dram("rank", [4, 128, 32], F32, "ExternalOutput")
    idxc = p.dram("idxc", [4, 32, 1], I32, "ExternalOutput"); gatec = p.dram("gatec", [4, 32, 1], F32, "ExternalOutput")
    rankc = p.dram("rankc", [4, 128, 2], F32, "ExternalOutput")
    io = p.sb("io", [128, 512]); tvt = p.sb("tvt", [128, 32])
    p.dma("sp", io, iota); p.dma("sp", tvt, tv)
    AR = [p.sb("AR%d" % i, [128, 4096]) for i in range(2)]; junk = p.sb("junk", [128, 4096]); junk2 = p.sb("junk2", [128, 4096])
    NAC = p.sb("NAC", [128, 32])
    AC = [p.sb("AC%d" % i, [128, 32]) for i in range(2)]; RK = [p.sb("RK%d" % i, [128, 32]) for i in range(2)]
    TV2 = [p.sb("TV2_%d" % i, [128, 32, 2]) for i in range(2)]
    OH = p.sb("OH", [128, 32, 512])
    PS = [p.ps("pr%d" % i, [128, 8]) for i in range(2)]
    IG = [p.sb("IG%d" % i, [128, 4, 2]) for i in range(2)]; II = [p.sb("II%d" % i, [128, 4], I32) for i in range(2)]
    GG = [p.sb("GG%d" % i, [128, 4]) for i in range(2)]
    ARc = [p.sb("ARc%d" % i, [128, 256]) for i in range(2)]; ACc = [p.sb("ACc%d" % i, [128, 2]) for i in range(2)]
    RKc = [p.sb("RKc%d" % i, [128, 2]) for i in range(2)]; TV2c = [p.sb("TV2c%d" % i, [128, 2, 2]) for i in range(2)]
    OHc = p.sb("OHc", [128, 2, 32]); IGc = [p.sb("IGc%d" % i, [32, 2]) for i in range(2)]
    IIc = [p.sb("IIc%d" % i, [32, 1], I32) for i in range(2)]; GGc = [p.sb("GGc%d" % i, [32, 1]) for i in range(2)]
    for q in range(4):
        A = AR[q % 2]; C = AC[q % 2]; R_ = RK[q % 2]; T2 = TV2[q % 2]; ps = PS[q % 2]
        p.dma("sp", A, arow[q]); p.dma("sp", C, acol[q])
        p.memset(R_, 0.0, e="pool")
        p.ts(NAC, C, -1.0, ALU.mult, e="pool")
        for tc in range(32):
            if tc % 2 == 0:
                p.ts(junk, A, C[:, tc:tc + 1], ALU.is_gt, 0.0, ALU.add, accum_out=R_[:, tc:tc + 1], e="dve")
            else:
                p.act(junk2, A, AF.Sign, bias=NAC[:, tc:tc + 1], accum_out=R_[:, tc:tc + 1])
        rodd = R_.rearrange("p (a two) -> p a two", two=2)[:, :, 1]
        p.ts(rodd, rodd, 0.5, ALU.mult, 2047.5, ALU.add, e="dve")
        p.copy(T2[:, :, 0], tvt, e="pool"); p.copy(T2[:, :, 1], C, e="pool")
        for tc in range(32):
            p.ts(OH[:, tc, :], io, R_[:, tc:tc + 1], ALU.is_equal, e="pool" if tc % 2 else "dve")
        for rs in range(4):
            for tc in range(32):
                p.mm(ps[:, rs * 2:rs * 2 + 2], OH[:, tc, rs * 128:(rs + 1) * 128], T2[:, tc, :], start=(tc == 0), stop=(tc == 31))
        p.copy(IG[q % 2], ps.rearrange("p (a c) -> p a c", c=2), e="dve")
        p.copy(II[q % 2], IG[q % 2][:, :, 0], e="dve")
        p.copy(GG[q % 2], IG[q % 2][:, :, 1], e="dve")
        p.dma("sp", idx[q], II[q % 2]); p.dma("sp", gate[q], GG[q % 2]); p.dma("sp", rank[q], R_)
        A = ARc[q % 2]; C = ACc[q % 2]; R_ = RKc[q % 2]; T2 = TV2c[q % 2]
        p.dma("sp", A, arowc[q]); p.dma("sp", C, acolc[q])
        p.memset(R_, 0.0, e="pool")
        for tc in range(2):
            p.ts(junk[:, :256], A, C[:, tc:tc + 1], ALU.is_gt, 0.0, ALU.add, accum_out=R_[:, tc:tc + 1], e="dve")
        p.copy(T2[:, :, 0], tvt[:, 0:2], e="pool"); p.copy(T2[:, :, 1], C, e="pool")
        for tc in range(2):
            p.ts(OHc[:, tc, :], io[:, :32], R_[:, tc:tc + 1], ALU.is_equal, e="dve")
        for tc in range(2):
            p.mm(ps[:32, 0:2], OHc[:, tc, :], T2[:, tc, :], start=(tc == 0), stop=(tc == 1))
        p.copy(IGc[q % 2], ps[:32, 0:2], e="dve")
        p.copy(IIc[q % 2], IGc[q % 2][:, 0:1], e="dve"); p.copy(GGc[q % 2], IGc[q % 2][:, 1:2], e="dve")
        p.dma("sp", idxc[q], IIc[q % 2]); p.dma("sp", gatec[q], GGc[q % 2]); p.dma("sp", rankc[q], R_)
    p.finish([idx, gate, rank, idxc, gatec, rankc])
    return p


DFF = 1024


def build_experts():
    p = P()
    idx = p.dram("idx", [4, 128, 4], I32, "ExternalInput"); gate = p.dram("gate", [4, 128, 4], F32, "ExternalInput")
    idxc = p.dram("idxc", [4, 32, 1], I32, "ExternalInput"); gatec = p.dram("gatec", [4, 32, 1], F32, "ExternalInput")
    h2 = [p.dram("h2_%d" % b, [4096, D], BF16, "ExternalInput") for b in range(2)]
    h2c = [p.dram("h2c_%d" % b, [256, D], BF16, "ExternalInput") for b in range(2)]
    wg = p.dram("wg", [2, 8, 128, 32 * 128], F32, "ExternalInput"); wu = p.dram("wu", [2, 8, 128, 32 * 128], F32, "ExternalInput")
    wd = p.dram("wd", [2, 8, 128, 8 * 512], F32, "ExternalInput")
    idn = p.dram("idn", [128, 128], BF16, "ExternalInput")
    yg = p.dram("yg", [4, 512, D], BF16, "ExternalOutput"); ygc = p.dram("ygc", [4, 32, D], BF16, "ExternalOutput")
    idt = p.sb("idt", [128, 128], BF16); p.dma("sp", idt, idn)
    IDX = p.sb("IDX", [128, 4, 4], I32); G = p.sb("G", [128, 4, 4]); IDXc = p.sb("IDXc", [32, 4], I32); Gc = p.sb("Gc", [32, 4])
    for q in range(4):
        p.dma("sp", IDX[:, q, :], idx[q]); p.dma("sp", G[:, q, :], gate[q])
        p.dma("sp", IDXc[:, q:q + 1], idxc[q]); p.dma("sp", Gc[:, q:q + 1], gatec[q])
    XG = p.sb("XG", [128, 4, D], BF16); XGc = p.sb("XGc", [32, D], BF16)
    XT = [p.sb("XT%d" % b, [128, 32, 512], BF16) for b in range(2)]; XTc = p.sb("XTc", [128, 32, 64], BF16)
    HT = [p.sb("HT%d" % b, [128, 8, 512], BF16) for b in range(2)]; HTc = p.sb("HTc", [128, 8, 64], BF16)
    stg = [p.sb("stg%d" % i, [128, 4096]) for i in range(2)]
    wb = [p.sb("wb%d" % i, [128, 4096], BF16) for i in range(4)]
    HS = [p.sb("HS%d" % i, [128, 512]) for i in range(2)]
    Y = [p.sb("Y%d" % i, [128, 512], BF16) for i in range(3)]
    PT = [p.ps("pt%d" % i, [128, 1024], BF16) for i in range(2)]
    PG = [p.ps("pg%d" % i, [128, 512]) for i in range(2)]; PU = [p.ps("pu%d" % i, [128, 512]) for i in range(2)]
    PY = [p.ps("py%d" % i, [128, 512]) for i in range(2)]
    sk = [0]; wk = [0]

    def load_w(src):
        s_ = stg[sk[0] % 2]; sk[0] += 1
        W = wb[wk[0] % 4]; wk[0] += 1
        p.dma("sp", s_, src)
        p.copy(W[:, :2048], s_[:, :2048], e="dve"); p.copy(W[:, 2048:], s_[:, 2048:], e="pool" if False else "act")
        return W

    for e in range(2):
        for b in range(2):
            q = e * 2 + b
            for rs in range(4):
                _idma(p, XG[:, rs, :], h2[b], IDX[:, q, rs:rs + 1])
            k = 0
            for rs in range(4):
                for g8 in range(4):
                    pt = PT[k % 2]; k += 1
                    for i in range(8):
                        kc = g8 * 8 + i
                        p.tr(pt[:, i * 128:(i + 1) * 128], XG[:, rs, kc * 128:(kc + 1) * 128], idt)
                    p.copy(XT[b][:, g8 * 8:(g8 + 1) * 8, rs * 128:(rs + 1) * 128], pt.rearrange("p (a r) -> p a r", a=8),
                           e=("dve", "act")[k % 2])
            _idma(p, XGc, h2c[b], IDXc[:, q:q + 1])
            for g8 in range(4):
                pt = PT[k % 2]; k += 1
                for i in range(8):
                    kc = g8 * 8 + i
                    p.tr(pt[:, i * 32:(i + 1) * 32], XGc[:, kc * 128:(kc + 1) * 128], idt[:32, :32])
                p.copy(XTc[:, g8 * 8:(g8 + 1) * 8, b * 32:(b + 1) * 32], pt[:, :256].rearrange("p (a r) -> p a r", a=8), e="dve")
        u = 0
        for ft in range(8):
            WG = load_w(wg[e, ft]).rearrange("p (k f) -> p k f", f=128)
            WU = load_w(wu[e, ft]).rearrange("p (k f) -> p k f", f=128)
            for b in range(3):
                rhs = XT[b] if b < 2 else XTc
                n = 512 if b < 2 else 64
                pg = PG[u % 2]; pu = PU[u % 2]; hs = HS[u % 2]; u += 1
                for kc in range(32):
                    p.mm(pg[:, :n], WG[:, kc, :], rhs[:, kc, :], start=(kc == 0), stop=(kc == 31))
                for kc in range(32):
                    p.mm(pu[:, :n], WU[:, kc, :], rhs[:, kc, :], start=(kc == 0), stop=(kc == 31))
                p.act(hs[:, :n], pg[:, :n], AF.Silu)
                dst = HT[b][:, ft, :] if b < 2 else HTc[:, ft, :]
                p.tt(dst, hs[:, :n], pu[:, :n], ALU.mult, e="dve")
        v = 0
        for dt in range(8):
            WD = load_w(wd[e, dt]).rearrange("p (k d) -> p k d", d=512)
            for b in range(2):
                q = e * 2 + b
                for rs in range(4):
                    py = PY[v % 2]; y = Y[v % 3]; v += 1
                    for fc in range(8):
                        p.mm(py, HT[b][:, fc, rs * 128:(rs + 1) * 128], WD[:, fc, :], start=(fc == 0), stop=(fc == 7))
                    p.ts(y, py, G[:, q, rs:rs + 1], ALU.mult, e=("dve", "act")[0])
                    p.dma("sp", yg[q, rs * 128:(rs + 1) * 128, dt * 512:(dt + 1) * 512], y)
                py = PY[v % 2]; y = Y[v % 3]; v += 1
                for fc in range(8):
                    p.mm(py[:32], HTc[:, fc, b * 32:(b + 1) * 32], WD[:, fc, :], start=(fc == 0), stop=(fc == 7))
                p.ts(y[:32], py[:32], Gc[:, q:q + 1], ALU.mult, e="dve")
                p.dma("sp", ygc[q, :, dt * 512:(dt + 1) * 512], y[:32])
    p.finish([yg, ygc])
    return p


def build_combine():
    p = P()
    xl = p.dram("xl", [1024, D], F32, "ExternalInput"); xc = p.dram("xc", [64, D], F32, "ExternalInput")
    g_l = p.dram("g_l", [128, D], F32, "ExternalInput"); g_c = p.dram("g_c", [128, D], F32, "ExternalInput")
    rk = p.dram("rk", [128, 9, 16], F32, "ExternalInput")
    eo = p.dram("eo", [128, 2, 16], F32, "ExternalInput")
    yall = p.dram("yall", [16 * 512, D], BF16, "ExternalInput")
    ycall = p.dram("ycall", [16 * 32, D], BF16, "ExternalInput")
    idn = p.dram("idn", [128, 128], BF16, "ExternalInput")
    ol = p.dram("ol", [1024, D], F32, "ExternalOutput"); oc = p.dram("oc", [64, D], F32, "ExternalOutput")
    BIG = 1.0e6
    GL = p.sb("GL", [128, D]); GC = p.sb("GC", [128, D]); idt = p.sb("idt", [128, 128], BF16)
    p.dma("sp", GL, g_l); p.dma("sp", GC, g_c); p.dma("sp", idt, idn)
    RK = p.sb("RK", [128, 9, 16]); EO = p.sb("EO", [128, 2, 16]); SEL = p.sb("SEL", [128, 9, 16]); IDF = p.sb("IDF", [128, 9, 16])
    IDI = p.sb("IDI", [128, 9, 16], I32)
    p.dma("sp", RK, rk); p.dma("sp", EO, eo)
    for (t0, t1, cap, ty) in ((0, 8, 512, 0), (8, 9, 32, 1)):
        r = RK[:, t0:t1, :]
        p.ts(SEL[:, t0:t1, :], r, float(cap), ALU.is_lt)
        p.tt(IDF[:, t0:t1, :], r, EO[:, ty:ty + 1, :].to_broadcast([128, t1 - t0, 16]), ALU.add)
        p.tt(IDF[:, t0:t1, :], IDF[:, t0:t1, :], SEL[:, t0:t1, :], ALU.mult)
        p.ts(IDF[:, t0:t1, :], IDF[:, t0:t1, :], BIG, ALU.add)
    p.copy(IDI, IDF)
    NG = 6
    GB = [p.sb("GB%d" % i, [128, D], BF16) for i in range(NG)]
    X = [p.sb("X%d" % i, [128, D]) for i in range(2)]
    TM = [p.sb("TM%d" % i, [128, 512]) for i in range(2)]
    PS = [p.ps("pc%d" % i, [128, 512]) for i in range(8)]
    k = 0; u = 0
    for t in range(9):
        n = 128 if t < 8 else 64
        src = xl[t * 128:(t + 1) * 128, :] if t < 8 else xc
        dst = ol[t * 128:(t + 1) * 128, :] if t < 8 else oc
        ysrc = yall if t < 8 else ycall
        nrows = 16 * 512 if t < 8 else 16 * 32
        Xt = X[t % 2]
        p.dma("sp", Xt[:n], src)
        for e in range(16):
            gb = GB[k % NG]; k += 1
            p.memset(gb[:n], 0.0, e="dve")
            _idma(p, gb[:n], ysrc, IDI[:n, t, e:e + 1], bounds=nrows - 1)
            for ct in range(8):
                p.mm(PS[ct][:n], idt[:n, :n], gb[:n, ct * 512:(ct + 1) * 512], start=(e == 0), stop=(e == 15))
        G_ = GL if t < 8 else GC
        for ct in range(8):
            cs = slice(ct * 512, (ct + 1) * 512)
            tm = TM[u % 2]; u += 1
            p.tt(tm[:n], PS[ct][:n], G_[:n, cs], ALU.mult, e="dve")
            p.tt(Xt[:n, cs], Xt[:n, cs], tm[:n], ALU.add, e="pool")
        p.dma("sp", dst, Xt[:n])
    p.finish([ol, oc])
    return p


def _prog(name, fn):
    if name not in _CACHE:
        _CACHE[name] = fn()
    return _CACHE[name]


def _rep(v, n=128):
    return np.ascontiguousarray(np.broadcast_to(np.asarray(v)[None], (n,) + tuple(np.asarray(v).shape)))


def _fm(a):
    return np.ascontiguousarray(a.reshape(2, TB, -1).transpose(0, 2, 1))


def _tokc(a):
    return np.ascontiguousarray(a.reshape(2, NCH, 64, -1).transpose(0, 2, 1, 3))


_U64 = np.triu(np.ones((64, 64), np.float32))
_V64 = np.tril(np.ones((64, 64), np.float32), -1)


def _run_norm(xl, xc, nw, mod_l, sc_i, sh_i, out_bf16=True, modulate=True, router_w=None):
    name = "norm_%d%d%d" % (out_bf16, modulate, router_w is not None)
    p = _prog(name, lambda: build_norm(out_bf16, modulate, router_w is not None))
    nwr = _rep(nw)
    ims = []
    extra = {}
    if router_w is not None:
        extra = {"wr": np.ascontiguousarray(router_w.reshape(32, 128, 16).transpose(1, 0, 2)), "idn": np.eye(128, dtype=np.float32)}
    reps = {}
    if modulate:
        for m in range(3):
            reps[m] = (_rep(mod_l[m, sc_i * D:(sc_i + 1) * D]), _rep(mod_l[m, sh_i * D:(sh_i + 1) * D]))
    for i in range(8):
        b = i // 4
        d = {"xl": xl[i * 1024:(i + 1) * 1024], "xc": xc[i * 64:(i + 1) * 64], "nw": nwr}
        if modulate:
            d.update({"sc_l": reps[b][0], "sh_l": reps[b][1], "sc_c": reps[2][0], "sh_c": reps[2][1]})
        d.update(extra)
        ims.append(d)
    res = run(p, ims).results
    ol = np.concatenate([r["ol"] for r in res], 0); oc = np.concatenate([r["oc"] for r in res], 0)
    if router_w is not None:
        return ol, oc, np.concatenate([r["al"] for r in res], 0), np.concatenate([r["ac"] for r in res], 0)
    return ol, oc


def _layer(l, xl, xc, mod_l, W):
    h1l, h1c = _run_norm(xl, xc, W["norm1_w"][l], mod_l, 1, 0)
    h = tok_order(h1l.reshape(2, 4096, D), h1c.reshape(2, 256, D))
    hT = np.ascontiguousarray(h.reshape(68, 128, 32, 128).transpose(0, 3, 2, 1))
    del h
    p = _prog("proj", build_proj)
    w_in = W["w_in"][l]
    ims = []
    sls = []
    for c in range(8):
        idx, sl = head_cols(c); sls.append(sl)
        ims.append({"hT": hT, "w": np.ascontiguousarray(w_in[:, idx].reshape(32, 128, NPROJ).transpose(1, 0, 2))})
    proj = [r["o"] for r in run(p, ims).results]
    del ims, hT
    tabs = fnet_tables()
    p = _prog("fnet", build_fnet_sconv)
    ims = []
    for c in range(8):
        pc = proj[c]; sl = sls[c]
        d = {"finT": _fm(pc[:, sl["fin"]]), "shT": _fm(pc[:, sl["sh"]]), "sbT": _fm(pc[:, sl["sb"]]), "sgT": _fm(pc[:, sl["sg"]]),
             "cw": np.ascontiguousarray(W["sc_conv_w"][l][:, c * 128:(c + 1) * 128].T)}
        d.update(tabs)
        ims.append(d)
    res = run(p, ims).results
    ob = np.stack([r["ob"] for r in res]); od = np.stack([r["od"] for r in res])
    p = _prog("gla", build_gla)
    ims = []
    for c in range(8):
        pc = proj[c]; sl = sls[c]
        lr = pc[:, sl["glr"]]
        ims.append({"qT": _fm(pc[:, sl["gq"]]), "kT": _fm(pc[:, sl["gk"]]), "vtok": _tokc(pc[:, sl["gv"]]),
                    "ggtok": _tokc(pc[:, sl["gg"]]).reshape(2, 64, -1),
                    "lrT": np.ascontiguousarray(np.stack([_fm(lr[:, :16]), _fm(lr[:, 16:])], 1)),
                    "up": np.ascontiguousarray(W["gla_gate_up"][l][:, :, c * 64:(c + 1) * 64]),
                    "gb": np.ascontiguousarray(W["gla_gate_b"][l][:, c * 64:(c + 1) * 64].T),
                    "nwr": _rep(W["gla_norm_w"][l], 64),
                    "msk": np.stack([_U64, _U64.T.copy()]), "idn": np.eye(64, dtype=np.float32)})
    oa = np.stack([r["oa"] for r in run(p, ims).results])
    p = _prog("gdn_pre", build_gdn_pre)
    ims = []
    gw = W["gdn_conv_w"][l]
    for c in range(8):
        pc = proj[c]; sl = sls[c]
        comps = [pc[:, sl[n]] for n in ("dq", "dk", "dv")]
        raw = np.ascontiguousarray(np.stack([_tokc(a) for a in comps], 1))
        rawc = np.ascontiguousarray(np.stack([a.reshape(2, TB, 128)[:, :256] for a in comps], 1))
        cwr = np.stack([np.broadcast_to(gw[:, j * 1024 + c * 128: j * 1024 + (c + 1) * 128][None], (64, 3, 128)) for j in range(3)])
        ims.append({"raw": raw, "rawc": rawc, "cwr": np.ascontiguousarray(cwr)})
    qkvn = [r["o"] for r in run(p, ims).results]
    p = _prog("gdn_main", build_gdn_main)
    ims = []
    msk = np.stack([_U64, _V64, _U64.T.copy(), _V64.T.copy()])
    for c in range(8):
        pc = proj[c]; sl = sls[c]
        q5 = qkvn[c].reshape(2, 3, 64, NCH, 128)

        def featmajor(x_):
            return np.ascontiguousarray(x_.transpose(0, 3, 2, 1).reshape(2, 128, TB))
        prm = np.array([W["gdn_dt_bias"][l][0, c], W["gdn_dt_bias"][l][1, c], W["gdn_A_log"][l][0, c], W["gdn_A_log"][l][1, c]], np.float32)
        ims.append({"qT": featmajor(q5[:, 0]), "kT": featmajor(q5[:, 1]), "ktok": np.ascontiguousarray(q5[:, 1]),
                    "vtok": np.ascontiguousarray(q5[:, 2]),
                    "atok": np.ascontiguousarray(np.stack([_tokc(pc[:, sl["af"]])[..., 0], _tokc(pc[:, sl["ab"]])[..., 0]], 1)),
                    "btok": np.ascontiguousarray(np.stack([_tokc(pc[:, sl["bf"]])[..., 0], _tokc(pc[:, sl["bb"]])[..., 0]], 1)),
                    "ztok": _tokc(pc[:, sl["dz"]]).reshape(2, 64, -1), "prm": _rep(prm),
                    "nwr": _rep(W["gdn_norm_w"][l], 64), "msk": msk, "idn": np.eye(64, dtype=np.float32)})
    oc_ = np.stack([r["oc"] for r in run(p, ims).results])
    del ims, proj, qkvn

    def tokmaj_to_fm(o):
        o = o.reshape(8, 2, 64, NCH, 128).transpose(0, 4, 1, 3, 2)
        return o.reshape(8 * 128, 2, TB)

    def fmh(o):
        return o.transpose(0, 2, 1, 3).reshape(8 * 128, 2, TB)
    mixT = np.concatenate([tokmaj_to_fm(oa), fmh(ob), tokmaj_to_fm(oc_), fmh(od)], 0)
    p = _prog("wout", build_wout)
    w = np.ascontiguousarray(W["w_out"][l].reshape(32, 128, D).transpose(1, 0, 2))
    g1 = [_rep(mod_l[m, 2 * D:3 * D]) for m in range(3)]
    ims = []
    for i in range(8):
        b = i // 4; j = i % 4
        lat = mixT[:, b, 256 + j * 1024: 256 + (j + 1) * 1024]; cx = mixT[:, b, j * 64:j * 64 + 64]
        mt = np.concatenate([lat, cx], 1).reshape(32, 128, 1088).transpose(1, 0, 2)
        ims.append({"mixT": np.ascontiguousarray(mt), "w": w, "xl": xl[i * 1024:(i + 1) * 1024], "xc": xc[i * 64:(i + 1) * 64],
                    "g_l": g1[b], "g_c": g1[2]})
    res = run(p, ims).results
    xal = np.concatenate([r["ol"] for r in res], 0); xac = np.concatenate([r["oc"] for r in res], 0)
    del ims, mixT
    h2, h2c, al, ac = _run_norm(xal, xac, W["norm2_w"][l], mod_l, 4, 3, router_w=W["w_router"][l])
    al = al.reshape(2, 4096, 16); ac = ac.reshape(2, 256, 16)
    h2 = h2.reshape(2, 4096, D); h2c = h2c.reshape(2, 256, D)
    p = _prog("route", build_route)
    iota = _rep(np.arange(512, dtype=np.float32))
    tv = (np.arange(32)[None, :] * 128 + np.arange(128)[:, None]).astype(np.float32)
    ims = []
    for j in range(8):
        arow = []; acol = []; arowc = []; acolc = []
        for e in (2 * j, 2 * j + 1):
            for b in range(2):
                a = al[b, :, e]; c_ = ac[b, :, e]
                arow.append(np.broadcast_to(a[None], (128, 4096))); acol.append(a.reshape(32, 128).T)
                arowc.append(np.broadcast_to(c_[None], (128, 256))); acolc.append(c_.reshape(2, 128).T)
        ims.append({"arow": np.ascontiguousarray(np.stack(arow)), "acol": np.ascontiguousarray(np.stack(acol)),
                    "arowc": np.ascontiguousarray(np.stack(arowc)), "acolc": np.ascontiguousarray(np.stack(acolc)), "iota": iota, "tv": tv})
    route = [dict(r) for r in run(p, ims).results]
    import ml_dtypes
    p = _prog("experts", build_experts)
    idn = np.eye(128, dtype=np.float32).astype(ml_dtypes.bfloat16)
    ims = []
    for j in range(8):
        es = slice(2 * j, 2 * j + 2)
        wg = np.ascontiguousarray(W["w_gate"][l][es].reshape(2, 32, 128, 8, 128).transpose(0, 3, 2, 1, 4).reshape(2, 8, 128, 4096))
        wu = np.ascontiguousarray(W["w_up"][l][es].reshape(2, 32, 128, 8, 128).transpose(0, 3, 2, 1, 4).reshape(2, 8, 128, 4096))
        wd = np.ascontiguousarray(W["w_down"][l][es].reshape(2, 8, 128, 8, 512).transpose(0, 3, 2, 1, 4).reshape(2, 8, 128, 4096))
        r = route[j]
        ims.append({"idx": r["idx"], "gate": r["gate"], "idxc": r["idxc"], "gatec": r["gatec"],
                    "h2_0": h2[0], "h2_1": h2[1], "h2c_0": h2c[0], "h2c_1": h2c[1], "wg": wg, "wu": wu, "wd": wd, "idn": idn})
    res = run(p, ims).results
    yg = [r["yg"] for r in res]; ygc = [r["ygc"] for r in res]
    del ims
    yall = []; ycall = []
    rank = np.zeros((2, 4096, 16), np.float32); rankc = np.zeros((2, 256, 16), np.float32)
    zr = np.zeros((1, D), np.float32)
    for b in range(2):
        rows = []; rowsc = []
        for e in range(16):
            j = e // 2; q = (e % 2) * 2 + b
            rows.append(yg[j][q]); rowsc.append(ygc[j][q])
            rank[b, :, e] = route[j]["rank"][q].T.reshape(-1)
            rankc[b, :, e] = route[j]["rankc"][q].T.reshape(-1)
        yall.append(np.concatenate(rows, 0)); ycall.append(np.concatenate(rowsc, 0))
    del yg, ygc
    eo = np.stack([np.arange(16) * 512 - 1.0e6, np.arange(16) * 32 - 1.0e6]).astype(np.float32)
    eo = _rep(eo)
    idn_bf = np.eye(128, dtype=np.float32).astype(ml_dtypes.bfloat16)
    p = _prog("combine", build_combine)
    g2 = [_rep(mod_l[m, 5 * D:6 * D]) for m in range(3)]
    ims = []
    for i in range(8):
        b = i // 4; j = i % 4
        rk = np.full((128, 9, 16), 1e6, np.float32)
        rk[:, :8, :] = rank[b, j * 1024:(j + 1) * 1024].reshape(8, 128, 16).transpose(1, 0, 2)
        rk[:64, 8, :] = rankc[b, j * 64:(j + 1) * 64]
        ims.append({"xl": xal[i * 1024:(i + 1) * 1024], "xc": xac[i * 64:(i + 1) * 64], "g_l": g2[b], "g_c": g2[2],
                    "rk": rk, "eo": eo, "yall": yall[b], "ycall": ycall[b], "idn": idn_bf})
    res = run(p, ims).results
    return np.concatenate([r["ol"] for r in res], 0), np.concatenate([r["oc"] for r in res], 0)


def kernel(x, c, ctx, c_ctx, w_ada, b_ada, norm1_w, norm2_w, w_in, w_out, gla_gate_up, gla_gate_b, gla_norm_w,
           gdn_conv_w, gdn_A_log, gdn_dt_bias, gdn_norm_w, sc_conv_w, w_router, w_gate, w_up, w_down, final_norm_w):
    W = dict(norm1_w=norm1_w, norm2_w=norm2_w, w_in=w_in, w_out=w_out, gla_gate_up=gla_gate_up, gla_gate_b=gla_gate_b,
             gla_norm_w=gla_norm_w, gdn_conv_w=gdn_conv_w, gdn_A_log=gdn_A_log, gdn_dt_bias=gdn_dt_bias, gdn_norm_w=gdn_norm_w,
             sc_conv_w=sc_conv_w, w_router=w_router, w_gate=w_gate, w_up=w_up, w_down=w_down)
    W = {k: np.asarray(v, np.float32) for k, v in W.items()}
    x = np.asarray(x, np.float32); ctx = np.asarray(ctx, np.float32)
    w_ada = np.asarray(w_ada, np.float32); b_ada = np.asarray(b_ada, np.float32)
    p = _prog("mod", build_mod)
    cvec = np.concatenate([np.asarray(c, np.float32), np.asarray(c_ctx, np.float32)[None]], 0)
    cT = np.ascontiguousarray(cvec.T.reshape(32, 128, 3).transpose(1, 0, 2))
    ims = []
    for j in range(8):
        wj = np.ascontiguousarray(w_ada[:, :, j * 3072:(j + 1) * 3072].reshape(2, 32, 128, 3072).transpose(0, 2, 1, 3))
        ims.append({"cT": cT, "w": wj, "b": np.ascontiguousarray(b_ada[:, None, j * 3072:(j + 1) * 3072])})
    mod = np.concatenate([r["o"] for r in run(p, ims).results], axis=2)
    del ims
    xl = x.reshape(8192, D); xc = ctx.reshape(512, D)
    for l in range(2):
        xl, xc = _layer(l, xl, xc, mod[l], W)
    ol, _ = _run_norm(xl, xc, np.asarray(final_norm_w, np.float32), None, 0, 0, out_bf16=False, modulate=False)
    return ol.reshape(2, 4096, D).astype(np.float32)
```

```python
import numpy as np
import concourse.bass as bass
import concourse.mybir as mybir
from concourse.bass_utils import run_bass_kernel_spmd

F32 = mybir.dt.float32
BF16 = mybir.dt.bfloat16
I32 = mybir.dt.int32
U32 = mybir.dt.uint32
AF = mybir.ActivationFunctionType
ALU = mybir.AluOpType
AX = mybir.AxisListType

SEM_WRAP = 1 << 30


class P:
    def __init__(self, n_dma_sems=24):
        self.nc = bass.Bass("TRN2", target_bir_lowering=False)
        nc = self.nc
        self.eng = {"pe": nc.tensor, "dve": nc.vector, "act": nc.scalar, "pool": nc.gpsimd, "sp": nc.sync}
        self.sem = {k: nc.alloc_semaphore("s_" + k) for k in self.eng}
        self.cnt = {k: 0 for k in self.eng}
        self.know = {k: {} for k in self.eng}
        self.dsem = [nc.alloc_semaphore("d%d" % i) for i in range(n_dma_sems)]
        self.dval = [0] * n_dma_sems
        self.dnext = 0
        self.state = {}
        self.n_ins = 0
        self.n_wait = 0

    def dram(self, name, shape, dt, kind):
        return self.nc.dram_tensor(name, list(shape), dt, kind=kind).ap()

    def sb(self, name, shape, dt=F32):
        return self.nc.alloc_sbuf_tensor("sb_" + name, list(shape), dt).ap()

    def ps(self, name, shape, dt=F32):
        return self.nc.alloc_psum_tensor("ps_" + name, list(shape), dt).ap()

    def _wait(self, e, tok):
        if tok is None:
            return
        if tok[0] == "e":
            src, pos = tok[1], tok[2]
            if src == e and e == "pe":
                return
            kn = self.know[e].get(src, 0)
            if kn >= pos:
                return
            self.eng[e].wait_ge(self.sem[src], pos)
            self.know[e][src] = pos
            self.n_wait += 1
        else:
            i, val = tok[1], tok[2]
            key = ("d", i)
            if self.know[e].get(key, 0) >= val:
                return
            self.eng[e].wait_ge(self.dsem[i], val)
            self.know[e][key] = val
            self.n_wait += 1

    def _name(self, ref):
        ap = ref[0] if isinstance(ref, tuple) else ref
        return ap if isinstance(ap, str) else ap.tensor.name

    def _st(self, ref):
        if isinstance(ref, tuple):
            ap, key = ref
        else:
            ap, key = ref, None
        name = ap if isinstance(ap, str) else ap.tensor.name
        d = self.state.setdefault(name, {})
        return d, key

    def _deps(self, e, reads, writes):
        need = {}

        def add(tok):
            if tok is None:
                return
            k = (tok[0], tok[1])
            if need.get(k, 0) < tok[2]:
                need[k] = tok[2]
        for r in reads:
            d, key = self._st(r)
            keys = list(d.keys()) if key is None else [k for k in d if k is None or k == key]
            for k in keys:
                add(d[k][0])
        for w in writes:
            d, key = self._st(w)
            keys = list(d.keys()) if key is None else [k for k in d if k is None or k == key]
            for k in keys:
                add(d[k][0])
                for t in d[k][1]:
                    add(t)
        for (ty, src), v in need.items():
            self._wait(e, (ty, src, v))

    @staticmethod
    def _addreader(lst, tok):
        for i, t in enumerate(lst):
            if t[0] == tok[0] and t[1] == tok[1]:
                if t[2] < tok[2]:
                    lst[i] = tok
                return
        lst.append(tok)

    def _commit(self, tok, reads, writes):
        for r in reads:
            d, key = self._st(r)
            if key is None:
                if not d:
                    d[None] = [None, []]
                for k in d:
                    self._addreader(d[k][1], tok)
            else:
                if key not in d:
                    base = d.get(None, [None, []])
                    d[key] = [base[0], list(base[1])]
                self._addreader(d[key][1], tok)
        for w in writes:
            d, key = self._st(w)
            if key is None:
                d.clear()
                d[None] = [tok, []]
            else:
                if None in d and key not in d:
                    pass
                d[key] = [tok, []]

    def op(self, e, fn, reads, writes):
        pr = [r for r in reads if self._name(r).startswith("ps_")]
        if pr:
            reads = [r for r in reads if not self._name(r).startswith("ps_")]
            writes = list(writes) + pr
        self._deps(e, reads, writes)
        ins = fn()
        self.cnt[e] += 1
        ins.then_inc(self.sem[e], 1)
        tok = ("e", e, self.cnt[e])
        self.know[e][e] = self.know[e].get(e, 0)
        self._commit(tok, reads, writes)
        self.n_ins += 1
        return ins

    def dma(self, q, out, in_, reads=None, writes=None, **kw):
        reads = [in_] if reads is None else reads
        writes = [out] if writes is None else writes
        out = self._a(out); in_ = self._a(in_)
        i = self.dnext
        self.dnext = (self.dnext + 1) % len(self.dsem)
        if self.dval[i] > 0:
            self._wait(q, ("d", i, self.dval[i]))
        self._deps(q, reads, writes)
        ins = self.eng[q].dma_start(out=out, in_=in_, **kw)
        self.dval[i] += 16
        ins.then_inc(self.dsem[i], 16)
        tok = ("d", i, self.dval[i])
        self._commit(tok, reads, writes)
        self.n_ins += 1
        return tok

    def finish(self, toks_or_refs):
        for r in toks_or_refs:
            d, key = self._st(r)
            for k in d:
                self._wait("sp", d[k][0])
        for e in self.eng:
            if e != "sp" and self.cnt[e] > 0:
                self._wait("sp", ("e", e, self.cnt[e]))

    @staticmethod
    def _a(x):
        return x[0] if isinstance(x, tuple) else x

    def mm(self, out, lhsT, rhs, start=True, stop=True, okey=None, rkeys=None):
        rd = [lhsT, rhs] if rkeys is None else rkeys
        wr = [out if okey is None else (self._a(out), okey)]
        o_, l_, r_ = self._a(out), self._a(lhsT), self._a(rhs)
        return self.op("pe", lambda: self.nc.tensor.matmul(o_, l_, r_, start=start, stop=stop), rd, wr)

    def tr(self, out, in_, ident):
        o_, i_, d_ = self._a(out), self._a(in_), self._a(ident)
        return self.op("pe", lambda: self.nc.tensor.transpose(o_, i_, d_), [in_, ident], [out])

    def act(self, out, in_, func, bias=None, scale=None, accum_out=None, extra_reads=()):
        kw = {}
        rd = [in_] + list(extra_reads)
        if bias is not None:
            kw["bias"] = self._a(bias)
            if not isinstance(bias, (int, float)):
                rd.append(bias)
        if scale is not None:
            kw["scale"] = self._a(scale)
            if not isinstance(scale, (int, float)):
                rd.append(scale)
        wr = [out]
        if accum_out is not None:
            kw["accum_out"] = self._a(accum_out)
            wr.append(accum_out)
        o_, i_ = self._a(out), self._a(in_)
        return self.op("act", lambda: self.nc.scalar.activation(o_, i_, func, **kw), rd, wr)

    def _veng(self, e):
        return self.nc.vector if e == "dve" else self.nc.gpsimd

    def copy(self, out, in_, e="dve"):
        o_, i_ = self._a(out), self._a(in_)
        if e == "act":
            return self.op("act", lambda: self.nc.scalar.copy(o_, i_), [in_], [out])
        return self.op(e, lambda: self._veng(e).tensor_copy(o_, i_), [in_], [out])

    def tt(self, out, in0, in1, op, e="dve"):
        o_, a_, b_ = self._a(out), self._a(in0), self._a(in1)
        return self.op(e, lambda: self._veng(e).tensor_tensor(o_, a_, b_, op), [in0, in1], [out])

    def ts(self, out, in0, s1, op0, s2=None, op1=None, accum_out=None, e="dve"):
        rd = [in0]
        if not isinstance(s1, (int, float)):
            rd.append(s1)
        if s2 is not None and not isinstance(s2, (int, float)):
            rd.append(s2)
        wr = [out]
        kw = {}
        if op1 is not None:
            kw["op1"] = op1
        if accum_out is not None:
            kw["accum_out"] = self._a(accum_out)
            wr.append(accum_out)
        o_, a_, s1_, s2_ = self._a(out), self._a(in0), self._a(s1), self._a(s2)
        return self.op(e, lambda: self._veng(e).tensor_scalar(o_, a_, s1_, s2_, op0, **kw), rd, wr)

    def stt(self, out, in0, scalar, in1, op0, op1, e="dve"):
        rd = [in0, in1]
        if not isinstance(scalar, (int, float)):
            rd.append(scalar)
        o_, a_, s_, b_ = self._a(out), self._a(in0), self._a(scalar), self._a(in1)
        return self.op(e, lambda: self._veng(e).scalar_tensor_tensor(o_, a_, s_, b_, op0, op1), rd, [out])

    def memset(self, out, val, e="dve"):
        o_ = self._a(out)
        return self.op(e, lambda: self._veng(e).memset(o_, val), [], [out])

    def reduce(self, out, in_, op=ALU.add, axis=AX.X, e="dve"):
        o_, i_ = self._a(out), self._a(in_)
        return self.op(e, lambda: self._veng(e).tensor_reduce(o_, i_, axis, op), [in_], [out])

    def recip(self, out, in_):
        o_, i_ = self._a(out), self._a(in_)
        return self.op("dve", lambda: self.nc.vector.reciprocal(o_, i_), [in_], [out])


def run(p, in_maps, n=8):
    return run_bass_kernel_spmd(p.nc, in_maps, core_ids=list(range(n)))


D = 4096
NCORES = 8


def EPS_AP(p, val=1e-6):
    key = "_eps%g" % val
    if not hasattr(p, key):
        t = p.sb("eps%d" % len([k for k in p.__dict__ if k.startswith("_eps")]), [128, 1])
        p.memset(t, val)
        setattr(p, key, t)
    return getattr(p, key)
_CACHE = {}


def build_norm(out_bf16=True, modulate=True, router=False):
    p = P()
    xl = p.dram("xl", [1024, D], F32, "ExternalInput")
    xc = p.dram("xc", [64, D], F32, "ExternalInput")
    nw = p.dram("nw", [128, D], F32, "ExternalInput")
    odt = BF16 if out_bf16 else F32
    ol = p.dram("ol", [1024, D], odt, "ExternalOutput")
    oc = p.dram("oc", [64, D], odt, "ExternalOutput")
    nw_t = p.sb("nw_t", [128, D])
    p.dma("sp", nw_t, nw)
    wv = {}; sh = {}
    if modulate:
        for ty in ("l", "c"):
            sc_d = p.dram("sc_" + ty, [128, D], F32, "ExternalInput")
            sh_d = p.dram("sh_" + ty, [128, D], F32, "ExternalInput")
            wv[ty] = p.sb("wv_" + ty, [128, D]); sh[ty] = p.sb("sh_" + ty, [128, D])
            p.dma("sp", wv[ty], sc_d); p.dma("sp", sh[ty], sh_d)
            p.stt(wv[ty], wv[ty], 1.0, nw_t, ALU.add, ALU.mult)
    else:
        wv = {"l": nw_t, "c": nw_t}
    if router:
        wr = p.dram("wr", [128, 32, 16], F32, "ExternalInput")
        idn = p.dram("idn", [128, 128], F32, "ExternalInput")
        al = p.dram("al", [1024, 16], F32, "ExternalOutput"); ac = p.dram("ac", [64, 16], F32, "ExternalOutput")
        wrt = p.sb("wrt", [128, 32, 16]); idt = p.sb("idt", [128, 128])
        p.dma("sp", wrt, wr); p.dma("sp", idt, idn)
        hT = p.sb("hT", [128, 32, 128])
        ptr = [p.ps("ptr%d" % i, [128, 512]) for i in range(3)]
        plg = p.ps("plg", [128, 16])
        lg = p.sb("lg", [128, 16]); mx = p.sb("mx", [128, 1]); sm = p.sb("sm", [128, 1]); af = [p.sb("af%d" % i, [128, 16]) for i in range(2)]
    xt = [p.sb("xt%d" % i, [128, D]) for i in range(2)]
    sq = p.sb("sq", [128, D])
    ot = [p.sb("ot%d" % i, [128, D], odt) for i in range(2)]
    ss = [p.sb("ss%d" % i, [128, 1]) for i in range(2)]
    rs = [p.sb("rs%d" % i, [128, 1]) for i in range(2)]
    for t in range(9):
        n = 128 if t < 8 else 64
        ty = "l" if t < 8 else "c"
        src = xl[t * 128:(t + 1) * 128, :] if t < 8 else xc
        dst = ol[t * 128:(t + 1) * 128, :] if t < 8 else oc
        X = xt[t % 2]; O = ot[t % 2]; S = ss[t % 2]; Rr = rs[t % 2]
        p.dma("sp", X[:n], src)
        p.act(sq[:n], X[:n], AF.Square, accum_out=S[:n])
        p.act(Rr[:n], S[:n], AF.Ln, scale=1.0 / D, bias=EPS_AP(p)[:n])
        p.act(Rr[:n], Rr[:n], AF.Exp, scale=-0.5)
        if modulate:
            p.stt(sq[:n], X[:n], Rr[:n], wv[ty][:n], ALU.mult, ALU.mult, e="dve")
            if router:
                p.tt(sq[:n], sq[:n], sh[ty][:n], ALU.add, e="pool")
                p.copy(O[:n], sq[:n], e="act")
            else:
                p.tt(O[:n], sq[:n], sh[ty][:n], ALU.add, e="pool")
        else:
            p.stt(O[:n], X[:n], Rr[:n], wv[ty][:n], ALU.mult, ALU.mult, e="dve")
        p.dma("sp", dst, O[:n])
        if router:
            for g in range(8):
                pt = ptr[g % 3]
                for i in range(4):
                    kc = g * 4 + i
                    p.tr(pt[:, i * 128:i * 128 + n], sq[:n, kc * 128:(kc + 1) * 128], idt[:n, :n])
                src_v = pt.rearrange("p (a t) -> p a t", a=4)[:, :, :n]
                p.copy(hT[:, g * 4:(g + 1) * 4, :n], src_v, e=("dve", "act")[g % 2])
            for kc in range(32):
                p.mm(plg[:n], hT[:, kc, :n], wrt[:, kc, :], start=(kc == 0), stop=(kc == 31))
            A_ = af[t % 2]
            p.copy(lg[:n], plg[:n], e="dve")
            p.reduce(mx[:n], lg[:n], op=ALU.max, axis=AX.X, e="dve")
            p.ts(mx[:n], mx[:n], -1.0, ALU.mult)
            p.act(A_[:n], lg[:n], AF.Exp, bias=mx[:n], accum_out=sm[:n])
            p.recip(sm[:n], sm[:n])
            p.ts(A_[:n], A_[:n], sm[:n], ALU.mult)
            p.dma("sp", al[t * 128:(t + 1) * 128, :] if t < 8 else ac, A_[:n])
    p.finish([ol, oc] + ([al, ac] if router else []))
    return p


NPROJ = 1444
NTOK = 8704


def build_proj():
    p = P()
    NT = NTOK // 128
    hT = p.dram("hT", [NT, 128, 32, 128], BF16, "ExternalInput")
    w = p.dram("w", [128, 32, NPROJ], F32, "ExternalInput")
    o = p.dram("o", [NTOK, NPROJ], F32, "ExternalOutput")
    wb = p.sb("wb", [128, 32, NPROJ], BF16)
    stg = [p.sb("stg%d" % i, [128, 2, NPROJ]) for i in range(2)]
    for c in range(16):
        s = stg[c % 2]
        p.dma("sp", s, w[:, 2 * c:2 * c + 2, :])
        p.copy((wb[:, 2 * c:2 * c + 2, :]), s, e=("dve", "act", "pool")[c % 3]) if False else \
            p.op(("dve", "pool")[c % 2], lambda s=s, c=c: (p.nc.vector if c % 2 == 0 else p.nc.gpsimd).tensor_copy(wb[:, 2 * c:2 * c + 2, :], s), [s], [(wb, c)])
    ht = [p.sb("ht%d" % i, [128, 32, 128], BF16) for i in range(3)]
    ot = [p.sb("ot%d" % i, [128, NPROJ]) for i in range(2)]
    pst = [p.ps("pp%d" % i, [128, 512]) for i in range(6)]
    ntl = [(0, 512), (512, 512), (1024, NPROJ - 1024)]
    for t in range(NT):
        H = ht[t % 3]; O = ot[t % 2]
        p.dma("sp", H, hT[t])
        for j, (n0, nn) in enumerate(ntl):
            ps = pst[(t % 2) * 3 + j]
            for kc in range(32):
                p.mm(ps[:, :nn], H[:, kc, :], wb[:, kc, n0:n0 + nn], start=(kc == 0), stop=(kc == 31),
                     rkeys=[H, (wb, kc // 2)])
            if j == 1:
                p.copy(O[:, n0:n0 + nn], ps[:, :nn], e="act")
            else:
                p.copy(O[:, n0:n0 + nn], ps[:, :nn], e="dve")
        p.dma("sp", o[t * 128:(t + 1) * 128, :], O)
    p.finish([o])
    return p


def build_mod():
    p = P()
    NCOL = 3072
    cT = p.dram("cT", [128, 32, 3], F32, "ExternalInput")
    w = p.dram("w", [2, 128, 32, NCOL], F32, "ExternalInput")
    b = p.dram("b", [2, 1, NCOL], F32, "ExternalInput")
    o = p.dram("o", [2, 3, NCOL], F32, "ExternalOutput")
    ct = p.sb("ct", [128, 32, 3]); st = p.sb("st", [128, 32, 3])
    ones = p.sb("ones", [1, 3]); bt = p.sb("bt", [1, 2, NCOL])
    p.dma("sp", ct, cT)
    p.dma("sp", bt[:, 0, :], b[0]); p.dma("sp", bt[:, 1, :], b[1])
    p.act(st, ct, AF.Silu)
    p.memset(ones, 1.0)
    wt = [p.sb("wt%d" % i, [128, 8, 512]) for i in range(3)]
    pst = [p.ps("pm%d" % i, [3, 512]) for i in range(2)]
    ot = p.sb("ot", [3, 2, NCOL])
    i = 0
    for l in range(2):
        for nt in range(6):
            ps = pst[(l * 6 + nt) % 2]
            for kg in range(4):
                W = wt[i % 3]; i += 1
                p.dma("sp", W, w[l, :, kg * 8:(kg + 1) * 8, nt * 512:(nt + 1) * 512])
                for k in range(8):
                    p.mm(ps, st[:, kg * 8 + k, :], W[:, k, :], start=(kg == 0 and k == 0), stop=False)
            p.mm(ps, ones, bt[:, l, nt * 512:(nt + 1) * 512], start=False, stop=True)
            p.copy(ot[:, l, nt * 512:(nt + 1) * 512], ps, e="dve")
    p.dma("sp", o[0], ot[:, 0, :]); p.dma("sp", o[1], ot[:, 1, :])
    p.finish([o])
    return p


IN_SIZES = (512, 512, 1024, 1024, 32, 1024, 3072, 1024, 16, 16, 1024, 1024, 1024)
IN_OFF = np.concatenate([[0], np.cumsum(IN_SIZES)]).astype(int)


def head_cols(h):
    o = IN_OFF
    segs = [("gq", o[0] + h * 64, 64), ("gk", o[1] + h * 64, 64), ("gv", o[2] + h * 128, 128), ("gg", o[3] + h * 128, 128),
            ("glr", o[4], 32), ("fin", o[5] + h * 128, 128),
            ("dq", o[6] + h * 128, 128), ("dk", o[6] + 1024 + h * 128, 128), ("dv", o[6] + 2048 + h * 128, 128),
            ("dz", o[7] + h * 128, 128), ("bf", o[8] + h, 1), ("bb", o[8] + 8 + h, 1), ("af", o[9] + h, 1), ("ab", o[9] + 8 + h, 1),
            ("sh", o[10] + h * 128, 128), ("sb", o[11] + h * 128, 128), ("sg", o[12] + h * 128, 128)]
    idx = np.concatenate([np.arange(s, s + n) for _, s, n in segs])
    sl = {}
    pos = 0
    for nm, s, n in segs:
        sl[nm] = slice(pos, pos + n); pos += n
    assert pos == NPROJ
    return idx, sl


def tok_order(lat, ctx):
    return np.concatenate([np.concatenate([ctx[b], lat[b]], 0) for b in range(2)], 0)


TB = 4352
NCH = 68


def build_fnet_sconv():
    p = P()
    finT = p.dram("finT", [2, 128, TB], F32, "ExternalInput")
    shT = p.dram("shT", [2, 128, TB], F32, "ExternalInput")
    sbT = p.dram("sbT", [2, 128, TB], F32, "ExternalInput")
    sgT = p.dram("sgT", [2, 128, TB], F32, "ExternalInput")
    cw = p.dram("cw", [128, 3], F32, "ExternalInput")
    cs_l = p.dram("cs_l", [128, 256], F32, "ExternalInput")
    cs_c = p.dram("cs_c", [128, 256], F32, "ExternalInput")
    tab = p.dram("tab", [8, 4, 128, 2, 8, 512], BF16, "ExternalInput")
    tabc = p.dram("tabc", [128, 2, 2, 256], BF16, "ExternalInput")
    ob = p.dram("ob", [2, 128, TB], BF16, "ExternalOutput")
    od = p.dram("od", [2, 128, TB], BF16, "ExternalOutput")
    A = [p.sb("A%d" % i, [128, TB]) for i in range(4)]
    cwt = p.sb("cwt", [128, 3]); csl = p.sb("csl", [128, 256]); csc = p.sb("csc", [128, 256])
    tct = p.sb("tct", [128, 2, 2, 256], BF16)
    p.dma("sp", cwt, cw); p.dma("sp", csl, cs_l); p.dma("sp", csc, cs_c); p.dma("sp", tct, tabc)
    gcs = [p.sb("gcs%d" % b, [128, 34, 256], BF16) for b in range(2)]
    ps1 = [p.ps("f1_%d" % i, [128, 512]) for i in range(2)]
    ps2 = [p.ps("f2_%d" % i, [128, 512]) for i in range(4)]
    for b in range(2):
        p.dma("sp", A[b], finT[b])
    k = 0
    for b in range(2):
        for g in range(17):
            ps = ps1[k % 2]; k += 1
            for j in range(2):
                tcn = 2 * g + j
                p.mm(ps[:, j * 256:(j + 1) * 256], A[b][:, tcn * 128:(tcn + 1) * 128], csc if tcn < 2 else csl)
            p.copy(gcs[b][:, 2 * g:2 * g + 2, :], ps.rearrange("p (a n) -> p a n", a=2), e=("dve", "act")[k % 2])
    obt = [p.sb("obt%d" % b, [128, TB], BF16) for b in range(2)]
    for b in range(2):
        ps = ps2[b]
        i = 0
        for cs in range(2):
            for tcn in range(2):
                p.mm(ps[:, :256], gcs[b][:, tcn, cs * 128:(cs + 1) * 128], tct[:, cs, tcn, :], start=(i == 0), stop=(i == 3)); i += 1
        p.copy(obt[b][:, 0:256], ps[:, :256], e="dve")
    tb_ = [p.sb("tb%d" % i, [128, 2, 8, 512], BF16) for i in range(4)]
    k = 0
    for j in range(8):
        for tg in range(4):
            T_ = tb_[k % 4]; k += 1
            p.dma("sp", T_, tab[j, tg])
            for b in range(2):
                ps = ps2[(j % 2) * 2 + b]
                for cs in range(2):
                    for t8 in range(8):
                        tcn = 2 + tg * 8 + t8
                        p.mm(ps, gcs[b][:, tcn, cs * 128:(cs + 1) * 128], T_[:, cs, t8, :],
                             start=(tg == 0 and cs == 0 and t8 == 0), stop=(tg == 3 and cs == 1 and t8 == 7))
        for b in range(2):
            p.copy(obt[b][:, 256 + j * 512:256 + (j + 1) * 512], ps2[(j % 2) * 2 + b], e=("dve", "act")[b])
    for b in range(2):
        p.dma("sp", ob[b], obt[b])
    odt = [p.sb("odt%d" % b, [128, TB], BF16) for b in range(2)]
    for b in range(2):
        SH, SB_, SG, ACC = A[0], A[1], A[2], A[3]
        p.dma("sp", SH, shT[b]); p.dma("sp", SB_, sbT[b]); p.dma("sp", SG, sgT[b])
        p.tt(SG, SG, SH, ALU.mult, e="pool")
        p.ts(ACC, SG, cwt[:, 1:2], ALU.mult, e="dve")
        for (lo, n, rows) in ((0, 256, 1), (256, 4096, 64)):
            w_ = n // rows
            u3 = SG[:, lo:lo + n].rearrange("p (r w) -> p r w", r=rows)
            a3 = ACC[:, lo:lo + n].rearrange("p (r w) -> p r w", r=rows)
            p.stt(a3[:, :, 1:], u3[:, :, :w_ - 1], cwt[:, 0:1], a3[:, :, 1:], ALU.mult, ALU.add, e="dve")
            p.stt(a3[:, :, :w_ - 1], u3[:, :, 1:], cwt[:, 2:3], a3[:, :, :w_ - 1], ALU.mult, ALU.add, e="dve")
        p.tt(odt[b], ACC, SB_, ALU.mult, e="pool")
        p.dma("sp", od[b], odt[b])
    p.finish([ob, od])
    return p


def fnet_tables():
    key = "fnet_tables"
    if key in _CACHE:
        return _CACHE[key]
    import ml_dtypes
    bf = ml_dtypes.bfloat16
    out = {}
    c = np.arange(128)
    ang = 2 * np.pi * np.outer(c, c) / 128.0
    for nm, T in (("cs_l", 4096), ("cs_c", 256)):
        s = 1.0 / np.sqrt(T * 128.0)
        out[nm] = np.concatenate([np.cos(ang) * s, np.sin(ang) * s], 1).astype(np.float32)
    t = np.arange(4096)
    m = (np.outer(t, t) % 4096).astype(np.int64)
    base = 2 * np.pi * np.arange(4096) / 4096.0
    cosv = np.cos(base).astype(np.float32); sinv = (-np.sin(base)).astype(np.float32)
    C = cosv[m].astype(bf); S = sinv[m].astype(bf)
    def lay(M):
        return M.reshape(4, 8, 128, 8, 512).transpose(3, 0, 2, 1, 4)
    out["tab"] = np.ascontiguousarray(np.stack([lay(C), lay(S)], axis=3))
    tc = np.arange(256)
    mc = (np.outer(tc, tc) % 256)
    bc = 2 * np.pi * np.arange(256) / 256.0
    Cc = np.cos(bc)[mc].astype(bf); Sc = (-np.sin(bc))[mc].astype(bf)
    def layc(M):
        return M.reshape(2, 128, 256).transpose(1, 0, 2)
    out["tabc"] = np.ascontiguousarray(np.stack([layc(Cc), layc(Sc)], axis=1))
    _CACHE[key] = out
    return out


def const_tile(p, val, parts=128):
    key = "_c_%g" % val
    if not hasattr(p, key):
        t = p.sb("cst%d" % len([k for k in p.__dict__ if k.startswith("_c_")]), [128, 1])
        p.memset(t, val)
        setattr(p, key, t)
    return getattr(p, key)


CH_FWD = list(range(NCH))
CH_BWD = [3, 2, 1, 0] + list(range(NCH - 1, 3, -1))


def hillis(p, A, B, nparts, bwd, C=64):
    src, dst = A, B
    j = 1
    while j < C:
        s3 = src.rearrange("p (n c) -> p n c", c=C); d3 = dst.rearrange("p (n c) -> p n c", c=C)
        if not bwd:
            p.tt(d3[:nparts, :, j:], s3[:nparts, :, j:], s3[:nparts, :, :C - j], ALU.add, e="dve")
            p.copy(d3[:nparts, :, :j], s3[:nparts, :, :j], e="pool")
        else:
            p.tt(d3[:nparts, :, :C - j], s3[:nparts, :, :C - j], s3[:nparts, :, j:], ALU.add, e="dve")
            p.copy(d3[:nparts, :, C - j:], s3[:nparts, :, C - j:], e="pool")
        src, dst = dst, src
        j *= 2
    return src


def build_gla():
    p = P()
    qT = p.dram("qT", [2, 64, TB], F32, "ExternalInput")
    kT = p.dram("kT", [2, 64, TB], F32, "ExternalInput")
    vtok = p.dram("vtok", [2, 64, NCH, 128], F32, "ExternalInput")
    ggtok = p.dram("ggtok", [2, 64, NCH * 128], F32, "ExternalInput")
    lrT = p.dram("lrT", [2, 2, 16, TB], F32, "ExternalInput")
    up = p.dram("up", [2, 16, 64], F32, "ExternalInput")
    gb = p.dram("gb", [64, 2], F32, "ExternalInput")
    nwr = p.dram("nwr", [64, 128], F32, "ExternalInput")
    msk = p.dram("msk", [2, 64, 64], F32, "ExternalInput")
    idn = p.dram("idn", [64, 64], F32, "ExternalInput")
    oa = p.dram("oa", [2, 64, NCH * 128], BF16, "ExternalOutput")

    WAB = p.sb("WAB", [64, 2 * TB]); LA = WAB[:, :TB]; LB = WAB[:, TB:]
    Q_ = p.sb("Q_", [64, TB]); K_ = p.sb("K_", [64, TB]); KT = p.sb("KT", [64, TB]); AT = p.sb("AT", [64, TB])
    V = p.sb("V", [64, NCH, 128]); O = p.sb("O", [64, NCH, 128])
    upt = p.sb("upt", [16, 2, 64]); gbt = p.sb("gbt", [64, 2]); nwt = p.sb("nwt", [64, 128])
    mk = p.sb("mk", [64, 2, 64]); idt = p.sb("idt", [64, 64])
    tot = p.sb("tot", [64, NCH]); dT = p.sb("dT", [64, NCH])
    S = [p.sb("S%d" % i, [64, 128]) for i in range(3)]
    ss = p.sb("ssq", [64, NCH]); rst = p.sb("rst", [64, NCH])
    one = const_tile(p, 1.0); eps = const_tile(p, 1e-6)
    for d in range(2):
        p.dma("sp", upt[:, d, :], up[d]); p.dma("sp", mk[:, d, :], msk[d])
    p.dma("sp", gbt, gb); p.dma("sp", nwt, nwr); p.dma("sp", idt, idn)
    p.ts(gbt, gbt, -1.0, ALU.mult)
    psA = [p.ps("ga%d" % i, [64, 512]) for i in range(2)]
    psT = [p.ps("gt%d" % i, [64, 512]) for i in range(2)]
    psO = [p.ps("go%d" % i, [64, 512]) for i in range(2)]
    psI = [p.ps("gi%d" % i, [64, 128]) for i in range(2)]
    LR = AT[:16, :]
    tiles = [(i * 512, 512) for i in range(8)] + [(4096, 256)]
    for b in range(2):
        p.dma("sp", V, vtok[b])
        for d in range(2):
            bwd = d == 1
            p.dma("sp", LR, lrT[b, d])
            for i, (t0, tn) in enumerate(tiles):
                ps = psA[i % 2]
                p.mm(ps[:, :tn], upt[:, d, :], LR[:, t0:t0 + tn])
                p.act(LA[:, t0:t0 + tn], ps[:, :tn], AF.Exp, scale=-1.0, bias=gbt[:, d:d + 1])
            p.act(LA, LA, AF.Ln, bias=one[:64])
            cum = hillis(p, LA, LB, 64, bwd)
            assert cum is LA
            c3 = LA.rearrange("p (n c) -> p n c", c=64)
            p.copy(tot, c3[:, :, 0] if bwd else c3[:, :, 63], e="dve")
            p.act(dT, tot, AF.Exp, scale=-1.0 / 16)
            p.act(AT, LA, AF.Exp, scale=-1.0 / 16)
            p.dma("sp", Q_, qT[b])
            p.stt(Q_, Q_, 0.125, AT, ALU.mult, ALU.mult, e="dve")
            p.act(AT, LA, AF.Exp, scale=1.0 / 16)
            p.dma("sp", K_, kT[b])
            p.tt(K_, K_, AT, ALU.mult, e="pool")
            b3 = LB.rearrange("p (n c) -> p n c", c=64)
            p.tt(b3, c3, tot.unsqueeze(2).to_broadcast([64, NCH, 64]), ALU.subtract, e="dve")
            p.act(LB, LB, AF.Exp, scale=1.0 / 16)
            p.dma("sp", AT, kT[b])
            p.tt(LB, LB, AT, ALU.mult, e="dve")
            kt3 = KT.rearrange("p (n c) -> p n c", c=64); at3 = AT.rearrange("p (n c) -> p n c", c=64)
            for g in range(9):
                ng = min(8, NCH - g * 8)
                pt = psT[g % 2]; pa = psA[g % 2]
                for i in range(ng):
                    n = g * 8 + i
                    p.tr(pt[:, i * 64:(i + 1) * 64], LB[:, n * 64:(n + 1) * 64], idt)
                p.copy(kt3[:, g * 8:g * 8 + ng, :], pt[:, :ng * 64].rearrange("p (n c) -> p n c", c=64), e="act")
            for g in range(9):
                ng = min(8, NCH - g * 8)
                pa = psA[g % 2]
                for i in range(ng):
                    n = g * 8 + i
                    p.mm(pa[:, i * 64:(i + 1) * 64], K_[:, n * 64:(n + 1) * 64], Q_[:, n * 64:(n + 1) * 64])
                p.tt(at3[:, g * 8:g * 8 + ng, :], pa[:, :ng * 64].rearrange("p (n c) -> p n c", c=64),
                     mk[:, d:d + 1, :].to_broadcast([64, ng, 64]), ALU.mult, e="dve")
            order = CH_BWD if bwd else CH_FWD
            p.memset(S[0], 0.0)
            si = 0
            for step, n in enumerate(order):
                grp = n // 4
                po = psO[grp % 2]
                sl = po[:, (n % 4) * 128:(n % 4 + 1) * 128]
                p.mm(sl, at3[:, n, :], V[:, n, :], start=True, stop=False)
                p.mm(sl, Q_[:, n * 64:(n + 1) * 64], S[si], start=False, stop=True)
                pi = psI[step % 2]
                p.mm(pi, kt3[:, n, :], V[:, n, :])
                sn = (si + 1) % 3
                p.stt(S[sn], S[si], dT[:, n:n + 1], pi, ALU.mult, ALU.add, e="dve")
                si = sn
                last_in_grp = (n % 4 == 0) if bwd else (n % 4 == 3)
                if last_in_grp:
                    og = O[:, grp * 4:grp * 4 + 4, :]
                    pv = po.rearrange("p (n v) -> p n v", v=128)
                    if not bwd:
                        p.copy(og, pv, e="act")
                    else:
                        p.tt(og, og, pv, ALU.add, e="pool" if False else "dve")
        GG = WAB.rearrange("p (n v) -> p n v", v=128)
        p.dma("sp", WAB, ggtok[b])
        sq = Q_.rearrange("p (n c) -> p n c", c=64)
        SQ = p_view_sq = None
        h = NCH // 2
        for half, buf in ((0, Q_), (1, K_)):
            o_h = O[:, half * h:(half + 1) * h, :]
            b_h = buf.rearrange("p (n v) -> p n v", v=128)
            p.tt(b_h, o_h, o_h, ALU.mult, e="dve")
            p.reduce(ss[:, half * h:(half + 1) * h], b_h, op=ALU.add, axis=AX.X, e="dve")
        p.act(rst, ss, AF.Ln, scale=1.0 / 128, bias=eps[:64])
        p.act(rst, rst, AF.Exp, scale=-0.5)
        p.act(WAB, WAB, AF.Silu)
        p.tt(O, O, rst.unsqueeze(2).to_broadcast([64, NCH, 128]), ALU.mult, e="dve")
        p.tt(O, O, nwt.unsqueeze(1).to_broadcast([64, NCH, 128]), ALU.mult, e="pool")
        OUT = KT.bitcast(BF16).rearrange("p (n v) -> p n v", v=128)
        p.tt(OUT, O, GG, ALU.mult, e="dve")
        p.dma("sp", oa[b], KT.bitcast(BF16))
    p.finish([oa])
    return p


def build_gdn_pre():
    p = P()
    raw = p.dram("raw", [2, 3, 64, NCH, 128], F32, "ExternalInput")
    rawc = p.dram("rawc", [2, 3, 256, 128], F32, "ExternalInput")
    cwr = p.dram("cwr", [3, 64, 3, 128], F32, "ExternalInput")
    o = p.dram("o", [2, 3, 64, NCH * 128], F32, "ExternalOutput")
    RAW = [p.sb("RAW%d" % i, [64, NCH, 128]) for i in range(2)]
    ACC = [p.sb("ACC%d" % i, [64, NCH, 128]) for i in range(2)]
    TMP = p.sb("TMP", [64, NCH, 128])
    M1 = p.sb("M1", [64, 4, 128]); P1 = p.sb("P1", [64, 4, 128]); TC = p.sb("TC", [64, 4, 128])
    cw = p.sb("cw", [64, 3, 3, 128])
    ss = p.sb("ss", [64, NCH]); rn = p.sb("rn", [64, NCH])
    eps = const_tile(p, 1e-6)
    for c in range(3):
        p.dma("sp", cw[:, c], cwr[c])
    k = 0
    for b in range(2):
        for c in range(3):
            R_ = RAW[k % 2]; A_ = ACC[k % 2]; k += 1
            p.dma("sp", R_, raw[b, c])
            p.memset(M1, 0.0, e="pool"); p.memset(P1, 0.0, e="pool")
            src = rawc[b, c]
            p.dma("sp", M1[1:64, 0, :], src[0:63, :])
            p.dma("sp", M1[:, 1:4, :], src[63:255, :].rearrange("(n s) c -> s n c", s=64))
            p.dma("sp", P1[:, 0:3, :], src[1:193, :].rearrange("(n s) c -> s n c", s=64))
            p.dma("sp", P1[0:63, 3, :], src[193:256, :])
            w0 = cw[:, c, 0:1, :]; w1 = cw[:, c, 1:2, :]; w2 = cw[:, c, 2:3, :]
            p.tt(A_, R_, w1.to_broadcast([64, NCH, 128]), ALU.mult, e="dve")
            p.tt(TMP[:, 5:68], R_[:, 4:67], w0.to_broadcast([64, 63, 128]), ALU.mult, e="pool")
            p.tt(A_[:, 5:68], A_[:, 5:68], TMP[:, 5:68], ALU.add, e="dve")
            p.tt(TMP[:, 4:67], R_[:, 5:68], w2.to_broadcast([64, 63, 128]), ALU.mult, e="pool")
            p.tt(A_[:, 4:67], A_[:, 4:67], TMP[:, 4:67], ALU.add, e="dve")
            p.tt(TC, M1, w0.to_broadcast([64, 4, 128]), ALU.mult, e="pool")
            p.tt(A_[:, 0:4], A_[:, 0:4], TC, ALU.add, e="dve")
            p.tt(TC, P1, w2.to_broadcast([64, 4, 128]), ALU.mult, e="pool")
            p.tt(A_[:, 0:4], A_[:, 0:4], TC, ALU.add, e="dve")
            p.act(A_, A_, AF.Silu)
            if c < 2:
                p.tt(TMP, A_, A_, ALU.mult, e="pool")
                p.reduce(ss, TMP, op=ALU.add, axis=AX.X, e="dve")
                p.act(rn, ss, AF.Ln, bias=eps[:64])
                p.act(rn, rn, AF.Exp, scale=-0.5)
                if c == 0:
                    p.ts(rn, rn, float(128 ** -0.5), ALU.mult)
                p.tt(A_, A_, rn.unsqueeze(2).to_broadcast([64, NCH, 128]), ALU.mult, e="dve")
            p.dma("sp", o[b, c], A_.rearrange("p n c -> p (n c)"))
    p.finish([o])
    return p


GROUPS = [(0, 4)] + [(4 + 8 * i, 8) for i in range(8)]


def build_gdn_main():
    p = P()
    qT = p.dram("qT", [2, 128, TB], F32, "ExternalInput")
    kT = p.dram("kT", [2, 128, TB], F32, "ExternalInput")
    ktok = p.dram("ktok", [2, 64, NCH, 128], F32, "ExternalInput")
    vtok = p.dram("vtok", [2, 64, NCH, 128], F32, "ExternalInput")
    atok = p.dram("atok", [2, 2, 64, NCH], F32, "ExternalInput")
    btok = p.dram("btok", [2, 2, 64, NCH], F32, "ExternalInput")
    ztok = p.dram("ztok", [2, 64, NCH * 128], F32, "ExternalInput")
    prm = p.dram("prm", [128, 4], F32, "ExternalInput")
    nwr = p.dram("nwr", [64, 128], F32, "ExternalInput")
    msk = p.dram("msk", [4, 64, 64], F32, "ExternalInput")
    idn = p.dram("idn", [64, 64], F32, "ExternalInput")
    oc = p.dram("oc", [2, 64, NCH * 128], BF16, "ExternalOutput")

    QT = p.sb("QT", [128, TB]); KTs = p.sb("KTs", [128, TB])
    O = p.sb("O", [64, NCH, 128]); Z = p.sb("Z", [64, NCH * 128]); SQ = p.sb("SQ", [64, TB])
    prt = p.sb("prt", [128, 4]); nwt = p.sb("nwt", [64, 128]); mk = p.sb("mk", [64, 4, 64]); idt = p.sb("idt", [64, 64])
    ones = p.sb("ones", [64, 128])
    one = const_tile(p, 1.0); eps = const_tile(p, 1e-6)
    p.dma("sp", prt, prm); p.dma("sp", nwt, nwr); p.dma("sp", idt, idn)
    for i in range(4):
        p.dma("sp", mk[:, i, :], msk[i])
    p.memset(ones, 1.0)
    expA = p.sb("expA", [128, 2])
    p.act(expA, prt[:, 2:4], AF.Exp)
    At = p.sb("At", [64, NCH]); Bt = p.sb("Bt", [64, NCH]); L = p.sb("L", [64, NCH])
    beta = p.sb("beta", [64, NCH]); nbeta = p.sb("nbeta", [64, NCH]); eg = p.sb("eg", [64, NCH])
    tot = p.sb("tot", [64, NCH]); kes = p.sb("kes", [64, NCH]); bw = p.sb("bw", [64, NCH]); dS = p.sb("dS", [128, NCH])
    ss = p.sb("ssq", [64, NCH]); rst = p.sb("rst", [64, NCH])
    def g64(nm):
        return p.sb(nm, [64, 8, 64])
    laU = g64("laU"); laV = g64("laV"); LM = g64("LM"); LMT = g64("LMT"); TMPg = g64("TMPg")
    Xa = g64("Xa"); XTa = g64("XTa"); Xb = g64("Xb"); XTb = g64("XTb"); QKM = g64("QKM")
    R = p.sb("R", [64, 8, 256]); R0 = p.sb("R0", [64, 8, 256]); TT = g64("TT")
    KG = p.sb("KG", [64, 8, 128]); VG = p.sb("VG", [64, 8, 128]); KE = p.sb("KE", [64, 8, 128]); VN = p.sb("VN", [64, 8, 128])
    WT = p.sb("WT", [128, 8, 64]); QG = p.sb("QG", [128, 512]); EGR = p.sb("EGR", [128, 512])
    S = [p.sb("S%d" % i, [128, 128]) for i in range(3)]
    PA = [p.ps("PA%d" % i, [64, 1024]) for i in range(2)]; PB = [p.ps("PB%d" % i, [128, 512]) for i in range(2)]
    PC = p.ps("PC", [128, 512]); PD = p.ps("PD", [64, 512])
    PCv = PC[:64, 0:128]; PCs = PC[:, 128:256]

    def fl(t, ng):
        return t[:, :ng, :].rearrange("p n c -> p (n c)")

    for b in range(2):
        p.dma("sp", QT, qT[b]); p.dma("sp", KTs, kT[b])
        for d in range(2):
            bwd = d == 1
            Um = mk[:, 2 * d, :]; Vm = mk[:, 2 * d + 1, :]
            p.dma("sp", At, atok[b, d]); p.dma("sp", Bt, btok[b, d])
            p.act(L, At, AF.Exp, bias=prt[:64, d:d + 1])
            p.act(L, L, AF.Ln, bias=one[:64])
            p.ts(L, L, expA[:64, d:d + 1], ALU.mult)
            p.act(beta, Bt, AF.Exp, scale=-1.0)
            p.ts(beta, beta, 1.0, ALU.add)
            p.recip(beta, beta)
            p.ts(nbeta, beta, -1.0, ALU.mult)
            p.mm(PB[0][:64, :NCH], Um, L)
            p.mm(PB[1][:, :NCH], ones, L)
            p.act(eg, PB[0][:64, :NCH], AF.Exp, scale=-1.0)
            p.act(dS, PB[1][:, :NCH], AF.Exp, scale=-1.0)
            p.copy(tot, PB[1][:64, :NCH], e="dve")
            p.tt(kes, tot, PB[0][:64, :NCH], ALU.subtract)
            p.act(kes, kes, AF.Exp, scale=-1.0)
            p.tt(bw, beta, eg, ALU.mult)
            p.memset(S[0], 0.0)
            si = 0
            gorder = ([GROUPS[0]] + GROUPS[:0:-1]) if bwd else GROUPS
            for (n0, ng) in gorder:
                t0 = n0 * 64; tn = ng * 64
                p.dma("sp", KG[:, :ng], ktok[b, :, n0:n0 + ng, :]); p.dma("sp", VG[:, :ng], vtok[b, :, n0:n0 + ng, :])
                Lg = L[:, n0:n0 + ng].unsqueeze(2).to_broadcast([64, ng, 64])
                p.tt(laU[:, :ng], Um.unsqueeze(1).to_broadcast([64, ng, 64]), Lg, ALU.mult, e="pool")
                p.tt(laV[:, :ng], Vm.unsqueeze(1).to_broadcast([64, ng, 64]), Lg, ALU.mult, e="pool")
                p.mm(PB[0][:64, :tn], Um, fl(laV, ng))
                p.act(fl(LM, ng), PB[0][:64, :tn], AF.Exp, scale=-1.0)
                p.tt(LM[:, :ng], LM[:, :ng], Vm.unsqueeze(1).to_broadcast([64, ng, 64]), ALU.mult, e="pool")
                p.mm(PB[1][:64, :tn], Vm, fl(laU, ng))
                p.act(fl(LMT, ng), PB[1][:64, :tn], AF.Exp, scale=-1.0)
                p.tt(LMT[:, :ng], LMT[:, :ng], Um.unsqueeze(1).to_broadcast([64, ng, 64]), ALU.mult, e="pool")
                for i in range(ng):
                    ks = KTs[:, t0 + i * 64:t0 + (i + 1) * 64]
                    p.mm(PB[0][:64, i * 64:(i + 1) * 64], ks, ks)
                p.tt(fl(TMPg, ng), PB[0][:64, :tn], fl(LM, ng), ALU.mult, e="dve")
                p.tt(Xa[:, :ng], TMPg[:, :ng], nbeta[:, n0:n0 + ng].unsqueeze(2).to_broadcast([64, ng, 64]), ALU.mult, e="dve")
                for i in range(ng):
                    p.tr(PB[1][:64, i * 64:(i + 1) * 64], Xa[:, i, :], idt)
                p.copy(fl(XTa, ng), PB[1][:64, :tn], e="act")
                p.tt(R0[:, :ng, 0:128], VG[:, :ng], beta[:, n0:n0 + ng].unsqueeze(2).to_broadcast([64, ng, 128]), ALU.mult, e="pool")
                p.tt(R0[:, :ng, 128:256], KG[:, :ng], bw[:, n0:n0 + ng].unsqueeze(2).to_broadcast([64, ng, 128]), ALU.mult, e="pool")
                p.tt(KE[:, :ng], KG[:, :ng], kes[:, n0:n0 + ng].unsqueeze(2).to_broadcast([64, ng, 128]), ALU.mult, e="pool")
                p.tt(TT[:, :ng], XTa[:, :ng], idt.unsqueeze(1).to_broadcast([64, ng, 64]), ALU.add, e="pool")
                X, XT, Xn, XTn = Xa, XTa, Xb, XTb
                for lev in range(1, 6):
                    for i in range(ng):
                        p.mm(PB[0][:64, i * 64:(i + 1) * 64], XT[:, i, :], X[:, i, :])
                    if lev < 5:
                        for i in range(ng):
                            p.mm(PB[1][:64, i * 64:(i + 1) * 64], X[:, i, :], XT[:, i, :])
                    p.copy(fl(Xn, ng), PB[0][:64, :tn], e="act")
                    if lev < 5:
                        p.copy(fl(XTn, ng), PB[1][:64, :tn], e="act")
                    X, XT, Xn, XTn = Xn, XTn, X, XT
                    for i in range(ng):
                        p.mm(PA[0][:, i * 64:(i + 1) * 64], X[:, i, :], TT[:, i, :])
                    p.tt(fl(TT, ng), fl(TT, ng), PA[0][:, :tn], ALU.add, e="dve")
                for hf in range((ng + 3) // 4):
                    for i in range(4):
                        p.mm(PA[hf][:, i * 256:(i + 1) * 256], TT[:, hf * 4 + i, :], R0[:, hf * 4 + i, :])
                    p.copy(R[:, hf * 4:hf * 4 + 4, :].rearrange("p n c -> p (n c)"), PA[hf], e=("dve", "act")[hf])
                for i in range(ng):
                    p.tr(PB[0][:, i * 64:(i + 1) * 64], R[:, i, 128:256], idt)
                p.copy(fl(WT, ng), PB[0][:, :tn], e="act")
                for i in range(ng):
                    sl_ = slice(t0 + i * 64, t0 + (i + 1) * 64)
                    p.mm(PB[1][:64, i * 64:(i + 1) * 64], KTs[:, sl_], QT[:, sl_])
                p.tt(fl(QKM, ng), PB[1][:64, :tn], fl(LMT, ng), ALU.mult, e="dve")
                p.mm(PB[0][:, :tn], ones, fl(laU, ng))
                p.act(EGR[:, :tn], PB[0][:, :tn], AF.Exp, scale=-1.0)
                p.tt(QG[:, :tn], QT[:, t0:t0 + tn], EGR[:, :tn], ALU.mult, e="dve")
                corder = range(ng - 1, -1, -1) if bwd else range(ng)
                for i in corder:
                    n = n0 + i
                    p.mm(PCv, WT[:, i, :], S[si])
                    p.tt(VN[:, i, :], R[:, i, 0:128], PCv, ALU.subtract, e="dve")
                    slot = n % 4
                    osl = PD[:, slot * 128:(slot + 1) * 128]
                    p.mm(osl, QG[:, i * 64:(i + 1) * 64], S[si], start=True, stop=False)
                    p.mm(osl, QKM[:, i, :], VN[:, i, :], start=False, stop=True)
                    p.mm(PCs, KE[:, i, :], VN[:, i, :])
                    sn = (si + 1) % 3
                    p.stt(S[sn], S[si], dS[:, n:n + 1], PCs, ALU.mult, ALU.add, e="dve")
                    si = sn
                    if (slot == 0) if bwd else (slot == 3):
                        grp = n // 4
                        og = O[:, grp * 4:grp * 4 + 4, :]
                        pv = PD.rearrange("p (n v) -> p n v", v=128)
                        if not bwd:
                            p.copy(og, pv, e="act")
                        else:
                            p.tt(og, og, pv, ALU.add, e="pool" if False else "dve")
        p.dma("sp", Z, ztok[b])
        h = NCH // 2
        for half in range(2):
            o_h = O[:, half * h:(half + 1) * h, :]
            b_h = SQ.rearrange("p (n v) -> p n v", v=128)
            p.tt(b_h, o_h, o_h, ALU.mult, e="dve")
            p.reduce(ss[:, half * h:(half + 1) * h], b_h, op=ALU.add, axis=AX.X, e="dve")
        p.act(rst, ss, AF.Ln, scale=1.0 / 128, bias=eps[:64])
        p.act(rst, rst, AF.Exp, scale=-0.5)
        p.act(Z, Z, AF.Silu)
        p.tt(O, O, rst.unsqueeze(2).to_broadcast([64, NCH, 128]), ALU.mult, e="dve")
        p.tt(O, O, nwt.unsqueeze(1).to_broadcast([64, NCH, 128]), ALU.mult, e="pool")
        OUT = SQ.bitcast(BF16).rearrange("p (n v) -> p n v", v=128)
        p.tt(OUT, O, Z.rearrange("p (n v) -> p n v", v=128), ALU.mult, e="dve")
        p.dma("sp", oc[b], SQ.bitcast(BF16))
    p.finish([oc])
    return p


def build_wout():
    p = P()
    NTK = 1088
    mixT = p.dram("mixT", [128, 32, NTK], BF16, "ExternalInput")
    w = p.dram("w", [128, 32, D], F32, "ExternalInput")
    xl = p.dram("xl", [1024, D], F32, "ExternalInput"); xc = p.dram("xc", [64, D], F32, "ExternalInput")
    g_l = p.dram("g_l", [128, D], F32, "ExternalInput"); g_c = p.dram("g_c", [128, D], F32, "ExternalInput")
    ol = p.dram("ol", [1024, D], F32, "ExternalOutput"); oc = p.dram("oc", [64, D], F32, "ExternalOutput")
    MT = p.sb("MT", [128, 32, NTK], BF16)
    for q in range(4):
        p.dma("sp", (MT[:, q * 8:(q + 1) * 8, :], q), mixT[:, q * 8:(q + 1) * 8, :])
    WB = [p.sb("WB%d" % i, [128, 32, 512], BF16) for i in range(2)]
    stg = [p.sb("stg%d" % i, [128, 8, 512]) for i in range(2)]
    gl = [p.sb("gl%d" % i, [128, 512]) for i in range(2)]; gc = [p.sb("gc%d" % i, [128, 512]) for i in range(2)]
    xt = [p.sb("xt%d" % i, [128, 512]) for i in range(3)]; tm = [p.sb("tm%d" % i, [128, 512]) for i in range(2)]
    ot = [p.sb("ot%d" % i, [128, 512]) for i in range(3)]
    pst = [p.ps("pw%d" % i, [128, 512]) for i in range(4)]
    k = 0; u = 0
    for j in range(8):
        cs = slice(j * 512, (j + 1) * 512)
        Wb = WB[j % 2]
        for q in range(4):
            s_ = stg[k % 2]; k += 1
            p.dma("sp", s_, w[:, q * 8:(q + 1) * 8, cs])
            e = ("dve", "pool")[q % 2]
            p.copy((Wb[:, q * 8:(q + 1) * 8, :], q), s_, e=e)
        GL = gl[j % 2]; GC = gc[j % 2]
        p.dma("sp", GL, g_l[:, cs]); p.dma("sp", GC, g_c[:, cs])
        for t in range(9):
            n = 128 if t < 8 else 64
            X = xt[u % 3]; O_ = ot[u % 3]; T_ = tm[u % 2]; ps = pst[u % 4]; u += 1
            src = xl[t * 128:(t + 1) * 128, cs] if t < 8 else xc[:, cs]
            dst = ol[t * 128:(t + 1) * 128, cs] if t < 8 else oc[:, cs]
            p.dma("sp", X[:n], src)
            for kc in range(32):
                p.mm(ps[:n], (MT[:, kc, t * 128:t * 128 + n], kc // 8), (Wb[:, kc, :], kc // 8), start=(kc == 0), stop=(kc == 31))
            p.tt(T_[:n], ps[:n], (GL if t < 8 else GC)[:n], ALU.mult, e="dve")
            p.tt(O_[:n], T_[:n], X[:n], ALU.add, e="pool")
            p.dma("sp", dst, O_[:n])
    p.finish([ol, oc])
    return p


def _idma(p, out, in_dram, idx_ap, bounds=None):
    q = "pool"
    i = p.dnext
    p.dnext = (p.dnext + 1) % len(p.dsem)
    if p.dval[i] > 0:
        p._wait(q, ("d", i, p.dval[i]))
    reads = [in_dram, idx_ap]; writes = [out]
    p._deps(q, reads, writes)
    kw = {}
    if bounds is not None:
        rk_ = "_breg_%d" % int(bounds)
        if not hasattr(p, rk_):
            setattr(p, rk_, p.nc.gpsimd.to_reg(int(bounds)))
        kw = {"bounds_check": getattr(p, rk_), "oob_is_err": False}
    ins = p.nc.gpsimd.indirect_dma_start(out=p._a(out), out_offset=None, in_=p._a(in_dram),
                                         in_offset=bass.IndirectOffsetOnAxis(ap=p._a(idx_ap), axis=0), **kw)
    p.dval[i] += 16
    ins.then_inc(p.dsem[i], 16)
    tok = ("d", i, p.dval[i])
    p._commit(tok, reads, writes)
    p.n_ins += 1
    return tok


def build_route():
    p = P()
    arow = p.dram("arow", [4, 128, 4096], F32, "ExternalInput"); acol = p.dram("acol", [4, 128, 32], F32, "ExternalInput")
    arowc = p.dram("arowc", [4, 128, 256], F32, "ExternalInput"); acolc = p.dram("acolc", [4, 128, 2], F32, "ExternalInput")
    iota = p.dram("iota", [128, 512], F32, "ExternalInput"); tv = p.dram("tv", [128, 32], F32, "ExternalInput")
    idx = p.dram("idxr", [4, 1, 512], I32, "ExternalOutput"); gate = p.dram("gater", [4, 1, 512], F32, "ExternalOutput")
    rank = p.dram("rank", [4, 128, 32], F32, "ExternalOutput")
    idxc = p.dram("idxcr", [4, 1, 32], I32, "ExternalOutput"); gatec = p.dram("gatecr", [4, 1, 32], F32, "ExternalOutput")
    rankc = p.dram("rankc", [4, 128, 2], F32, "ExternalOutput")
    io = p.sb("io", [128, 512]); tvt = p.sb("tvt", [128, 32])
    p.dma("sp", io, iota); p.dma("sp", tvt, tv)
    AR = [p.sb("AR%d" % i, [128, 4096]) for i in range(2)]; junk = p.sb("junk", [128, 4096]); junk2 = p.sb("junk2", [128, 4096])
    NAC = p.sb("NAC", [128, 32])
    AC = [p.sb("AC%d" % i, [128, 32]) for i in range(2)]; RK = [p.sb("RK%d" % i, [128, 32]) for i in range(2)]
    TV2 = [p.sb("TV2_%d" % i, [128, 32, 2]) for i in range(2)]
    OH = p.sb("OH", [128, 32, 512])
    PS = [p.ps("pr%d" % i, [2, 512]) for i in range(2)]
    IG = [p.sb("IG%d" % i, [2, 512]) for i in range(2)]; II = [p.sb("II%d" % i, [2, 512], I32) for i in range(2)]
    ARc = [p.sb("ARc%d" % i, [128, 256]) for i in range(2)]; ACc = [p.sb("ACc%d" % i, [128, 2]) for i in range(2)]
    RKc = [p.sb("RKc%d" % i, [128, 2]) for i in range(2)]; TV2c = [p.sb("TV2c%d" % i, [128, 2, 2]) for i in range(2)]
    OHc = p.sb("OHc", [128, 2, 32]); IGc = [p.sb("IGc%d" % i, [2, 32]) for i in range(2)]
    IIc = [p.sb("IIc%d" % i, [2, 32], I32) for i in range(2)]
    for q in range(4):
        A = AR[q % 2]; C = AC[q % 2]; R_ = RK[q % 2]; T2 = TV2[q % 2]; ps = PS[q % 2]
        p.dma("sp", A, arow[q]); p.dma("sp", C, acol[q])
        p.memset(R_, 0.0, e="pool")
        p.ts(NAC, C, -1.0, ALU.mult, e="pool")
        for tc in range(32):
            if tc % 2 == 0:
                p.ts(junk, A, C[:, tc:tc + 1], ALU.is_gt, 0.0, ALU.add, accum_out=R_[:, tc:tc + 1], e="dve")
            else:
                p.act(junk2, A, AF.Sign, bias=NAC[:, tc:tc + 1], accum_out=R_[:, tc:tc + 1])
        rodd = R_.rearrange("p (a two) -> p a two", two=2)[:, :, 1]
        p.ts(rodd, rodd, 0.5, ALU.mult, 2047.5, ALU.add, e="dve")
        p.copy(T2[:, :, 0], tvt, e="pool"); p.copy(T2[:, :, 1], C, e="pool")
        for tc in range(32):
            p.ts(OH[:, tc, :], io, R_[:, tc:tc + 1], ALU.is_equal, e="pool" if tc % 2 else "dve")
        for tc in range(32):
            p.mm(ps, T2[:, tc, :], OH[:, tc, :], start=(tc == 0), stop=(tc == 31))
        p.copy(IG[q % 2], ps, e="dve")
        p.copy(II[q % 2], IG[q % 2], e="dve")
        p.dma("sp", idx[q], II[q % 2][0:1, :]); p.dma("sp", gate[q], IG[q % 2][1:2, :]); p.dma("sp", rank[q], R_)
        A = ARc[q % 2]; C = ACc[q % 2]; R_ = RKc[q % 2]; T2 = TV2c[q % 2]
        p.dma("sp", A, arowc[q]); p.dma("sp", C, acolc[q])
        p.memset(R_, 0.0, e="pool")
        for tc in range(2):
            p.ts(junk[:, :256], A, C[:, tc:tc + 1], ALU.is_gt, 0.0, ALU.add, accum_out=R_[:, tc:tc + 1], e="dve")
        p.copy(T2[:, :, 0], tvt[:, 0:2], e="pool"); p.copy(T2[:, :, 1], C, e="pool")
        for tc in range(2):
            p.ts(OHc[:, tc, :], io[:, :32], R_[:, tc:tc + 1], ALU.is_equal, e="dve")
        for tc in range(2):
            p.mm(ps[:, :32], T2[:, tc, :], OHc[:, tc, :], start=(tc == 0), stop=(tc == 1))
        p.copy(IGc[q % 2], ps[:, :32], e="dve")
        p.copy(IIc[q % 2], IGc[q % 2], e="dve")
        p.dma("sp", idxc[q], IIc[q % 2][0:1, :]); p.dma("sp", gatec[q], IGc[q % 2][1:2, :]); p.dma("sp", rankc[q], R_)
    p.finish([idx, gate, rank, idxc, gatec, rankc])
    return p


DFF = 1024


def build_experts():
    p = P()
    idx = p.dram("idx", [4, 128, 4], I32, "ExternalInput"); gate = p.dram("gate", [4, 128, 4], F32, "ExternalInput")
    idxc = p.dram("idxc", [4, 32, 1], I32, "ExternalInput"); gatec = p.dram("gatec", [4, 32, 1], F32, "ExternalInput")
    h2 = [p.dram("h2_%d" % b, [4096, D], BF16, "ExternalInput") for b in range(2)]
    h2c = [p.dram("h2c_%d" % b, [256, D], BF16, "ExternalInput") for b in range(2)]
    wg = p.dram("wg", [2, 8, 128, 32 * 128], F32, "ExternalInput"); wu = p.dram("wu", [2, 8, 128, 32 * 128], F32, "ExternalInput")
    wd = p.dram("wd", [2, 8, 128, 8 * 512], F32, "ExternalInput")
    idn = p.dram("idn", [128, 128], BF16, "ExternalInput")
    yg = p.dram("yg", [4, 512, D], BF16, "ExternalOutput"); ygc = p.dram("ygc", [4, 32, D], BF16, "ExternalOutput")
    idt = p.sb("idt", [128, 128], BF16); p.dma("sp", idt, idn)
    IDX = p.sb("IDX", [128, 4, 4], I32); G = p.sb("G", [128, 4, 4]); IDXc = p.sb("IDXc", [32, 4], I32); Gc = p.sb("Gc", [32, 4])
    for q in range(4):
        p.dma("sp", IDX[:, q, :], idx[q]); p.dma("sp", G[:, q, :], gate[q])
        p.dma("sp", IDXc[:, q:q + 1], idxc[q]); p.dma("sp", Gc[:, q:q + 1], gatec[q])
    XG = p.sb("XG", [128, 4, D], BF16); XGc = p.sb("XGc", [32, D], BF16)
    XT = [p.sb("XT%d" % b, [128, 32, 512], BF16) for b in range(2)]; XTc = p.sb("XTc", [128, 32, 64], BF16)
    HT = [p.sb("HT%d" % b, [128, 8, 512], BF16) for b in range(2)]; HTc = p.sb("HTc", [128, 8, 64], BF16)
    stg = [p.sb("stg%d" % i, [128, 4096]) for i in range(2)]
    wb = [p.sb("wb%d" % i, [128, 4096], BF16) for i in range(4)]
    HS = [p.sb("HS%d" % i, [128, 512]) for i in range(2)]
    Y = [p.sb("Y%d" % i, [128, 512], BF16) for i in range(3)]
    PT = [p.ps("pt%d" % i, [128, 1024], BF16) for i in range(2)]
    PG = [p.ps("pg%d" % i, [128, 512]) for i in range(2)]; PU = [p.ps("pu%d" % i, [128, 512]) for i in range(2)]
    PY = [p.ps("py%d" % i, [128, 512]) for i in range(2)]
    sk = [0]; wk = [0]

    def load_w(src):
        s_ = stg[sk[0] % 2]; sk[0] += 1
        W = wb[wk[0] % 4]; wk[0] += 1
        p.dma("sp", s_, src)
        p.copy(W[:, :2048], s_[:, :2048], e="dve"); p.copy(W[:, 2048:], s_[:, 2048:], e="pool" if False else "act")
        return W

    for e in range(2):
        for b in range(2):
            q = e * 2 + b
            for rs in range(4):
                _idma(p, XG[:, rs, :], h2[b], IDX[:, q, rs:rs + 1])
            k = 0
            for rs in range(4):
                for g8 in range(4):
                    pt = PT[k % 2]; k += 1
                    for i in range(8):
                        kc = g8 * 8 + i
                        p.tr(pt[:, i * 128:(i + 1) * 128], XG[:, rs, kc * 128:(kc + 1) * 128], idt)
                    p.copy(XT[b][:, g8 * 8:(g8 + 1) * 8, rs * 128:(rs + 1) * 128], pt.rearrange("p (a r) -> p a r", a=8),
                           e=("dve", "act")[k % 2])
            _idma(p, XGc, h2c[b], IDXc[:, q:q + 1])
            for g8 in range(4):
                pt = PT[k % 2]; k += 1
                for i in range(8):
                    kc = g8 * 8 + i
                    p.tr(pt[:, i * 32:(i + 1) * 32], XGc[:, kc * 128:(kc + 1) * 128], idt[:32, :32])
                p.copy(XTc[:, g8 * 8:(g8 + 1) * 8, b * 32:(b + 1) * 32], pt[:, :256].rearrange("p (a r) -> p a r", a=8), e="dve")
        u = 0
        for ft in range(8):
            WG = load_w(wg[e, ft]).rearrange("p (k f) -> p k f", f=128)
            WU = load_w(wu[e, ft]).rearrange("p (k f) -> p k f", f=128)
            for b in range(3):
                rhs = XT[b] if b < 2 else XTc
                n = 512 if b < 2 else 64
                pg = PG[u % 2]; pu = PU[u % 2]; hs = HS[u % 2]; u += 1
                for kc in range(32):
                    p.mm(pg[:, :n], WG[:, kc, :], rhs[:, kc, :], start=(kc == 0), stop=(kc == 31))
                for kc in range(32):
                    p.mm(pu[:, :n], WU[:, kc, :], rhs[:, kc, :], start=(kc == 0), stop=(kc == 31))
                p.act(hs[:, :n], pg[:, :n], AF.Silu)
                dst = HT[b][:, ft, :] if b < 2 else HTc[:, ft, :]
                p.tt(dst, hs[:, :n], pu[:, :n], ALU.mult, e="dve")
        v = 0
        for dt in range(8):
            WD = load_w(wd[e, dt]).rearrange("p (k d) -> p k d", d=512)
            for b in range(2):
                q = e * 2 + b
                for rs in range(4):
                    py = PY[v % 2]; y = Y[v % 3]; v += 1
                    for fc in range(8):
                        p.mm(py, HT[b][:, fc, rs * 128:(rs + 1) * 128], WD[:, fc, :], start=(fc == 0), stop=(fc == 7))
                    p.ts(y, py, G[:, q, rs:rs + 1], ALU.mult, e=("dve", "act")[0])
                    p.dma("sp", yg[q, rs * 128:(rs + 1) * 128, dt * 512:(dt + 1) * 512], y)
                py = PY[v % 2]; y = Y[v % 3]; v += 1
                for fc in range(8):
                    p.mm(py[:32], HTc[:, fc, b * 32:(b + 1) * 32], WD[:, fc, :], start=(fc == 0), stop=(fc == 7))
                p.ts(y[:32], py[:32], Gc[:, q:q + 1], ALU.mult, e="dve")
                p.dma("sp", ygc[q, :, dt * 512:(dt + 1) * 512], y[:32])
    p.finish([yg, ygc])
    return p


def build_combine():
    p = P()
    xl = p.dram("xl", [1024, D], F32, "ExternalInput"); xc = p.dram("xc", [64, D], F32, "ExternalInput")
    g_l = p.dram("g_l", [128, D], F32, "ExternalInput"); g_c = p.dram("g_c", [128, D], F32, "ExternalInput")
    rk = p.dram("rk", [128, 9, 16], F32, "ExternalInput")
    eo = p.dram("eo", [128, 2, 16], F32, "ExternalInput")
    yall = p.dram("yall", [16 * 512, D], BF16, "ExternalInput")
    ycall = p.dram("ycall", [16 * 32, D], BF16, "ExternalInput")
    idn = p.dram("idn", [128, 128], BF16, "ExternalInput")
    ol = p.dram("ol", [1024, D], F32, "ExternalOutput"); oc = p.dram("oc", [64, D], F32, "ExternalOutput")
    BIG = 1.0e6
    GL = p.sb("GL", [128, D]); GC = p.sb("GC", [128, D]); idt = p.sb("idt", [128, 128], BF16)
    p.dma("sp", GL, g_l); p.dma("sp", GC, g_c); p.dma("sp", idt, idn)
    RK = p.sb("RK", [128, 9, 16]); EO = p.sb("EO", [128, 2, 16]); SEL = p.sb("SEL", [128, 9, 16]); IDF = p.sb("IDF", [128, 9, 16])
    IDI = p.sb("IDI", [128, 9, 16], I32)
    p.dma("sp", RK, rk); p.dma("sp", EO, eo)
    for (t0, t1, cap, ty) in ((0, 8, 512, 0), (8, 9, 32, 1)):
        r = RK[:, t0:t1, :]
        p.ts(SEL[:, t0:t1, :], r, float(cap), ALU.is_lt)
        p.tt(IDF[:, t0:t1, :], r, EO[:, ty:ty + 1, :].to_broadcast([128, t1 - t0, 16]), ALU.add)
        p.tt(IDF[:, t0:t1, :], IDF[:, t0:t1, :], SEL[:, t0:t1, :], ALU.mult)
        p.ts(IDF[:, t0:t1, :], IDF[:, t0:t1, :], BIG, ALU.add)
    p.copy(IDI, IDF)
    NG = 6
    GB = [p.sb("GB%d" % i, [128, D], BF16) for i in range(NG)]
    X = [p.sb("X%d" % i, [128, D]) for i in range(2)]
    TM = [p.sb("TM%d" % i, [128, 512]) for i in range(2)]
    PS = [p.ps("pc%d" % i, [128, 512]) for i in range(8)]
    k = 0; u = 0
    for t in range(9):
        n = 128 if t < 8 else 64
        src = xl[t * 128:(t + 1) * 128, :] if t < 8 else xc
        dst = ol[t * 128:(t + 1) * 128, :] if t < 8 else oc
        ysrc = yall if t < 8 else ycall
        nrows = 16 * 512 if t < 8 else 16 * 32
        Xt = X[t % 2]
        p.dma("sp", Xt[:n], src)
        for e in range(16):
            gb = GB[k % NG]; k += 1
            p.memset(gb[:n], 0.0, e="dve")
            _idma(p, gb[:n], ysrc, IDI[:n, t, e:e + 1], bounds=nrows - 1)
            for ct in range(8):
                p.mm(PS[ct][:n], idt[:n, :n], gb[:n, ct * 512:(ct + 1) * 512], start=(e == 0), stop=(e == 15))
        G_ = GL if t < 8 else GC
        for ct in range(8):
            cs = slice(ct * 512, (ct + 1) * 512)
            tm = TM[u % 2]; u += 1
            p.tt(tm[:n], PS[ct][:n], G_[:n, cs], ALU.mult, e="dve")
            p.tt(Xt[:n, cs], Xt[:n, cs], tm[:n], ALU.add, e="pool")
        p.dma("sp", dst, Xt[:n])
    p.finish([ol, oc])
    return p


def _prog(name, fn):
    if name not in _CACHE:
        _CACHE[name] = fn()
    return _CACHE[name]


def _rep(v, n=128):
    return np.ascontiguousarray(np.broadcast_to(np.asarray(v)[None], (n,) + tuple(np.asarray(v).shape)))


def _fm(a):
    return np.ascontiguousarray(a.reshape(2, TB, -1).transpose(0, 2, 1))


def _tokc(a):
    return np.ascontiguousarray(a.reshape(2, NCH, 64, -1).transpose(0, 2, 1, 3))


_U64 = np.triu(np.ones((64, 64), np.float32))
_V64 = np.tril(np.ones((64, 64), np.float32), -1)


def _run_norm(xl, xc, nw, mod_l, sc_i, sh_i, out_bf16=True, modulate=True, router_w=None):
    name = "norm_%d%d%d" % (out_bf16, modulate, router_w is not None)
    p = _prog(name, lambda: build_norm(out_bf16, modulate, router_w is not None))
    nwr = _rep(nw)
    ims = []
    extra = {}
    if router_w is not None:
        extra = {"wr": np.ascontiguousarray(router_w.reshape(32, 128, 16).transpose(1, 0, 2)), "idn": np.eye(128, dtype=np.float32)}
    reps = {}
    if modulate:
        for m in range(3):
            reps[m] = (_rep(mod_l[m, sc_i * D:(sc_i + 1) * D]), _rep(mod_l[m, sh_i * D:(sh_i + 1) * D]))
    for i in range(8):
        b = i // 4
        d = {"xl": xl[i * 1024:(i + 1) * 1024], "xc": xc[i * 64:(i + 1) * 64], "nw": nwr}
        if modulate:
            d.update({"sc_l": reps[b][0], "sh_l": reps[b][1], "sc_c": reps[2][0], "sh_c": reps[2][1]})
        d.update(extra)
        ims.append(d)
    res = run(p, ims).results
    ol = np.concatenate([r["ol"] for r in res], 0); oc = np.concatenate([r["oc"] for r in res], 0)
    if router_w is not None:
        return ol, oc, np.concatenate([r["al"] for r in res], 0), np.concatenate([r["ac"] for r in res], 0)
    return ol, oc


def _layer(l, xl, xc, mod_l, W):
    h1l, h1c = _run_norm(xl, xc, W["norm1_w"][l], mod_l, 1, 0)
    h = tok_order(h1l.reshape(2, 4096, D), h1c.reshape(2, 256, D))
    hT = np.ascontiguousarray(h.reshape(68, 128, 32, 128).transpose(0, 3, 2, 1))
    del h
    p = _prog("proj", build_proj)
    w_in = W["w_in"][l]
    ims = []
    sls = []
    for c in range(8):
        idx, sl = head_cols(c); sls.append(sl)
        ims.append({"hT": hT, "w": np.ascontiguousarray(w_in[:, idx].reshape(32, 128, NPROJ).transpose(1, 0, 2))})
    proj = [r["o"] for r in run(p, ims).results]
    del ims, hT
    tabs = fnet_tables()
    p = _prog("fnet", build_fnet_sconv)
    ims = []
    for c in range(8):
        pc = proj[c]; sl = sls[c]
        d = {"finT": _fm(pc[:, sl["fin"]]), "shT": _fm(pc[:, sl["sh"]]), "sbT": _fm(pc[:, sl["sb"]]), "sgT": _fm(pc[:, sl["sg"]]),
             "cw": np.ascontiguousarray(W["sc_conv_w"][l][:, c * 128:(c + 1) * 128].T)}
        d.update(tabs)
        ims.append(d)
    res = run(p, ims).results
    ob = np.stack([r["ob"] for r in res]); od = np.stack([r["od"] for r in res])
    p = _prog("gla", build_gla)
    ims = []
    for c in range(8):
        pc = proj[c]; sl = sls[c]
        lr = pc[:, sl["glr"]]
        ims.append({"qT": _fm(pc[:, sl["gq"]]), "kT": _fm(pc[:, sl["gk"]]), "vtok": _tokc(pc[:, sl["gv"]]),
                    "ggtok": _tokc(pc[:, sl["gg"]]).reshape(2, 64, -1),
                    "lrT": np.ascontiguousarray(np.stack([_fm(lr[:, :16]), _fm(lr[:, 16:])], 1)),
                    "up": np.ascontiguousarray(W["gla_gate_up"][l][:, :, c * 64:(c + 1) * 64]),
                    "gb": np.ascontiguousarray(W["gla_gate_b"][l][:, c * 64:(c + 1) * 64].T),
                    "nwr": _rep(W["gla_norm_w"][l], 64),
                    "msk": np.stack([_U64, _U64.T.copy()]), "idn": np.eye(64, dtype=np.float32)})
    oa = np.stack([r["oa"] for r in run(p, ims).results])
    p = _prog("gdn_pre", build_gdn_pre)
    ims = []
    gw = W["gdn_conv_w"][l]
    for c in range(8):
        pc = proj[c]; sl = sls[c]
        comps = [pc[:, sl[n]] for n in ("dq", "dk", "dv")]
        raw = np.ascontiguousarray(np.stack([_tokc(a) for a in comps], 1))
        rawc = np.ascontiguousarray(np.stack([a.reshape(2, TB, 128)[:, :256] for a in comps], 1))
        cwr = np.stack([np.broadcast_to(gw[:, j * 1024 + c * 128: j * 1024 + (c + 1) * 128][None], (64, 3, 128)) for j in range(3)])
        ims.append({"raw": raw, "rawc": rawc, "cwr": np.ascontiguousarray(cwr)})
    qkvn = [r["o"] for r in run(p, ims).results]
    p = _prog("gdn_main", build_gdn_main)
    ims = []
    msk = np.stack([_U64, _V64, _U64.T.copy(), _V64.T.copy()])
    for c in range(8):
        pc = proj[c]; sl = sls[c]
        q5 = qkvn[c].reshape(2, 3, 64, NCH, 128)

        def featmajor(x_):
            return np.ascontiguousarray(x_.transpose(0, 3, 2, 1).reshape(2, 128, TB))
        prm = np.array([W["gdn_dt_bias"][l][0, c], W["gdn_dt_bias"][l][1, c], W["gdn_A_log"][l][0, c], W["gdn_A_log"][l][1, c]], np.float32)
        ims.append({"qT": featmajor(q5[:, 0]), "kT": featmajor(q5[:, 1]), "ktok": np.ascontiguousarray(q5[:, 1]),
                    "vtok": np.ascontiguousarray(q5[:, 2]),
                    "atok": np.ascontiguousarray(np.stack([_tokc(pc[:, sl["af"]])[..., 0], _tokc(pc[:, sl["ab"]])[..., 0]], 1)),
                    "btok": np.ascontiguousarray(np.stack([_tokc(pc[:, sl["bf"]])[..., 0], _tokc(pc[:, sl["bb"]])[..., 0]], 1)),
                    "ztok": _tokc(pc[:, sl["dz"]]).reshape(2, 64, -1), "prm": _rep(prm),
                    "nwr": _rep(W["gdn_norm_w"][l], 64), "msk": msk, "idn": np.eye(64, dtype=np.float32)})
    oc_ = np.stack([r["oc"] for r in run(p, ims).results])
    del ims, proj, qkvn

    def tokmaj_to_fm(o):
        o = o.reshape(8, 2, 64, NCH, 128).transpose(0, 4, 1, 3, 2)
        return o.reshape(8 * 128, 2, TB)

    def fmh(o):
        return o.transpose(0, 2, 1, 3).reshape(8 * 128, 2, TB)
    mixT = np.concatenate([tokmaj_to_fm(oa), fmh(ob), tokmaj_to_fm(oc_), fmh(od)], 0)
    p = _prog("wout", build_wout)
    w = np.ascontiguousarray(W["w_out"][l].reshape(32, 128, D).transpose(1, 0, 2))
    g1 = [_rep(mod_l[m, 2 * D:3 * D]) for m in range(3)]
    ims = []
    for i in range(8):
        b = i // 4; j = i % 4
        lat = mixT[:, b, 256 + j * 1024: 256 + (j + 1) * 1024]; cx = mixT[:, b, j * 64:j * 64 + 64]
        mt = np.concatenate([lat, cx], 1).reshape(32, 128, 1088).transpose(1, 0, 2)
        ims.append({"mixT": np.ascontiguousarray(mt), "w": w, "xl": xl[i * 1024:(i + 1) * 1024], "xc": xc[i * 64:(i + 1) * 64],
                    "g_l": g1[b], "g_c": g1[2]})
    res = run(p, ims).results
    xal = np.concatenate([r["ol"] for r in res], 0); xac = np.concatenate([r["oc"] for r in res], 0)
    del ims, mixT
    h2, h2c, al, ac = _run_norm(xal, xac, W["norm2_w"][l], mod_l, 4, 3, router_w=W["w_router"][l])
    al = al.reshape(2, 4096, 16); ac = ac.reshape(2, 256, 16)
    h2 = h2.reshape(2, 4096, D); h2c = h2c.reshape(2, 256, D)
    p = _prog("route", build_route)
    iota = _rep(np.arange(512, dtype=np.float32))
    tv = (np.arange(32)[None, :] * 128 + np.arange(128)[:, None]).astype(np.float32)
    ims = []
    for j in range(8):
        arow = []; acol = []; arowc = []; acolc = []
        for e in (2 * j, 2 * j + 1):
            for b in range(2):
                a = al[b, :, e]; c_ = ac[b, :, e]
                arow.append(np.broadcast_to(a[None], (128, 4096))); acol.append(a.reshape(32, 128).T)
                arowc.append(np.broadcast_to(c_[None], (128, 256))); acolc.append(c_.reshape(2, 128).T)
        ims.append({"arow": np.ascontiguousarray(np.stack(arow)), "acol": np.ascontiguousarray(np.stack(acol)),
                    "arowc": np.ascontiguousarray(np.stack(arowc)), "acolc": np.ascontiguousarray(np.stack(acolc)), "iota": iota, "tv": tv})
    route = []
    for r in run(p, ims).results:
        route.append({"idx": np.ascontiguousarray(r["idxr"].reshape(4, 4, 128).transpose(0, 2, 1)),
                      "gate": np.ascontiguousarray(r["gater"].reshape(4, 4, 128).transpose(0, 2, 1)),
                      "idxc": np.ascontiguousarray(r["idxcr"].reshape(4, 32, 1)), "gatec": np.ascontiguousarray(r["gatecr"].reshape(4, 32, 1)),
                      "rank": r["rank"], "rankc": r["rankc"]})
    import ml_dtypes
    p = _prog("experts", build_experts)
    idn = np.eye(128, dtype=np.float32).astype(ml_dtypes.bfloat16)
    ims = []
    for j in range(8):
        es = slice(2 * j, 2 * j + 2)
        wg = np.ascontiguousarray(W["w_gate"][l][es].reshape(2, 32, 128, 8, 128).transpose(0, 3, 2, 1, 4).reshape(2, 8, 128, 4096))
        wu = np.ascontiguousarray(W["w_up"][l][es].reshape(2, 32, 128, 8, 128).transpose(0, 3, 2, 1, 4).reshape(2, 8, 128, 4096))
        wd = np.ascontiguousarray(W["w_down"][l][es].reshape(2, 8, 128, 8, 512).transpose(0, 3, 2, 1, 4).reshape(2, 8, 128, 4096))
        r = route[j]
        ims.append({"idx": r["idx"], "gate": r["gate"], "idxc": r["idxc"], "gatec": r["gatec"],
                    "h2_0": h2[0], "h2_1": h2[1], "h2c_0": h2c[0], "h2c_1": h2c[1], "wg": wg, "wu": wu, "wd": wd, "idn": idn})
    res = run(p, ims).results
    yg = [r["yg"] for r in res]; ygc = [r["ygc"] for r in res]
    del ims
    yall = []; ycall = []
    rank = np.zeros((2, 4096, 16), np.float32); rankc = np.zeros((2, 256, 16), np.float32)
    zr = np.zeros((1, D), np.float32)
    for b in range(2):
        rows = []; rowsc = []
        for e in range(16):
            j = e // 2; q = (e % 2) * 2 + b
            rows.append(yg[j][q]); rowsc.append(ygc[j][q])
            rank[b, :, e] = route[j]["rank"][q].T.reshape(-1)
            rankc[b, :, e] = route[j]["rankc"][q].T.reshape(-1)
        yall.append(np.concatenate(rows, 0)); ycall.append(np.concatenate(rowsc, 0))
    del yg, ygc
    eo = np.stack([np.arange(16) * 512 - 1.0e6, np.arange(16) * 32 - 1.0e6]).astype(np.float32)
    eo = _rep(eo)
    idn_bf = np.eye(128, dtype=np.float32).astype(ml_dtypes.bfloat16)
    p = _prog("combine", build_combine)
    g2 = [_rep(mod_l[m, 5 * D:6 * D]) for m in range(3)]
    ims = []
    for i in range(8):
        b = i // 4; j = i % 4
        rk = np.full((128, 9, 16), 1e6, np.float32)
        rk[:, :8, :] = rank[b, j * 1024:(j + 1) * 1024].reshape(8, 128, 16).transpose(1, 0, 2)
        rk[:64, 8, :] = rankc[b, j * 64:(j + 1) * 64]
        ims.append({"xl": xal[i * 1024:(i + 1) * 1024], "xc": xac[i * 64:(i + 1) * 64], "g_l": g2[b], "g_c": g2[2],
                    "rk": rk, "eo": eo, "yall": yall[b], "ycall": ycall[b], "idn": idn_bf})
    res = run(p, ims).results
    return np.concatenate([r["ol"] for r in res], 0), np.concatenate([r["oc"] for r in res], 0)


def kernel(x, c, ctx, c_ctx, w_ada, b_ada, norm1_w, norm2_w, w_in, w_out, gla_gate_up, gla_gate_b, gla_norm_w,
           gdn_conv_w, gdn_A_log, gdn_dt_bias, gdn_norm_w, sc_conv_w, w_router, w_gate, w_up, w_down, final_norm_w):
    W = dict(norm1_w=norm1_w, norm2_w=norm2_w, w_in=w_in, w_out=w_out, gla_gate_up=gla_gate_up, gla_gate_b=gla_gate_b,
             gla_norm_w=gla_norm_w, gdn_conv_w=gdn_conv_w, gdn_A_log=gdn_A_log, gdn_dt_bias=gdn_dt_bias, gdn_norm_w=gdn_norm_w,
             sc_conv_w=sc_conv_w, w_router=w_router, w_gate=w_gate, w_up=w_up, w_down=w_down)
    W = {k: np.asarray(v, np.float32) for k, v in W.items()}
    x = np.asarray(x, np.float32); ctx = np.asarray(ctx, np.float32)
    w_ada = np.asarray(w_ada, np.float32); b_ada = np.asarray(b_ada, np.float32)
    p = _prog("mod", build_mod)
    cvec = np.concatenate([np.asarray(c, np.float32), np.asarray(c_ctx, np.float32)[None]], 0)
    cT = np.ascontiguousarray(cvec.T.reshape(32, 128, 3).transpose(1, 0, 2))
    ims = []
    for j in range(8):
        wj = np.ascontiguousarray(w_ada[:, :, j * 3072:(j + 1) * 3072].reshape(2, 32, 128, 3072).transpose(0, 2, 1, 3))
        ims.append({"cT": cT, "w": wj, "b": np.ascontiguousarray(b_ada[:, None, j * 3072:(j + 1) * 3072])})
    mod = np.concatenate([r["o"] for r in run(p, ims).results], axis=2)
    del ims
    xl = x.reshape(8192, D); xc = ctx.reshape(512, D)
    for l in range(2):
        xl, xc = _layer(l, xl, xc, mod[l], W)
    ol, _ = _run_norm(xl, xc, np.asarray(final_norm_w, np.float32), None, 0, 0, out_bf16=False, modulate=False)
    return ol.reshape(2, 4096, D).astype(np.float32)
```

```python
import numpy as np
import concourse.bass as bass
import concourse.mybir as mybir
from concourse.bass_utils import run_bass_kernel_spmd

F32 = mybir.dt.float32
BF16 = mybir.dt.bfloat16
I32 = mybir.dt.int32
U32 = mybir.dt.uint32
AF = mybir.ActivationFunctionType
ALU = mybir.AluOpType
AX = mybir.AxisListType

SEM_WRAP = 1 << 30


class P:
    def __init__(self, n_dma_sems=24):
        self.nc = bass.Bass("TRN2", target_bir_lowering=False)
        nc = self.nc
        self.eng = {"pe": nc.tensor, "dve": nc.vector, "act": nc.scalar, "pool": nc.gpsimd, "sp": nc.sync}
        self.sem = {k: nc.alloc_semaphore("s_" + k) for k in self.eng}
        self.cnt = {k: 0 for k in self.eng}
        self.know = {k: {} for k in self.eng}
        self.dsem = [nc.alloc_semaphore("d%d" % i) for i in range(n_dma_sems)]
        self.dval = [0] * n_dma_sems
        self.dnext = 0
        self.state = {}
        self.n_ins = 0
        self.n_wait = 0

    def dram(self, name, shape, dt, kind):
        return self.nc.dram_tensor(name, list(shape), dt, kind=kind).ap()

    def sb(self, name, shape, dt=F32):
        return self.nc.alloc_sbuf_tensor("sb_" + name, list(shape), dt).ap()

    def ps(self, name, shape, dt=F32):
        return self.nc.alloc_psum_tensor("ps_" + name, list(shape), dt).ap()

    def _wait(self, e, tok):
        if tok is None:
            return
        if tok[0] == "e":
            src, pos = tok[1], tok[2]
            if src == e and e == "pe":
                return
            kn = self.know[e].get(src, 0)
            if kn >= pos:
                return
            self.eng[e].wait_ge(self.sem[src], pos)
            self.know[e][src] = pos
            self.n_wait += 1
        else:
            i, val = tok[1], tok[2]
            key = ("d", i)
            if self.know[e].get(key, 0) >= val:
                return
            self.eng[e].wait_ge(self.dsem[i], val)
            self.know[e][key] = val
            self.n_wait += 1

    def _name(self, ref):
        ap = ref[0] if isinstance(ref, tuple) else ref
        return ap if isinstance(ap, str) else ap.tensor.name

    def _st(self, ref):
        if isinstance(ref, tuple):
            ap, key = ref
        else:
            ap, key = ref, None
        name = ap if isinstance(ap, str) else ap.tensor.name
        d = self.state.setdefault(name, {})
        return d, key

    def _deps(self, e, reads, writes):
        need = {}

        def add(tok):
            if tok is None:
                return
            k = (tok[0], tok[1])
            if need.get(k, 0) < tok[2]:
                need[k] = tok[2]
        for r in reads:
            d, key = self._st(r)
            keys = list(d.keys()) if key is None else [k for k in d if k is None or k == key]
            for k in keys:
                add(d[k][0])
        for w in writes:
            d, key = self._st(w)
            keys = list(d.keys()) if key is None else [k for k in d if k is None or k == key]
            for k in keys:
                add(d[k][0])
                for t in d[k][1]:
                    add(t)
        for (ty, src), v in need.items():
            self._wait(e, (ty, src, v))

    @staticmethod
    def _addreader(lst, tok):
        for i, t in enumerate(lst):
            if t[0] == tok[0] and t[1] == tok[1]:
                if t[2] < tok[2]:
                    lst[i] = tok
                return
        lst.append(tok)

    def _commit(self, tok, reads, writes):
        for r in reads:
            d, key = self._st(r)
            if key is None:
                if not d:
                    d[None] = [None, []]
                for k in d:
                    self._addreader(d[k][1], tok)
            else:
                if key not in d:
                    base = d.get(None, [None, []])
                    d[key] = [base[0], list(base[1])]
                self._addreader(d[key][1], tok)
        for w in writes:
            d, key = self._st(w)
            if key is None:
                d.clear()
                d[None] = [tok, []]
            else:
                if None in d and key not in d:
                    pass
                d[key] = [tok, []]

    def op(self, e, fn, reads, writes):
        pr = [r for r in reads if self._name(r).startswith("ps_")]
        if pr:
            reads = [r for r in reads if not self._name(r).startswith("ps_")]
            writes = list(writes) + pr
        self._deps(e, reads, writes)
        ins = fn()
        self.cnt[e] += 1
        ins.then_inc(self.sem[e], 1)
        tok = ("e", e, self.cnt[e])
        self.know[e][e] = self.know[e].get(e, 0)
        self._commit(tok, reads, writes)
        self.n_ins += 1
        return ins

    def dma(self, q, out, in_, reads=None, writes=None, **kw):
        reads = [in_] if reads is None else reads
        writes = [out] if writes is None else writes
        out = self._a(out); in_ = self._a(in_)
        i = self.dnext
        self.dnext = (self.dnext + 1) % len(self.dsem)
        if self.dval[i] > 0:
            self._wait(q, ("d", i, self.dval[i]))
        self._deps(q, reads, writes)
        ins = self.eng[q].dma_start(out=out, in_=in_, **kw)
        self.dval[i] += 16
        ins.then_inc(self.dsem[i], 16)
        tok = ("d", i, self.dval[i])
        self._commit(tok, reads, writes)
        self.n_ins += 1
        return tok

    def finish(self, toks_or_refs):
        for r in toks_or_refs:
            d, key = self._st(r)
            for k in d:
                self._wait("sp", d[k][0])
        for e in self.eng:
            if e != "sp" and self.cnt[e] > 0:
                self._wait("sp", ("e", e, self.cnt[e]))

    @staticmethod
    def _a(x):
        return x[0] if isinstance(x, tuple) else x

    def mm(self, out, lhsT, rhs, start=True, stop=True, okey=None, rkeys=None):
        rd = [lhsT, rhs] if rkeys is None else rkeys
        wr = [out if okey is None else (self._a(out), okey)]
        o_, l_, r_ = self._a(out), self._a(lhsT), self._a(rhs)
        return self.op("pe", lambda: self.nc.tensor.matmul(o_, l_, r_, start=start, stop=stop), rd, wr)

    def tr(self, out, in_, ident):
        o_, i_, d_ = self._a(out), self._a(in_), self._a(ident)
        return self.op("pe", lambda: self.nc.tensor.transpose(o_, i_, d_), [in_, ident], [out])

    def act(self, out, in_, func, bias=None, scale=None, accum_out=None, extra_reads=()):
        kw = {}
        rd = [in_] + list(extra_reads)
        if bias is not None:
            kw["bias"] = self._a(bias)
            if not isinstance(bias, (int, float)):
                rd.append(bias)
        if scale is not None:
            kw["scale"] = self._a(scale)
            if not isinstance(scale, (int, float)):
                rd.append(scale)
        wr = [out]
        if accum_out is not None:
            kw["accum_out"] = self._a(accum_out)
            wr.append(accum_out)
        o_, i_ = self._a(out), self._a(in_)
        return self.op("act", lambda: self.nc.scalar.activation(o_, i_, func, **kw), rd, wr)

    def _veng(self, e):
        return self.nc.vector if e == "dve" else self.nc.gpsimd

    def copy(self, out, in_, e="dve"):
        o_, i_ = self._a(out), self._a(in_)
        if e == "act":
            return self.op("act", lambda: self.nc.scalar.copy(o_, i_), [in_], [out])
        return self.op(e, lambda: self._veng(e).tensor_copy(o_, i_), [in_], [out])

    def tt(self, out, in0, in1, op, e="dve"):
        o_, a_, b_ = self._a(out), self._a(in0), self._a(in1)
        return self.op(e, lambda: self._veng(e).tensor_tensor(o_, a_, b_, op), [in0, in1], [out])

    def ts(self, out, in0, s1, op0, s2=None, op1=None, accum_out=None, e="dve"):
        rd = [in0]
        if not isinstance(s1, (int, float)):
            rd.append(s1)
        if s2 is not None and not isinstance(s2, (int, float)):
            rd.append(s2)
        wr = [out]
        kw = {}
        if op1 is not None:
            kw["op1"] = op1
        if accum_out is not None:
            kw["accum_out"] = self._a(accum_out)
            wr.append(accum_out)
        o_, a_, s1_, s2_ = self._a(out), self._a(in0), self._a(s1), self._a(s2)
        return self.op(e, lambda: self._veng(e).tensor_scalar(o_, a_, s1_, s2_, op0, **kw), rd, wr)

    def stt(self, out, in0, scalar, in1, op0, op1, e="dve"):
        rd = [in0, in1]
        if not isinstance(scalar, (int, float)):
            rd.append(scalar)
        o_, a_, s_, b_ = self._a(out), self._a(in0), self._a(scalar), self._a(in1)
        return self.op(e, lambda: self._veng(e).scalar_tensor_tensor(o_, a_, s_, b_, op0, op1), rd, [out])

    def memset(self, out, val, e="dve"):
        o_ = self._a(out)
        return self.op(e, lambda: self._veng(e).memset(o_, val), [], [out])

    def reduce(self, out, in_, op=ALU.add, axis=AX.X, e="dve"):
        o_, i_ = self._a(out), self._a(in_)
        return self.op(e, lambda: self._veng(e).tensor_reduce(o_, i_, axis, op), [in_], [out])

    def recip(self, out, in_):
        o_, i_ = self._a(out), self._a(in_)
        return self.op("dve", lambda: self.nc.vector.reciprocal(o_, i_), [in_], [out])


def run(p, in_maps, n=8):
    return run_bass_kernel_spmd(p.nc, in_maps, core_ids=list(range(n)))


D = 4096
NCORES = 8


def EPS_AP(p, val=1e-6):
    key = "_eps%g" % val
    if not hasattr(p, key):
        t = p.sb("eps%d" % len([k for k in p.__dict__ if k.startswith("_eps")]), [128, 1])
        p.memset(t, val)
        setattr(p, key, t)
    return getattr(p, key)
_CACHE = {}


def build_norm(out_bf16=True, modulate=True, router=False):
    p = P()
    xl = p.dram("xl", [1024, D], F32, "ExternalInput")
    xc = p.dram("xc", [64, D], F32, "ExternalInput")
    nw = p.dram("nw", [128, D], F32, "ExternalInput")
    odt = BF16 if out_bf16 else F32
    ol = p.dram("ol", [1024, D], odt, "ExternalOutput")
    oc = p.dram("oc", [64, D], odt, "ExternalOutput")
    nw_t = p.sb("nw_t", [128, D])
    p.dma("sp", nw_t, nw)
    wv = {}; sh = {}
    if modulate:
        for ty in ("l", "c"):
            sc_d = p.dram("sc_" + ty, [128, D], F32, "ExternalInput")
            sh_d = p.dram("sh_" + ty, [128, D], F32, "ExternalInput")
            wv[ty] = p.sb("wv_" + ty, [128, D]); sh[ty] = p.sb("sh_" + ty, [128, D])
            p.dma("sp", wv[ty], sc_d); p.dma("sp", sh[ty], sh_d)
            p.stt(wv[ty], wv[ty], 1.0, nw_t, ALU.add, ALU.mult)
    else:
        wv = {"l": nw_t, "c": nw_t}
    if router:
        wr = p.dram("wr", [128, 32, 16], F32, "ExternalInput")
        idn = p.dram("idn", [128, 128], F32, "ExternalInput")
        al = p.dram("al", [1024, 16], F32, "ExternalOutput"); ac = p.dram("ac", [64, 16], F32, "ExternalOutput")
        wrt = p.sb("wrt", [128, 32, 16]); idt = p.sb("idt", [128, 128])
        p.dma("sp", wrt, wr); p.dma("sp", idt, idn)
        hT = p.sb("hT", [128, 32, 128])
        ptr = [p.ps("ptr%d" % i, [128, 512]) for i in range(3)]
        plg = p.ps("plg", [128, 16])
        lg = p.sb("lg", [128, 16]); mx = p.sb("mx", [128, 1]); sm = p.sb("sm", [128, 1]); af = [p.sb("af%d" % i, [128, 16]) for i in range(2)]
    xt = [p.sb("xt%d" % i, [128, D]) for i in range(2)]
    sq = p.sb("sq", [128, D])
    ot = [p.sb("ot%d" % i, [128, D], odt) for i in range(2)]
    ss = [p.sb("ss%d" % i, [128, 1]) for i in range(2)]
    rs = [p.sb("rs%d" % i, [128, 1]) for i in range(2)]
    for t in range(9):
        n = 128 if t < 8 else 64
        ty = "l" if t < 8 else "c"
        src = xl[t * 128:(t + 1) * 128, :] if t < 8 else xc
        dst = ol[t * 128:(t + 1) * 128, :] if t < 8 else oc
        X = xt[t % 2]; O = ot[t % 2]; S = ss[t % 2]; Rr = rs[t % 2]
        p.dma("sp", X[:n], src)
        p.act(sq[:n], X[:n], AF.Square, accum_out=S[:n])
        p.act(Rr[:n], S[:n], AF.Ln, scale=1.0 / D, bias=EPS_AP(p)[:n])
        p.act(Rr[:n], Rr[:n], AF.Exp, scale=-0.5)
        if modulate:
            p.stt(sq[:n], X[:n], Rr[:n], wv[ty][:n], ALU.mult, ALU.mult, e="dve")
            if router:
                p.tt(sq[:n], sq[:n], sh[ty][:n], ALU.add, e="dve")
                p.copy(O[:n], sq[:n], e="act")
            else:
                p.tt(O[:n], sq[:n], sh[ty][:n], ALU.add, e="dve")
        else:
            p.stt(O[:n], X[:n], Rr[:n], wv[ty][:n], ALU.mult, ALU.mult, e="dve")
        p.dma("sp", dst, O[:n])
        if router:
            for g in range(8):
                pt = ptr[g % 3]
                for i in range(4):
                    kc = g * 4 + i
                    p.tr(pt[:, i * 128:i * 128 + n], sq[:n, kc * 128:(kc + 1) * 128], idt[:n, :n])
                src_v = pt.rearrange("p (a t) -> p a t", a=4)[:, :, :n]
                p.copy(hT[:, g * 4:(g + 1) * 4, :n], src_v, e=("dve", "act")[g % 2])
            for kc in range(32):
                p.mm(plg[:n], hT[:, kc, :n], wrt[:, kc, :], start=(kc == 0), stop=(kc == 31))
            A_ = af[t % 2]
            p.copy(lg[:n], plg[:n], e="dve")
            p.reduce(mx[:n], lg[:n], op=ALU.max, axis=AX.X, e="dve")
            p.ts(mx[:n], mx[:n], -1.0, ALU.mult)
            p.act(A_[:n], lg[:n], AF.Exp, bias=mx[:n], accum_out=sm[:n])
            p.recip(sm[:n], sm[:n])
            p.ts(A_[:n], A_[:n], sm[:n], ALU.mult)
            p.dma("sp", al[t * 128:(t + 1) * 128, :] if t < 8 else ac, A_[:n])
    p.finish([ol, oc] + ([al, ac] if router else []))
    return p


NPROJ = 1444
NTOK = 8704


def build_proj():
    p = P()
    NT = NTOK // 128
    hT = p.dram("hT", [NT, 128, 32, 128], BF16, "ExternalInput")
    w = p.dram("w", [128, 32, NPROJ], F32, "ExternalInput")
    o = p.dram("o", [NTOK, NPROJ], F32, "ExternalOutput")
    wb = p.sb("wb", [128, 32, NPROJ], BF16)
    stg = [p.sb("stg%d" % i, [128, 2, NPROJ]) for i in range(2)]
    for c in range(16):
        s = stg[c % 2]
        p.dma("sp", s, w[:, 2 * c:2 * c + 2, :])
        p.copy((wb[:, 2 * c:2 * c + 2, :], c), s, e=("dve", "act")[c % 2])
    ht = [p.sb("ht%d" % i, [128, 32, 128], BF16) for i in range(3)]
    ot = [p.sb("ot%d" % i, [128, NPROJ]) for i in range(2)]
    pst = [p.ps("pp%d" % i, [128, 512]) for i in range(6)]
    ntl = [(0, 512), (512, 512), (1024, NPROJ - 1024)]
    for t in range(NT):
        H = ht[t % 3]; O = ot[t % 2]
        p.dma("sp", H, hT[t])
        for j, (n0, nn) in enumerate(ntl):
            ps = pst[(t % 2) * 3 + j]
            for kc in range(32):
                p.mm(ps[:, :nn], H[:, kc, :], wb[:, kc, n0:n0 + nn], start=(kc == 0), stop=(kc == 31),
                     rkeys=[H, (wb, kc // 2)])
            if j == 1:
                p.copy(O[:, n0:n0 + nn], ps[:, :nn], e="act")
            else:
                p.copy(O[:, n0:n0 + nn], ps[:, :nn], e="dve")
        p.dma("sp", o[t * 128:(t + 1) * 128, :], O)
    p.finish([o])
    return p


def build_mod():
    p = P()
    NCOL = 3072
    cT = p.dram("cT", [128, 32, 3], F32, "ExternalInput")
    w = p.dram("w", [2, 128, 32, NCOL], F32, "ExternalInput")
    b = p.dram("b", [2, 1, NCOL], F32, "ExternalInput")
    o = p.dram("o", [2, 3, NCOL], F32, "ExternalOutput")
    ct = p.sb("ct", [128, 32, 3]); st = p.sb("st", [128, 32, 3])
    ones = p.sb("ones", [1, 3]); bt = p.sb("bt", [1, 2, NCOL])
    p.dma("sp", ct, cT)
    p.dma("sp", bt[:, 0, :], b[0]); p.dma("sp", bt[:, 1, :], b[1])
    p.act(st, ct, AF.Silu)
    p.memset(ones, 1.0)
    wt = [p.sb("wt%d" % i, [128, 8, 512]) for i in range(3)]
    pst = [p.ps("pm%d" % i, [3, 512]) for i in range(2)]
    ot = p.sb("ot", [3, 2, NCOL])
    i = 0
    for l in range(2):
        for nt in range(6):
            ps = pst[(l * 6 + nt) % 2]
            for kg in range(4):
                W = wt[i % 3]; i += 1
                p.dma("sp", W, w[l, :, kg * 8:(kg + 1) * 8, nt * 512:(nt + 1) * 512])
                for k in range(8):
                    p.mm(ps, st[:, kg * 8 + k, :], W[:, k, :], start=(kg == 0 and k == 0), stop=False)
            p.mm(ps, ones, bt[:, l, nt * 512:(nt + 1) * 512], start=False, stop=True)
            p.copy(ot[:, l, nt * 512:(nt + 1) * 512], ps, e="dve")
    p.dma("sp", o[0], ot[:, 0, :]); p.dma("sp", o[1], ot[:, 1, :])
    p.finish([o])
    return p


IN_SIZES = (512, 512, 1024, 1024, 32, 1024, 3072, 1024, 16, 16, 1024, 1024, 1024)
IN_OFF = np.concatenate([[0], np.cumsum(IN_SIZES)]).astype(int)


def head_cols(h):
    o = IN_OFF
    segs = [("gq", o[0] + h * 64, 64), ("gk", o[1] + h * 64, 64), ("gv", o[2] + h * 128, 128), ("gg", o[3] + h * 128, 128),
            ("glr", o[4], 32), ("fin", o[5] + h * 128, 128),
            ("dq", o[6] + h * 128, 128), ("dk", o[6] + 1024 + h * 128, 128), ("dv", o[6] + 2048 + h * 128, 128),
            ("dz", o[7] + h * 128, 128), ("bf", o[8] + h, 1), ("bb", o[8] + 8 + h, 1), ("af", o[9] + h, 1), ("ab", o[9] + 8 + h, 1),
            ("sh", o[10] + h * 128, 128), ("sb", o[11] + h * 128, 128), ("sg", o[12] + h * 128, 128)]
    idx = np.concatenate([np.arange(s, s + n) for _, s, n in segs])
    sl = {}
    pos = 0
    for nm, s, n in segs:
        sl[nm] = slice(pos, pos + n); pos += n
    assert pos == NPROJ
    return idx, sl


def tok_order(lat, ctx):
    return np.concatenate([np.concatenate([ctx[b], lat[b]], 0) for b in range(2)], 0)


TB = 4352
NCH = 68


def build_fnet_sconv():
    p = P()
    finT = p.dram("finT", [2, 128, TB], F32, "ExternalInput")
    shT = p.dram("shT", [2, 128, TB], F32, "ExternalInput")
    sbT = p.dram("sbT", [2, 128, TB], F32, "ExternalInput")
    sgT = p.dram("sgT", [2, 128, TB], F32, "ExternalInput")
    cw = p.dram("cw", [128, 3], F32, "ExternalInput")
    cs_l = p.dram("cs_l", [128, 256], F32, "ExternalInput")
    cs_c = p.dram("cs_c", [128, 256], F32, "ExternalInput")
    tab = p.dram("tab", [8, 4, 128, 2, 8, 512], BF16, "ExternalInput")
    tabc = p.dram("tabc", [128, 2, 2, 256], BF16, "ExternalInput")
    ob = p.dram("ob", [2, 128, TB], BF16, "ExternalOutput")
    od = p.dram("od", [2, 128, TB], BF16, "ExternalOutput")
    A = [p.sb("A%d" % i, [128, TB]) for i in range(4)]
    cwt = p.sb("cwt", [128, 3]); csl = p.sb("csl", [128, 256]); csc = p.sb("csc", [128, 256])
    tct = p.sb("tct", [128, 2, 2, 256], BF16)
    p.dma("sp", cwt, cw); p.dma("sp", csl, cs_l); p.dma("sp", csc, cs_c); p.dma("sp", tct, tabc)
    gcs = [p.sb("gcs%d" % b, [128, 34, 256], BF16) for b in range(2)]
    ps1 = [p.ps("f1_%d" % i, [128, 512]) for i in range(2)]
    ps2 = [p.ps("f2_%d" % i, [128, 512]) for i in range(4)]
    for b in range(2):
        p.dma("sp", A[b], finT[b])
    k = 0
    for b in range(2):
        for g in range(17):
            ps = ps1[k % 2]; k += 1
            for j in range(2):
                tcn = 2 * g + j
                p.mm(ps[:, j * 256:(j + 1) * 256], A[b][:, tcn * 128:(tcn + 1) * 128], csc if tcn < 2 else csl)
            p.copy(gcs[b][:, 2 * g:2 * g + 2, :], ps.rearrange("p (a n) -> p a n", a=2), e=("dve", "act")[k % 2])
    obt = [p.sb("obt%d" % b, [128, TB], BF16) for b in range(2)]
    for b in range(2):
        ps = ps2[b]
        i = 0
        for cs in range(2):
            for tcn in range(2):
                p.mm(ps[:, :256], gcs[b][:, tcn, cs * 128:(cs + 1) * 128], tct[:, cs, tcn, :], start=(i == 0), stop=(i == 3)); i += 1
        p.copy(obt[b][:, 0:256], ps[:, :256], e="dve")
    tb_ = [p.sb("tb%d" % i, [128, 2, 8, 512], BF16) for i in range(4)]
    k = 0
    for j in range(8):
        for tg in range(4):
            T_ = tb_[k % 4]; k += 1
            p.dma("sp", T_, tab[j, tg])
            for b in range(2):
                ps = ps2[(j % 2) * 2 + b]
                for cs in range(2):
                    for t8 in range(8):
                        tcn = 2 + tg * 8 + t8
                        p.mm(ps, gcs[b][:, tcn, cs * 128:(cs + 1) * 128], T_[:, cs, t8, :],
                             start=(tg == 0 and cs == 0 and t8 == 0), stop=(tg == 3 and cs == 1 and t8 == 7))
        for b in range(2):
            p.copy(obt[b][:, 256 + j * 512:256 + (j + 1) * 512], ps2[(j % 2) * 2 + b], e=("dve", "act")[b])
    for b in range(2):
        p.dma("sp", ob[b], obt[b])
    odt = [p.sb("odt%d" % b, [128, TB], BF16) for b in range(2)]
    for b in range(2):
        SH, SB_, SG, ACC = A[0], A[1], A[2], A[3]
        p.dma("sp", SH, shT[b]); p.dma("sp", SB_, sbT[b]); p.dma("sp", SG, sgT[b])
        p.tt(SG, SG, SH, ALU.mult, e="dve")
        p.ts(ACC, SG, cwt[:, 1:2], ALU.mult, e="dve")
        for (lo, n, rows) in ((0, 256, 1), (256, 4096, 64)):
            w_ = n // rows
            u3 = SG[:, lo:lo + n].rearrange("p (r w) -> p r w", r=rows)
            a3 = ACC[:, lo:lo + n].rearrange("p (r w) -> p r w", r=rows)
            p.stt(a3[:, :, 1:], u3[:, :, :w_ - 1], cwt[:, 0:1], a3[:, :, 1:], ALU.mult, ALU.add, e="dve")
            p.stt(a3[:, :, :w_ - 1], u3[:, :, 1:], cwt[:, 2:3], a3[:, :, :w_ - 1], ALU.mult, ALU.add, e="dve")
        p.tt(odt[b], ACC, SB_, ALU.mult, e="dve")
        p.dma("sp", od[b], odt[b])
    p.finish([ob, od])
    return p


def fnet_tables():
    key = "fnet_tables"
    if key in _CACHE:
        return _CACHE[key]
    import ml_dtypes
    bf = ml_dtypes.bfloat16
    out = {}
    c = np.arange(128)
    ang = 2 * np.pi * np.outer(c, c) / 128.0
    for nm, T in (("cs_l", 4096), ("cs_c", 256)):
        s = 1.0 / np.sqrt(T * 128.0)
        out[nm] = np.concatenate([np.cos(ang) * s, np.sin(ang) * s], 1).astype(np.float32)
    t = np.arange(4096)
    m = (np.outer(t, t) % 4096).astype(np.int64)
    base = 2 * np.pi * np.arange(4096) / 4096.0
    cosv = np.cos(base).astype(np.float32); sinv = (-np.sin(base)).astype(np.float32)
    C = cosv[m].astype(bf); S = sinv[m].astype(bf)
    def lay(M):
        return M.reshape(4, 8, 128, 8, 512).transpose(3, 0, 2, 1, 4)
    out["tab"] = np.ascontiguousarray(np.stack([lay(C), lay(S)], axis=3))
    tc = np.arange(256)
    mc = (np.outer(tc, tc) % 256)
    bc = 2 * np.pi * np.arange(256) / 256.0
    Cc = np.cos(bc)[mc].astype(bf); Sc = (-np.sin(bc))[mc].astype(bf)
    def layc(M):
        return M.reshape(2, 128, 256).transpose(1, 0, 2)
    out["tabc"] = np.ascontiguousarray(np.stack([layc(Cc), layc(Sc)], axis=1))
    _CACHE[key] = out
    return out


def const_tile(p, val, parts=128):
    key = "_c_%g" % val
    if not hasattr(p, key):
        t = p.sb("cst%d" % len([k for k in p.__dict__ if k.startswith("_c_")]), [128, 1])
        p.memset(t, val)
        setattr(p, key, t)
    return getattr(p, key)


CH_FWD = list(range(NCH))
CH_BWD = [3, 2, 1, 0] + list(range(NCH - 1, 3, -1))


def hillis(p, A, B, nparts, bwd, C=64):
    src, dst = A, B
    j = 1
    while j < C:
        s3 = src.rearrange("p (n c) -> p n c", c=C); d3 = dst.rearrange("p (n c) -> p n c", c=C)
        if not bwd:
            p.tt(d3[:nparts, :, j:], s3[:nparts, :, j:], s3[:nparts, :, :C - j], ALU.add, e="dve")
            p.copy(d3[:nparts, :, :j], s3[:nparts, :, :j], e="act")
        else:
            p.tt(d3[:nparts, :, :C - j], s3[:nparts, :, :C - j], s3[:nparts, :, j:], ALU.add, e="dve")
            p.copy(d3[:nparts, :, C - j:], s3[:nparts, :, C - j:], e="act")
        src, dst = dst, src
        j *= 2
    return src


def build_gla():
    p = P()
    qT = p.dram("qT", [2, 64, TB], F32, "ExternalInput")
    kT = p.dram("kT", [2, 64, TB], F32, "ExternalInput")
    vtok = p.dram("vtok", [2, 64, NCH, 128], F32, "ExternalInput")
    ggtok = p.dram("ggtok", [2, 64, NCH * 128], F32, "ExternalInput")
    lrT = p.dram("lrT", [2, 2, 16, TB], F32, "ExternalInput")
    up = p.dram("up", [2, 16, 64], F32, "ExternalInput")
    gb = p.dram("gb", [64, 2], F32, "ExternalInput")
    nwr = p.dram("nwr", [64, 128], F32, "ExternalInput")
    msk = p.dram("msk", [2, 64, 64], F32, "ExternalInput")
    idn = p.dram("idn", [64, 64], F32, "ExternalInput")
    oa = p.dram("oa", [2, 64, NCH * 128], BF16, "ExternalOutput")

    WAB = p.sb("WAB", [64, 2 * TB]); LA = WAB[:, :TB]; LB = WAB[:, TB:]
    Q_ = p.sb("Q_", [64, TB]); K_ = p.sb("K_", [64, TB]); KT = p.sb("KT", [64, TB]); AT = p.sb("AT", [64, TB])
    V = p.sb("V", [64, NCH, 128]); O = p.sb("O", [64, NCH, 128])
    upt = p.sb("upt", [16, 2, 64]); gbt = p.sb("gbt", [64, 2]); nwt = p.sb("nwt", [64, 128])
    mk = p.sb("mk", [64, 2, 64]); idt = p.sb("idt", [64, 64])
    tot = p.sb("tot", [64, NCH]); dT = p.sb("dT", [64, NCH])
    S = [p.sb("S%d" % i, [64, 128]) for i in range(3)]
    ss = p.sb("ssq", [64, NCH]); rst = p.sb("rst", [64, NCH])
    one = const_tile(p, 1.0); eps = const_tile(p, 1e-6)
    for d in range(2):
        p.dma("sp", upt[:, d, :], up[d]); p.dma("sp", mk[:, d, :], msk[d])
    p.dma("sp", gbt, gb); p.dma("sp", nwt, nwr); p.dma("sp", idt, idn)
    p.ts(gbt, gbt, -1.0, ALU.mult)
    psA = [p.ps("ga%d" % i, [64, 512]) for i in range(2)]
    psT = [p.ps("gt%d" % i, [64, 512]) for i in range(2)]
    psO = [p.ps("go%d" % i, [64, 512]) for i in range(2)]
    psI = [p.ps("gi%d" % i, [64, 128]) for i in range(2)]
    LR = AT[:16, :]
    tiles = [(i * 512, 512) for i in range(8)] + [(4096, 256)]
    for b in range(2):
        p.dma("sp", V, vtok[b])
        for d in range(2):
            bwd = d == 1
            p.dma("sp", LR, lrT[b, d])
            for i, (t0, tn) in enumerate(tiles):
                ps = psA[i % 2]
                p.mm(ps[:, :tn], upt[:, d, :], LR[:, t0:t0 + tn])
                p.act(LA[:, t0:t0 + tn], ps[:, :tn], AF.Exp, scale=-1.0, bias=gbt[:, d:d + 1])
            p.act(LA, LA, AF.Ln, bias=one[:64])
            cum = hillis(p, LA, LB, 64, bwd)
            assert cum is LA
            c3 = LA.rearrange("p (n c) -> p n c", c=64)
            p.copy(tot, c3[:, :, 0] if bwd else c3[:, :, 63], e="dve")
            p.act(dT, tot, AF.Exp, scale=-1.0 / 16)
            p.act(AT, LA, AF.Exp, scale=-1.0 / 16)
            p.dma("sp", Q_, qT[b])
            p.stt(Q_, Q_, 0.125, AT, ALU.mult, ALU.mult, e="dve")
            p.act(AT, LA, AF.Exp, scale=1.0 / 16)
            p.dma("sp", K_, kT[b])
            p.tt(K_, K_, AT, ALU.mult, e="dve")
            b3 = LB.rearrange("p (n c) -> p n c", c=64)
            p.tt(b3, c3, tot.unsqueeze(2).to_broadcast([64, NCH, 64]), ALU.subtract, e="dve")
            p.act(LB, LB, AF.Exp, scale=1.0 / 16)
            p.dma("sp", AT, kT[b])
            p.tt(LB, LB, AT, ALU.mult, e="dve")
            kt3 = KT.rearrange("p (n c) -> p n c", c=64); at3 = AT.rearrange("p (n c) -> p n c", c=64)
            for g in range(9):
                ng = min(8, NCH - g * 8)
                pt = psT[g % 2]; pa = psA[g % 2]
                for i in range(ng):
                    n = g * 8 + i
                    p.tr(pt[:, i * 64:(i + 1) * 64], LB[:, n * 64:(n + 1) * 64], idt)
                p.copy(kt3[:, g * 8:g * 8 + ng, :], pt[:, :ng * 64].rearrange("p (n c) -> p n c", c=64), e="act")
            for g in range(9):
                ng = min(8, NCH - g * 8)
                pa = psA[g % 2]
                for i in range(ng):
                    n = g * 8 + i
                    p.mm(pa[:, i * 64:(i + 1) * 64], K_[:, n * 64:(n + 1) * 64], Q_[:, n * 64:(n + 1) * 64])
                p.tt(at3[:, g * 8:g * 8 + ng, :], pa[:, :ng * 64].rearrange("p (n c) -> p n c", c=64),
                     mk[:, d:d + 1, :].to_broadcast([64, ng, 64]), ALU.mult, e="dve")
            order = CH_BWD if bwd else CH_FWD
            p.memset(S[0], 0.0)
            si = 0
            for step, n in enumerate(order):
                grp = n // 4
                po = psO[grp % 2]
                sl = po[:, (n % 4) * 128:(n % 4 + 1) * 128]
                p.mm(sl, at3[:, n, :], V[:, n, :], start=True, stop=False)
                p.mm(sl, Q_[:, n * 64:(n + 1) * 64], S[si], start=False, stop=True)
                pi = psI[step % 2]
                p.mm(pi, kt3[:, n, :], V[:, n, :])
                sn = (si + 1) % 3
                p.stt(S[sn], S[si], dT[:, n:n + 1], pi, ALU.mult, ALU.add, e="dve")
                si = sn
                last_in_grp = (n % 4 == 0) if bwd else (n % 4 == 3)
                if last_in_grp:
                    og = O[:, grp * 4:grp * 4 + 4, :]
                    pv = po.rearrange("p (n v) -> p n v", v=128)
                    if not bwd:
                        p.copy(og, pv, e="act")
                    else:
                        p.tt(og, og, pv, ALU.add, e="pool" if False else "dve")
        GG = WAB.rearrange("p (n v) -> p n v", v=128)
        p.dma("sp", WAB, ggtok[b])
        sq = Q_.rearrange("p (n c) -> p n c", c=64)
        SQ = p_view_sq = None
        h = NCH // 2
        for half, buf in ((0, Q_), (1, K_)):
            o_h = O[:, half * h:(half + 1) * h, :]
            b_h = buf.rearrange("p (n v) -> p n v", v=128)
            p.tt(b_h, o_h, o_h, ALU.mult, e="dve")
            p.reduce(ss[:, half * h:(half + 1) * h], b_h, op=ALU.add, axis=AX.X, e="dve")
        p.act(rst, ss, AF.Ln, scale=1.0 / 128, bias=eps[:64])
        p.act(rst, rst, AF.Exp, scale=-0.5)
        p.act(WAB, WAB, AF.Silu)
        p.tt(O, O, rst.unsqueeze(2).to_broadcast([64, NCH, 128]), ALU.mult, e="dve")
        p.tt(O, O, nwt.unsqueeze(1).to_broadcast([64, NCH, 128]), ALU.mult, e="dve")
        OUT = KT.bitcast(BF16).rearrange("p (n v) -> p n v", v=128)
        p.tt(OUT, O, GG, ALU.mult, e="dve")
        p.dma("sp", oa[b], KT.bitcast(BF16))
    p.finish([oa])
    return p


def build_gdn_pre():
    p = P()
    raw = p.dram("raw", [2, 3, 64, NCH, 128], F32, "ExternalInput")
    rawc = p.dram("rawc", [2, 3, 256, 128], F32, "ExternalInput")
    cwr = p.dram("cwr", [3, 64, 3, 128], F32, "ExternalInput")
    o = p.dram("o", [2, 3, 64, NCH * 128], F32, "ExternalOutput")
    RAW = [p.sb("RAW%d" % i, [64, NCH, 128]) for i in range(2)]
    ACC = [p.sb("ACC%d" % i, [64, NCH, 128]) for i in range(2)]
    TMP = p.sb("TMP", [64, NCH, 128])
    M1 = p.sb("M1", [64, 4, 128]); P1 = p.sb("P1", [64, 4, 128]); TC = p.sb("TC", [64, 4, 128])
    cw = p.sb("cw", [64, 3, 3, 128])
    ss = p.sb("ss", [64, NCH]); rn = p.sb("rn", [64, NCH])
    eps = const_tile(p, 1e-6)
    for c in range(3):
        p.dma("sp", cw[:, c], cwr[c])
    k = 0
    for b in range(2):
        for c in range(3):
            R_ = RAW[k % 2]; A_ = ACC[k % 2]; k += 1
            p.dma("sp", R_, raw[b, c])
            p.memset(M1, 0.0, e="pool"); p.memset(P1, 0.0, e="pool")
            src = rawc[b, c]
            p.dma("sp", M1[1:64, 0, :], src[0:63, :])
            p.dma("sp", M1[:, 1:4, :], src[63:255, :].rearrange("(n s) c -> s n c", s=64))
            p.dma("sp", P1[:, 0:3, :], src[1:193, :].rearrange("(n s) c -> s n c", s=64))
            p.dma("sp", P1[0:63, 3, :], src[193:256, :])
            w0 = cw[:, c, 0:1, :]; w1 = cw[:, c, 1:2, :]; w2 = cw[:, c, 2:3, :]
            p.tt(A_, R_, w1.to_broadcast([64, NCH, 128]), ALU.mult, e="dve")
            p.tt(TMP[:, 5:68], R_[:, 4:67], w0.to_broadcast([64, 63, 128]), ALU.mult, e="dve")
            p.tt(A_[:, 5:68], A_[:, 5:68], TMP[:, 5:68], ALU.add, e="dve")
            p.tt(TMP[:, 4:67], R_[:, 5:68], w2.to_broadcast([64, 63, 128]), ALU.mult, e="dve")
            p.tt(A_[:, 4:67], A_[:, 4:67], TMP[:, 4:67], ALU.add, e="dve")
            p.tt(TC, M1, w0.to_broadcast([64, 4, 128]), ALU.mult, e="dve")
            p.tt(A_[:, 0:4], A_[:, 0:4], TC, ALU.add, e="dve")
            p.tt(TC, P1, w2.to_broadcast([64, 4, 128]), ALU.mult, e="dve")
            p.tt(A_[:, 0:4], A_[:, 0:4], TC, ALU.add, e="dve")
            p.act(A_, A_, AF.Silu)
            if c < 2:
                p.tt(TMP, A_, A_, ALU.mult, e="dve")
                p.reduce(ss, TMP, op=ALU.add, axis=AX.X, e="dve")
                p.act(rn, ss, AF.Ln, bias=eps[:64])
                p.act(rn, rn, AF.Exp, scale=-0.5)
                if c == 0:
                    p.ts(rn, rn, float(128 ** -0.5), ALU.mult)
                p.tt(A_, A_, rn.unsqueeze(2).to_broadcast([64, NCH, 128]), ALU.mult, e="dve")
            p.dma("sp", o[b, c], A_.rearrange("p n c -> p (n c)"))
    p.finish([o])
    return p


GROUPS = [(0, 4)] + [(4 + 8 * i, 8) for i in range(8)]


def build_gdn_main():
    p = P()
    qT = p.dram("qT", [2, 128, TB], F32, "ExternalInput")
    kT = p.dram("kT", [2, 128, TB], F32, "ExternalInput")
    ktok = p.dram("ktok", [2, 64, NCH, 128], F32, "ExternalInput")
    vtok = p.dram("vtok", [2, 64, NCH, 128], F32, "ExternalInput")
    atok = p.dram("atok", [2, 2, 64, NCH], F32, "ExternalInput")
    btok = p.dram("btok", [2, 2, 64, NCH], F32, "ExternalInput")
    ztok = p.dram("ztok", [2, 64, NCH * 128], F32, "ExternalInput")
    prm = p.dram("prm", [128, 4], F32, "ExternalInput")
    nwr = p.dram("nwr", [64, 128], F32, "ExternalInput")
    msk = p.dram("msk", [4, 64, 64], F32, "ExternalInput")
    idn = p.dram("idn", [64, 64], F32, "ExternalInput")
    oc = p.dram("oc", [2, 64, NCH * 128], BF16, "ExternalOutput")

    QT = p.sb("QT", [128, TB]); KTs = p.sb("KTs", [128, TB])
    O = p.sb("O", [64, NCH, 128]); Z = p.sb("Z", [64, NCH * 128]); SQ = p.sb("SQ", [64, TB])
    prt = p.sb("prt", [128, 4]); nwt = p.sb("nwt", [64, 128]); mk = p.sb("mk", [64, 4, 64]); idt = p.sb("idt", [64, 64])
    ones = p.sb("ones", [64, 128])
    one = const_tile(p, 1.0); eps = const_tile(p, 1e-6)
    p.dma("sp", prt, prm); p.dma("sp", nwt, nwr); p.dma("sp", idt, idn)
    for i in range(4):
        p.dma("sp", mk[:, i, :], msk[i])
    p.memset(ones, 1.0)
    expA = p.sb("expA", [128, 2])
    p.act(expA, prt[:, 2:4], AF.Exp)
    At = p.sb("At", [64, NCH]); Bt = p.sb("Bt", [64, NCH]); L = p.sb("L", [64, NCH])
    beta = p.sb("beta", [64, NCH]); nbeta = p.sb("nbeta", [64, NCH]); eg = p.sb("eg", [64, NCH])
    tot = p.sb("tot", [64, NCH]); kes = p.sb("kes", [64, NCH]); bw = p.sb("bw", [64, NCH]); dS = p.sb("dS", [128, NCH])
    ss = p.sb("ssq", [64, NCH]); rst = p.sb("rst", [64, NCH])
    def g64(nm):
        return p.sb(nm, [64, 8, 64])
    laU = g64("laU"); laV = g64("laV"); LM = g64("LM"); LMT = g64("LMT"); TMPg = g64("TMPg")
    Xa = g64("Xa"); XTa = g64("XTa"); Xb = g64("Xb"); XTb = g64("XTb"); QKM = g64("QKM")
    R = p.sb("R", [64, 8, 256]); R0 = p.sb("R0", [64, 8, 256]); TT = g64("TT")
    KG = p.sb("KG", [64, 8, 128]); VG = p.sb("VG", [64, 8, 128]); KE = p.sb("KE", [64, 8, 128]); VN = p.sb("VN", [64, 8, 128])
    WT = p.sb("WT", [128, 8, 64]); QG = p.sb("QG", [128, 512]); EGR = p.sb("EGR", [128, 512])
    S = [p.sb("S%d" % i, [128, 128]) for i in range(3)]
    PA = [p.ps("PA%d" % i, [64, 1024]) for i in range(2)]; PB = [p.ps("PB%d" % i, [128, 512]) for i in range(2)]
    PC = p.ps("PC", [128, 512]); PD = p.ps("PD", [64, 512])
    PCv = PC[:64, 0:128]; PCs = PC[:, 128:256]

    def fl(t, ng):
        return t[:, :ng, :].rearrange("p n c -> p (n c)")

    for b in range(2):
        p.dma("sp", QT, qT[b]); p.dma("sp", KTs, kT[b])
        for d in range(2):
            bwd = d == 1
            Um = mk[:, 2 * d, :]; Vm = mk[:, 2 * d + 1, :]
            p.dma("sp", At, atok[b, d]); p.dma("sp", Bt, btok[b, d])
            p.act(L, At, AF.Exp, bias=prt[:64, d:d + 1])
            p.act(L, L, AF.Ln, bias=one[:64])
            p.ts(L, L, expA[:64, d:d + 1], ALU.mult)
            p.act(beta, Bt, AF.Exp, scale=-1.0)
            p.ts(beta, beta, 1.0, ALU.add)
            p.recip(beta, beta)
            p.ts(nbeta, beta, -1.0, ALU.mult)
            p.mm(PB[0][:64, :NCH], Um, L)
            p.mm(PB[1][:, :NCH], ones, L)
            p.act(eg, PB[0][:64, :NCH], AF.Exp, scale=-1.0)
            p.act(dS, PB[1][:, :NCH], AF.Exp, scale=-1.0)
            p.copy(tot, PB[1][:64, :NCH], e="dve")
            p.tt(kes, tot, PB[0][:64, :NCH], ALU.subtract)
            p.act(kes, kes, AF.Exp, scale=-1.0)
            p.tt(bw, beta, eg, ALU.mult)
            p.memset(S[0], 0.0)
            si = 0
            gorder = ([GROUPS[0]] + GROUPS[:0:-1]) if bwd else GROUPS
            for (n0, ng) in gorder:
                t0 = n0 * 64; tn = ng * 64
                p.dma("sp", KG[:, :ng], ktok[b, :, n0:n0 + ng, :]); p.dma("sp", VG[:, :ng], vtok[b, :, n0:n0 + ng, :])
                Lg = L[:, n0:n0 + ng].unsqueeze(2).to_broadcast([64, ng, 64])
                p.tt(laU[:, :ng], Um.unsqueeze(1).to_broadcast([64, ng, 64]), Lg, ALU.mult, e="dve")
                p.tt(laV[:, :ng], Vm.unsqueeze(1).to_broadcast([64, ng, 64]), Lg, ALU.mult, e="dve")
                p.mm(PB[0][:64, :tn], Um, fl(laV, ng))
                p.act(fl(LM, ng), PB[0][:64, :tn], AF.Exp, scale=-1.0)
                p.tt(LM[:, :ng], LM[:, :ng], Vm.unsqueeze(1).to_broadcast([64, ng, 64]), ALU.mult, e="dve")
                p.mm(PB[1][:64, :tn], Vm, fl(laU, ng))
                p.act(fl(LMT, ng), PB[1][:64, :tn], AF.Exp, scale=-1.0)
                p.tt(LMT[:, :ng], LMT[:, :ng], Um.unsqueeze(1).to_broadcast([64, ng, 64]), ALU.mult, e="dve")
                for i in range(ng):
                    ks = KTs[:, t0 + i * 64:t0 + (i + 1) * 64]
                    p.mm(PB[0][:64, i * 64:(i + 1) * 64], ks, ks)
                p.tt(fl(TMPg, ng), PB[0][:64, :tn], fl(LM, ng), ALU.mult, e="dve")
                p.tt(Xa[:, :ng], TMPg[:, :ng], nbeta[:, n0:n0 + ng].unsqueeze(2).to_broadcast([64, ng, 64]), ALU.mult, e="dve")
                for i in range(ng):
                    p.tr(PB[1][:64, i * 64:(i + 1) * 64], Xa[:, i, :], idt)
                p.copy(fl(XTa, ng), PB[1][:64, :tn], e="act")
                p.tt(R0[:, :ng, 0:128], VG[:, :ng], beta[:, n0:n0 + ng].unsqueeze(2).to_broadcast([64, ng, 128]), ALU.mult, e="dve")
                p.tt(R0[:, :ng, 128:256], KG[:, :ng], bw[:, n0:n0 + ng].unsqueeze(2).to_broadcast([64, ng, 128]), ALU.mult, e="dve")
                p.tt(KE[:, :ng], KG[:, :ng], kes[:, n0:n0 + ng].unsqueeze(2).to_broadcast([64, ng, 128]), ALU.mult, e="dve")
                p.tt(TT[:, :ng], XTa[:, :ng], idt.unsqueeze(1).to_broadcast([64, ng, 64]), ALU.add, e="dve")
                X, XT, Xn, XTn = Xa, XTa, Xb, XTb
                for lev in range(1, 6):
                    for i in range(ng):
                        p.mm(PB[0][:64, i * 64:(i + 1) * 64], XT[:, i, :], X[:, i, :])
                    if lev < 5:
                        for i in range(ng):
                            p.mm(PB[1][:64, i * 64:(i + 1) * 64], X[:, i, :], XT[:, i, :])
                    p.copy(fl(Xn, ng), PB[0][:64, :tn], e="act")
                    if lev < 5:
                        p.copy(fl(XTn, ng), PB[1][:64, :tn], e="act")
                    X, XT, Xn, XTn = Xn, XTn, X, XT
                    for i in range(ng):
                        p.mm(PA[0][:, i * 64:(i + 1) * 64], X[:, i, :], TT[:, i, :])
                    p.tt(fl(TT, ng), fl(TT, ng), PA[0][:, :tn], ALU.add, e="dve")
                for hf in range((ng + 3) // 4):
                    for i in range(4):
                        p.mm(PA[hf][:, i * 256:(i + 1) * 256], TT[:, hf * 4 + i, :], R0[:, hf * 4 + i, :])
                    p.copy(R[:, hf * 4:hf * 4 + 4, :].rearrange("p n c -> p (n c)"), PA[hf], e=("dve", "act")[hf])
                for i in range(ng):
                    p.tr(PB[0][:, i * 64:(i + 1) * 64], R[:, i, 128:256], idt)
                p.copy(fl(WT, ng), PB[0][:, :tn], e="act")
                for i in range(ng):
                    sl_ = slice(t0 + i * 64, t0 + (i + 1) * 64)
                    p.mm(PB[1][:64, i * 64:(i + 1) * 64], KTs[:, sl_], QT[:, sl_])
                p.tt(fl(QKM, ng), PB[1][:64, :tn], fl(LMT, ng), ALU.mult, e="dve")
                p.mm(PB[0][:, :tn], ones, fl(laU, ng))
                p.act(EGR[:, :tn], PB[0][:, :tn], AF.Exp, scale=-1.0)
                p.tt(QG[:, :tn], QT[:, t0:t0 + tn], EGR[:, :tn], ALU.mult, e="dve")
                corder = range(ng - 1, -1, -1) if bwd else range(ng)
                for i in corder:
                    n = n0 + i
                    p.mm(PCv, WT[:, i, :], S[si])
                    p.tt(VN[:, i, :], R[:, i, 0:128], PCv, ALU.subtract, e="dve")
                    slot = n % 4
                    osl = PD[:, slot * 128:(slot + 1) * 128]
                    p.mm(osl, QG[:, i * 64:(i + 1) * 64], S[si], start=True, stop=False)
                    p.mm(osl, QKM[:, i, :], VN[:, i, :], start=False, stop=True)
                    p.mm(PCs, KE[:, i, :], VN[:, i, :])
                    sn = (si + 1) % 3
                    p.stt(S[sn], S[si], dS[:, n:n + 1], PCs, ALU.mult, ALU.add, e="dve")
                    si = sn
                    if (slot == 0) if bwd else (slot == 3):
                        grp = n // 4
                        og = O[:, grp * 4:grp * 4 + 4, :]
                        pv = PD.rearrange("p (n v) -> p n v", v=128)
                        if not bwd:
                            p.copy(og, pv, e="act")
                        else:
                            p.tt(og, og, pv, ALU.add, e="pool" if False else "dve")
        p.dma("sp", Z, ztok[b])
        h = NCH // 2
        for half in range(2):
            o_h = O[:, half * h:(half + 1) * h, :]
            b_h = SQ.rearrange("p (n v) -> p n v", v=128)
            p.tt(b_h, o_h, o_h, ALU.mult, e="dve")
            p.reduce(ss[:, half * h:(half + 1) * h], b_h, op=ALU.add, axis=AX.X, e="dve")
        p.act(rst, ss, AF.Ln, scale=1.0 / 128, bias=eps[:64])
        p.act(rst, rst, AF.Exp, scale=-0.5)
        p.act(Z, Z, AF.Silu)
        p.tt(O, O, rst.unsqueeze(2).to_broadcast([64, NCH, 128]), ALU.mult, e="dve")
        p.tt(O, O, nwt.unsqueeze(1).to_broadcast([64, NCH, 128]), ALU.mult, e="dve")
        OUT = SQ.bitcast(BF16).rearrange("p (n v) -> p n v", v=128)
        p.tt(OUT, O, Z.rearrange("p (n v) -> p n v", v=128), ALU.mult, e="dve")
        p.dma("sp", oc[b], SQ.bitcast(BF16))
    p.finish([oc])
    return p


def build_wout():
    p = P()
    NTK = 1088
    mixT = p.dram("mixT", [128, 32, NTK], BF16, "ExternalInput")
    w = p.dram("w", [128, 32, D], F32, "ExternalInput")
    xl = p.dram("xl", [1024, D], F32, "ExternalInput"); xc = p.dram("xc", [64, D], F32, "ExternalInput")
    g_l = p.dram("g_l", [128, D], F32, "ExternalInput"); g_c = p.dram("g_c", [128, D], F32, "ExternalInput")
    ol = p.dram("ol", [1024, D], F32, "ExternalOutput"); oc = p.dram("oc", [64, D], F32, "ExternalOutput")
    MT = p.sb("MT", [128, 32, NTK], BF16)
    for q in range(4):
        p.dma("sp", (MT[:, q * 8:(q + 1) * 8, :], q), mixT[:, q * 8:(q + 1) * 8, :])
    WB = [p.sb("WB%d" % i, [128, 32, 512], BF16) for i in range(2)]
    stg = [p.sb("stg%d" % i, [128, 8, 512]) for i in range(2)]
    gl = [p.sb("gl%d" % i, [128, 512]) for i in range(2)]; gc = [p.sb("gc%d" % i, [128, 512]) for i in range(2)]
    xt = [p.sb("xt%d" % i, [128, 512]) for i in range(3)]; tm = [p.sb("tm%d" % i, [128, 512]) for i in range(2)]
    ot = [p.sb("ot%d" % i, [128, 512]) for i in range(3)]
    pst = [p.ps("pw%d" % i, [128, 512]) for i in range(4)]
    k = 0; u = 0
    for j in range(8):
        cs = slice(j * 512, (j + 1) * 512)
        Wb = WB[j % 2]
        for q in range(4):
            s_ = stg[k % 2]; k += 1
            p.dma("sp", s_, w[:, q * 8:(q + 1) * 8, cs])
            e = ("dve", "act")[q % 2]
            p.copy((Wb[:, q * 8:(q + 1) * 8, :], q), s_, e=e)
        GL = gl[j % 2]; GC = gc[j % 2]
        p.dma("sp", GL, g_l[:, cs]); p.dma("sp", GC, g_c[:, cs])
        for t in range(9):
            n = 128 if t < 8 else 64
            X = xt[u % 3]; O_ = ot[u % 3]; T_ = tm[u % 2]; ps = pst[u % 4]; u += 1
            src = xl[t * 128:(t + 1) * 128, cs] if t < 8 else xc[:, cs]
            dst = ol[t * 128:(t + 1) * 128, cs] if t < 8 else oc[:, cs]
            p.dma("sp", X[:n], src)
            for kc in range(32):
                p.mm(ps[:n], (MT[:, kc, t * 128:t * 128 + n], kc // 8), (Wb[:, kc, :], kc // 8), start=(kc == 0), stop=(kc == 31))
            p.tt(T_[:n], ps[:n], (GL if t < 8 else GC)[:n], ALU.mult, e="dve")
            p.tt(O_[:n], T_[:n], X[:n], ALU.add, e="dve")
            p.dma("sp", dst, O_[:n])
    p.finish([ol, oc])
    return p


def _idma(p, out, in_dram, idx_ap, bounds=None):
    q = "pool"
    i = p.dnext
    p.dnext = (p.dnext + 1) % len(p.dsem)
    if p.dval[i] > 0:
        p._wait(q, ("d", i, p.dval[i]))
    reads = [in_dram, idx_ap]; writes = [out]
    p._deps(q, reads, writes)
    kw = {}
    if bounds is not None:
        rk_ = "_breg_%d" % int(bounds)
        if not hasattr(p, rk_):
            setattr(p, rk_, p.nc.gpsimd.to_reg(int(bounds)))
        kw = {"bounds_check": getattr(p, rk_), "oob_is_err": False}
    ins = p.nc.gpsimd.indirect_dma_start(out=p._a(out), out_offset=None, in_=p._a(in_dram),
                                         in_offset=bass.IndirectOffsetOnAxis(ap=p._a(idx_ap), axis=0), **kw)
    p.dval[i] += 16
    ins.then_inc(p.dsem[i], 16)
    tok = ("d", i, p.dval[i])
    p._commit(tok, reads, writes)
    p.n_ins += 1
    return tok


def build_route():
    p = P()
    arow = p.dram("arow", [4, 128, 4096], F32, "ExternalInput"); acol = p.dram("acol", [4, 128, 32], F32, "ExternalInput")
    arowc = p.dram("arowc", [4, 128, 256], F32, "ExternalInput"); acolc = p.dram("acolc", [4, 128, 2], F32, "ExternalInput")
    iota = p.dram("iota", [128, 512], F32, "ExternalInput"); tv = p.dram("tv", [128, 32], F32, "ExternalInput")
    idx = p.dram("idxr", [4, 1, 512], I32, "ExternalOutput"); gate = p.dram("gater", [4, 1, 512], F32, "ExternalOutput")
    rank = p.dram("rank", [4, 128, 32], F32, "ExternalOutput")
    idxc = p.dram("idxcr", [4, 1, 32], I32, "ExternalOutput"); gatec = p.dram("gatecr", [4, 1, 32], F32, "ExternalOutput")
    rankc = p.dram("rankc", [4, 128, 2], F32, "ExternalOutput")
    io = p.sb("io", [128, 512]); tvt = p.sb("tvt", [128, 32])
    p.dma("sp", io, iota); p.dma("sp", tvt, tv)
    AR = [p.sb("AR%d" % i, [128, 4096]) for i in range(2)]; junk = p.sb("junk", [128, 4096]); junk2 = p.sb("junk2", [128, 4096])
    NAC = p.sb("NAC", [128, 32])
    AC = [p.sb("AC%d" % i, [128, 32]) for i in range(2)]; RK = [p.sb("RK%d" % i, [128, 32]) for i in range(2)]
    TV2 = [p.sb("TV2_%d" % i, [128, 32, 2]) for i in range(2)]
    OH = p.sb("OH", [128, 32, 512])
    PS = [p.ps("pr%d" % i, [2, 512]) for i in range(2)]
    IG = [p.sb("IG%d" % i, [2, 512]) for i in range(2)]; II = [p.sb("II%d" % i, [2, 512], I32) for i in range(2)]
    ARc = [p.sb("ARc%d" % i, [128, 256]) for i in range(2)]; ACc = [p.sb("ACc%d" % i, [128, 2]) for i in range(2)]
    RKc = [p.sb("RKc%d" % i, [128, 2]) for i in range(2)]; TV2c = [p.sb("TV2c%d" % i, [128, 2, 2]) for i in range(2)]
    OHc = p.sb("OHc", [128, 2, 32]); IGc = [p.sb("IGc%d" % i, [2, 32]) for i in range(2)]
    IIc = [p.sb("IIc%d" % i, [2, 32], I32) for i in range(2)]
    for q in range(4):
        A = AR[q % 2]; C = AC[q % 2]; R_ = RK[q % 2]; T2 = TV2[q % 2]; ps = PS[q % 2]
        p.dma("sp", A, arow[q]); p.dma("sp", C, acol[q])
        p.memset(R_, 0.0, e="pool")
        p.ts(NAC, C, -1.0, ALU.mult, e="dve")
        for tc in range(32):
            if tc % 2 == 0:
                p.ts(junk, A, C[:, tc:tc + 1], ALU.is_gt, 0.0, ALU.add, accum_out=R_[:, tc:tc + 1], e="dve")
            else:
                p.act(junk2, A, AF.Sign, bias=NAC[:, tc:tc + 1], accum_out=R_[:, tc:tc + 1])
        rodd = R_.rearrange("p (a two) -> p a two", two=2)[:, :, 1]
        p.ts(rodd, rodd, 0.5, ALU.mult, 2047.5, ALU.add, e="dve")
        p.copy(T2[:, :, 0], tvt, e="dve"); p.copy(T2[:, :, 1], C, e="dve")
        for tc in range(32):
            p.ts(OH[:, tc, :], io, R_[:, tc:tc + 1], ALU.is_equal, e="dve")
        for tc in range(32):
            p.mm(ps, T2[:, tc, :], OH[:, tc, :], start=(tc == 0), stop=(tc == 31))
        p.copy(IG[q % 2], ps, e="dve")
        p.copy(II[q % 2], IG[q % 2], e="dve")
        p.dma("sp", idx[q], II[q % 2][0:1, :]); p.dma("sp", gate[q], IG[q % 2][1:2, :]); p.dma("sp", rank[q], R_)
        A = ARc[q % 2]; C = ACc[q % 2]; R_ = RKc[q % 2]; T2 = TV2c[q % 2]
        p.dma("sp", A, arowc[q]); p.dma("sp", C, acolc[q])
        p.memset(R_, 0.0, e="pool")
        for tc in range(2):
            p.ts(junk[:, :256], A, C[:, tc:tc + 1], ALU.is_gt, 0.0, ALU.add, accum_out=R_[:, tc:tc + 1], e="dve")
        p.copy(T2[:, :, 0], tvt[:, 0:2], e="dve"); p.copy(T2[:, :, 1], C, e="dve")
        for tc in range(2):
            p.ts(OHc[:, tc, :], io[:, :32], R_[:, tc:tc + 1], ALU.is_equal, e="dve")
        for tc in range(2):
            p.mm(ps[:, :32], T2[:, tc, :], OHc[:, tc, :], start=(tc == 0), stop=(tc == 1))
        p.copy(IGc[q % 2], ps[:, :32], e="dve")
        p.copy(IIc[q % 2], IGc[q % 2], e="dve")
        p.dma("sp", idxc[q], IIc[q % 2][0:1, :]); p.dma("sp", gatec[q], IGc[q % 2][1:2, :]); p.dma("sp", rankc[q], R_)
    p.finish([idx, gate, rank, idxc, gatec, rankc])
    return p


DFF = 1024


def build_experts():
    p = P()
    idx = p.dram("idx", [4, 128, 4], I32, "ExternalInput"); gate = p.dram("gate", [4, 128, 4], F32, "ExternalInput")
    idxc = p.dram("idxc", [4, 32, 1], I32, "ExternalInput"); gatec = p.dram("gatec", [4, 32, 1], F32, "ExternalInput")
    h2 = [p.dram("h2_%d" % b, [4096, D], BF16, "ExternalInput") for b in range(2)]
    h2c = [p.dram("h2c_%d" % b, [256, D], BF16, "ExternalInput") for b in range(2)]
    wg = p.dram("wg", [2, 8, 128, 32 * 128], F32, "ExternalInput"); wu = p.dram("wu", [2, 8, 128, 32 * 128], F32, "ExternalInput")
    wd = p.dram("wd", [2, 8, 128, 8 * 512], F32, "ExternalInput")
    idn = p.dram("idn", [128, 128], BF16, "ExternalInput")
    yg = p.dram("yg", [4, 512, D], BF16, "ExternalOutput"); ygc = p.dram("ygc", [4, 32, D], BF16, "ExternalOutput")
    idt = p.sb("idt", [128, 128], BF16); p.dma("sp", idt, idn)
    IDX = p.sb("IDX", [128, 4, 4], I32); G = p.sb("G", [128, 4, 4]); IDXc = p.sb("IDXc", [32, 4], I32); Gc = p.sb("Gc", [32, 4])
    for q in range(4):
        p.dma("sp", IDX[:, q, :], idx[q]); p.dma("sp", G[:, q, :], gate[q])
        p.dma("sp", IDXc[:, q:q + 1], idxc[q]); p.dma("sp", Gc[:, q:q + 1], gatec[q])
    XG = p.sb("XG", [128, 4, D], BF16); XGc = p.sb("XGc", [32, D], BF16)
    XT = [p.sb("XT%d" % b, [128, 32, 512], BF16) for b in range(2)]; XTc = p.sb("XTc", [128, 32, 64], BF16)
    HT = [p.sb("HT%d" % b, [128, 8, 512], BF16) for b in range(2)]; HTc = p.sb("HTc", [128, 8, 64], BF16)
    stg = [p.sb("stg%d" % i, [128, 4096]) for i in range(2)]
    wb = [p.sb("wb%d" % i, [128, 4096], BF16) for i in range(4)]
    HS = [p.sb("HS%d" % i, [128, 512]) for i in range(2)]
    Y = [p.sb("Y%d" % i, [128, 512], BF16) for i in range(3)]
    PT = [p.ps("pt%d" % i, [128, 1024], BF16) for i in range(2)]
    PG = [p.ps("pg%d" % i, [128, 512]) for i in range(2)]; PU = [p.ps("pu%d" % i, [128, 512]) for i in range(2)]
    PY = [p.ps("py%d" % i, [128, 512]) for i in range(2)]
    sk = [0]; wk = [0]

    def load_w(src):
        s_ = stg[sk[0] % 2]; sk[0] += 1
        W = wb[wk[0] % 4]; wk[0] += 1
        p.dma("sp", s_, src)
        p.copy(W[:, :2048], s_[:, :2048], e="dve"); p.copy(W[:, 2048:], s_[:, 2048:], e="pool" if False else "act")
        return W

    for e in range(2):
        for b in range(2):
            q = e * 2 + b
            for rs in range(4):
                _idma(p, XG[:, rs, :], h2[b], IDX[:, q, rs:rs + 1])
            k = 0
            for rs in range(4):
                for g8 in range(4):
                    pt = PT[k % 2]; k += 1
                    for i in range(8):
                        kc = g8 * 8 + i
                        p.tr(pt[:, i * 128:(i + 1) * 128], XG[:, rs, kc * 128:(kc + 1) * 128], idt)
                    p.copy(XT[b][:, g8 * 8:(g8 + 1) * 8, rs * 128:(rs + 1) * 128], pt.rearrange("p (a r) -> p a r", a=8),
                           e=("dve", "act")[k % 2])
            _idma(p, XGc, h2c[b], IDXc[:, q:q + 1])
            for g8 in range(4):
                pt = PT[k % 2]; k += 1
                for i in range(8):
                    kc = g8 * 8 + i
                    p.tr(pt[:, i * 32:(i + 1) * 32], XGc[:, kc * 128:(kc + 1) * 128], idt[:32, :32])
                p.copy(XTc[:, g8 * 8:(g8 + 1) * 8, b * 32:(b + 1) * 32], pt[:, :256].rearrange("p (a r) -> p a r", a=8), e="dve")
        u = 0
        for ft in range(8):
            WG = load_w(wg[e, ft]).rearrange("p (k f) -> p k f", f=128)
            WU = load_w(wu[e, ft]).rearrange("p (k f) -> p k f", f=128)
            for b in range(3):
                rhs = XT[b] if b < 2 else XTc
                n = 512 if b < 2 else 64
                pg = PG[u % 2]; pu = PU[u % 2]; hs = HS[u % 2]; u += 1
                for kc in range(32):
                    p.mm(pg[:, :n], WG[:, kc, :], rhs[:, kc, :], start=(kc == 0), stop=(kc == 31))
                for kc in range(32):
                    p.mm(pu[:, :n], WU[:, kc, :], rhs[:, kc, :], start=(kc == 0), stop=(kc == 31))
                p.act(hs[:, :n], pg[:, :n], AF.Silu)
                dst = HT[b][:, ft, :] if b < 2 else HTc[:, ft, :]
                p.tt(dst, hs[:, :n], pu[:, :n], ALU.mult, e="dve")
        v = 0
        for dt in range(8):
            WD = load_w(wd[e, dt]).rearrange("p (k d) -> p k d", d=512)
            for b in range(2):
                q = e * 2 + b
                for rs in range(4):
                    py = PY[v % 2]; y = Y[v % 3]; v += 1
                    for fc in range(8):
                        p.mm(py, HT[b][:, fc, rs * 128:(rs + 1) * 128], WD[:, fc, :], start=(fc == 0), stop=(fc == 7))
                    p.ts(y, py, G[:, q, rs:rs + 1], ALU.mult, e=("dve", "act")[0])
                    p.dma("sp", yg[q, rs * 128:(rs + 1) * 128, dt * 512:(dt + 1) * 512], y)
                py = PY[v % 2]; y = Y[v % 3]; v += 1
                for fc in range(8):
                    p.mm(py[:32], HTc[:, fc, b * 32:(b + 1) * 32], WD[:, fc, :], start=(fc == 0), stop=(fc == 7))
                p.ts(y[:32], py[:32], Gc[:, q:q + 1], ALU.mult, e="dve")
                p.dma("sp", ygc[q, :, dt * 512:(dt + 1) * 512], y[:32])
    p.finish([yg, ygc])
    return p


def build_combine():
    p = P()
    xl = p.dram("xl", [1024, D], F32, "ExternalInput"); xc = p.dram("xc", [64, D], F32, "ExternalInput")
    g_l = p.dram("g_l", [128, D], F32, "ExternalInput"); g_c = p.dram("g_c", [128, D], F32, "ExternalInput")
    rk = p.dram("rk", [128, 9, 16], F32, "ExternalInput")
    eo = p.dram("eo", [128, 2, 16], F32, "ExternalInput")
    yall = p.dram("yall", [16 * 512, D], BF16, "ExternalInput")
    ycall = p.dram("ycall", [16 * 32, D], BF16, "ExternalInput")
    idn = p.dram("idn", [128, 128], BF16, "ExternalInput")
    ol = p.dram("ol", [1024, D], F32, "ExternalOutput"); oc = p.dram("oc", [64, D], F32, "ExternalOutput")
    BIG = 1.0e6
    GL = p.sb("GL", [128, D]); GC = p.sb("GC", [128, D]); idt = p.sb("idt", [128, 128], BF16)
    p.dma("sp", GL, g_l); p.dma("sp", GC, g_c); p.dma("sp", idt, idn)
    RK = p.sb("RK", [128, 9, 16]); EO = p.sb("EO", [128, 2, 16]); SEL = p.sb("SEL", [128, 9, 16]); IDF = p.sb("IDF", [128, 9, 16])
    IDI = p.sb("IDI", [128, 9, 16], I32)
    p.dma("sp", RK, rk); p.dma("sp", EO, eo)
    for (t0, t1, cap, ty) in ((0, 8, 512, 0), (8, 9, 32, 1)):
        r = RK[:, t0:t1, :]
        p.ts(SEL[:, t0:t1, :], r, float(cap), ALU.is_lt)
        p.tt(IDF[:, t0:t1, :], r, EO[:, ty:ty + 1, :].to_broadcast([128, t1 - t0, 16]), ALU.add)
        p.tt(IDF[:, t0:t1, :], IDF[:, t0:t1, :], SEL[:, t0:t1, :], ALU.mult)
        p.ts(IDF[:, t0:t1, :], IDF[:, t0:t1, :], BIG, ALU.add)
    p.copy(IDI, IDF)
    NG = 6
    GB = [p.sb("GB%d" % i, [128, D], BF16) for i in range(NG)]
    X = [p.sb("X%d" % i, [128, D]) for i in range(2)]
    TM = [p.sb("TM%d" % i, [128, 512]) for i in range(2)]
    PS = [p.ps("pc%d" % i, [128, 512]) for i in range(8)]
    k = 0; u = 0
    for t in range(9):
        n = 128 if t < 8 else 64
        src = xl[t * 128:(t + 1) * 128, :] if t < 8 else xc
        dst = ol[t * 128:(t + 1) * 128, :] if t < 8 else oc
        ysrc = yall if t < 8 else ycall
        nrows = 16 * 512 if t < 8 else 16 * 32
        Xt = X[t % 2]
        p.dma("sp", Xt[:n], src)
        for e in range(16):
            gb = GB[k % NG]; k += 1
            p.memset(gb[:n], 0.0, e="dve")
            _idma(p, gb[:n], ysrc, IDI[:n, t, e:e + 1], bounds=nrows - 1)
            for ct in range(8):
                p.mm(PS[ct][:n], idt[:n, :n], gb[:n, ct * 512:(ct + 1) * 512], start=(e == 0), stop=(e == 15))
        G_ = GL if t < 8 else GC
        for ct in range(8):
            cs = slice(ct * 512, (ct + 1) * 512)
            tm = TM[u % 2]; u += 1
            p.tt(tm[:n], PS[ct][:n], G_[:n, cs], ALU.mult, e="dve")
            p.tt(Xt[:n, cs], Xt[:n, cs], tm[:n], ALU.add, e="dve")
        p.dma("sp", dst, Xt[:n])
    p.finish([ol, oc])
    return p


def _prog(name, fn):
    if name not in _CACHE:
        _CACHE[name] = fn()
    return _CACHE[name]


def _rep(v, n=128):
    return np.ascontiguousarray(np.broadcast_to(np.asarray(v)[None], (n,) + tuple(np.asarray(v).shape)))


def _fm(a):
    return np.ascontiguousarray(a.reshape(2, TB, -1).transpose(0, 2, 1))


def _tokc(a):
    return np.ascontiguousarray(a.reshape(2, NCH, 64, -1).transpose(0, 2, 1, 3))


_U64 = np.triu(np.ones((64, 64), np.float32))
_V64 = np.tril(np.ones((64, 64), np.float32), -1)


def _run_norm(xl, xc, nw, mod_l, sc_i, sh_i, out_bf16=True, modulate=True, router_w=None):
    name = "norm_%d%d%d" % (out_bf16, modulate, router_w is not None)
    p = _prog(name, lambda: build_norm(out_bf16, modulate, router_w is not None))
    nwr = _rep(nw)
    ims = []
    extra = {}
    if router_w is not None:
        extra = {"wr": np.ascontiguousarray(router_w.reshape(32, 128, 16).transpose(1, 0, 2)), "idn": np.eye(128, dtype=np.float32)}
    reps = {}
    if modulate:
        for m in range(3):
            reps[m] = (_rep(mod_l[m, sc_i * D:(sc_i + 1) * D]), _rep(mod_l[m, sh_i * D:(sh_i + 1) * D]))
    for i in range(8):
        b = i // 4
        d = {"xl": xl[i * 1024:(i + 1) * 1024], "xc": xc[i * 64:(i + 1) * 64], "nw": nwr}
        if modulate:
            d.update({"sc_l": reps[b][0], "sh_l": reps[b][1], "sc_c": reps[2][0], "sh_c": reps[2][1]})
        d.update(extra)
        ims.append(d)
    res = run(p, ims).results
    ol = np.concatenate([r["ol"] for r in res], 0); oc = np.concatenate([r["oc"] for r in res], 0)
    if router_w is not None:
        return ol, oc, np.concatenate([r["al"] for r in res], 0), np.concatenate([r["ac"] for r in res], 0)
    return ol, oc


def _layer(l, xl, xc, mod_l, W):
    h1l, h1c = _run_norm(xl, xc, W["norm1_w"][l], mod_l, 1, 0)
    h = tok_order(h1l.reshape(2, 4096, D), h1c.reshape(2, 256, D))
    hT = np.ascontiguousarray(h.reshape(68, 128, 32, 128).transpose(0, 3, 2, 1))
    del h
    p = _prog("proj", build_proj)
    w_in = W["w_in"][l]
    ims = []
    sls = []
    for c in range(8):
        idx, sl = head_cols(c); sls.append(sl)
        ims.append({"hT": hT, "w": np.ascontiguousarray(w_in[:, idx].reshape(32, 128, NPROJ).transpose(1, 0, 2))})
    proj = [r["o"] for r in run(p, ims).results]
    del ims, hT
    tabs = fnet_tables()
    p = _prog("fnet", build_fnet_sconv)
    ims = []
    for c in range(8):
        pc = proj[c]; sl = sls[c]
        d = {"finT": _fm(pc[:, sl["fin"]]), "shT": _fm(pc[:, sl["sh"]]), "sbT": _fm(pc[:, sl["sb"]]), "sgT": _fm(pc[:, sl["sg"]]),
             "cw": np.ascontiguousarray(W["sc_conv_w"][l][:, c * 128:(c + 1) * 128].T)}
        d.update(tabs)
        ims.append(d)
    res = run(p, ims).results
    ob = np.stack([r["ob"] for r in res]); od = np.stack([r["od"] for r in res])
    p = _prog("gla", build_gla)
    ims = []
    for c in range(8):
        pc = proj[c]; sl = sls[c]
        lr = pc[:, sl["glr"]]
        ims.append({"qT": _fm(pc[:, sl["gq"]]), "kT": _fm(pc[:, sl["gk"]]), "vtok": _tokc(pc[:, sl["gv"]]),
                    "ggtok": _tokc(pc[:, sl["gg"]]).reshape(2, 64, -1),
                    "lrT": np.ascontiguousarray(np.stack([_fm(lr[:, :16]), _fm(lr[:, 16:])], 1)),
                    "up": np.ascontiguousarray(W["gla_gate_up"][l][:, :, c * 64:(c + 1) * 64]),
                    "gb": np.ascontiguousarray(W["gla_gate_b"][l][:, c * 64:(c + 1) * 64].T),
                    "nwr": _rep(W["gla_norm_w"][l], 64),
                    "msk": np.stack([_U64, _U64.T.copy()]), "idn": np.eye(64, dtype=np.float32)})
    oa = np.stack([r["oa"] for r in run(p, ims).results])
    p = _prog("gdn_pre", build_gdn_pre)
    ims = []
    gw = W["gdn_conv_w"][l]
    for c in range(8):
        pc = proj[c]; sl = sls[c]
        comps = [pc[:, sl[n]] for n in ("dq", "dk", "dv")]
        raw = np.ascontiguousarray(np.stack([_tokc(a) for a in comps], 1))
        rawc = np.ascontiguousarray(np.stack([a.reshape(2, TB, 128)[:, :256] for a in comps], 1))
        cwr = np.stack([np.broadcast_to(gw[:, j * 1024 + c * 128: j * 1024 + (c + 1) * 128][None], (64, 3, 128)) for j in range(3)])
        ims.append({"raw": raw, "rawc": rawc, "cwr": np.ascontiguousarray(cwr)})
    qkvn = [r["o"] for r in run(p, ims).results]
    p = _prog("gdn_main", build_gdn_main)
    ims = []
    msk = np.stack([_U64, _V64, _U64.T.copy(), _V64.T.copy()])
    for c in range(8):
        pc = proj[c]; sl = sls[c]
        q5 = qkvn[c].reshape(2, 3, 64, NCH, 128)

        def featmajor(x_):
            return np.ascontiguousarray(x_.transpose(0, 3, 2, 1).reshape(2, 128, TB))
        prm = np.array([W["gdn_dt_bias"][l][0, c], W["gdn_dt_bias"][l][1, c], W["gdn_A_log"][l][0, c], W["gdn_A_log"][l][1, c]], np.float32)
        ims.append({"qT": featmajor(q5[:, 0]), "kT": featmajor(q5[:, 1]), "ktok": np.ascontiguousarray(q5[:, 1]),
                    "vtok": np.ascontiguousarray(q5[:, 2]),
                    "atok": np.ascontiguousarray(np.stack([_tokc(pc[:, sl["af"]])[..., 0], _tokc(pc[:, sl["ab"]])[..., 0]], 1)),
                    "btok": np.ascontiguousarray(np.stack([_tokc(pc[:, sl["bf"]])[..., 0], _tokc(pc[:, sl["bb"]])[..., 0]], 1)),
                    "ztok": _tokc(pc[:, sl["dz"]]).reshape(2, 64, -1), "prm": _rep(prm),
                    "nwr": _rep(W["gdn_norm_w"][l], 64), "msk": msk, "idn": np.eye(64, dtype=np.float32)})
    oc_ = np.stack([r["oc"] for r in run(p, ims).results])
    del ims, proj, qkvn

    def tokmaj_to_fm(o):
        o = o.reshape(8, 2, 64, NCH, 128).transpose(0, 4, 1, 3, 2)
        return o.reshape(8 * 128, 2, TB)

    def fmh(o):
        return o.transpose(0, 2, 1, 3).reshape(8 * 128, 2, TB)
    mixT = np.concatenate([tokmaj_to_fm(oa), fmh(ob), tokmaj_to_fm(oc_), fmh(od)], 0)
    p = _prog("wout", build_wout)
    w = np.ascontiguousarray(W["w_out"][l].reshape(32, 128, D).transpose(1, 0, 2))
    g1 = [_rep(mod_l[m, 2 * D:3 * D]) for m in range(3)]
    ims = []
    for i in range(8):
        b = i // 4; j = i % 4
        lat = mixT[:, b, 256 + j * 1024: 256 + (j + 1) * 1024]; cx = mixT[:, b, j * 64:j * 64 + 64]
        mt = np.concatenate([lat, cx], 1).reshape(32, 128, 1088).transpose(1, 0, 2)
        ims.append({"mixT": np.ascontiguousarray(mt), "w": w, "xl": xl[i * 1024:(i + 1) * 1024], "xc": xc[i * 64:(i + 1) * 64],
                    "g_l": g1[b], "g_c": g1[2]})
    res = run(p, ims).results
    xal = np.concatenate([r["ol"] for r in res], 0); xac = np.concatenate([r["oc"] for r in res], 0)
    del ims, mixT
    h2, h2c, al, ac = _run_norm(xal, xac, W["norm2_w"][l], mod_l, 4, 3, router_w=W["w_router"][l])
    al = al.reshape(2, 4096, 16); ac = ac.reshape(2, 256, 16)
    h2 = h2.reshape(2, 4096, D); h2c = h2c.reshape(2, 256, D)
    p = _prog("route", build_route)
    iota = _rep(np.arange(512, dtype=np.float32))
    tv = (np.arange(32)[None, :] * 128 + np.arange(128)[:, None]).astype(np.float32)
    ims = []
    for j in range(8):
        arow = []; acol = []; arowc = []; acolc = []
        for e in (2 * j, 2 * j + 1):
            for b in range(2):
                a = al[b, :, e]; c_ = ac[b, :, e]
                arow.append(np.broadcast_to(a[None], (128, 4096))); acol.append(a.reshape(32, 128).T)
                arowc.append(np.broadcast_to(c_[None], (128, 256))); acolc.append(c_.reshape(2, 128).T)
        ims.append({"arow": np.ascontiguousarray(np.stack(arow)), "acol": np.ascontiguousarray(np.stack(acol)),
                    "arowc": np.ascontiguousarray(np.stack(arowc)), "acolc": np.ascontiguousarray(np.stack(acolc)), "iota": iota, "tv": tv})
    route = []
    for r in run(p, ims).results:
        route.append({"idx": np.ascontiguousarray(r["idxr"].reshape(4, 4, 128).transpose(0, 2, 1)),
                      "gate": np.ascontiguousarray(r["gater"].reshape(4, 4, 128).transpose(0, 2, 1)),
                      "idxc": np.ascontiguousarray(r["idxcr"].reshape(4, 32, 1)), "gatec": np.ascontiguousarray(r["gatecr"].reshape(4, 32, 1)),
                      "rank": r["rank"], "rankc": r["rankc"]})
    import ml_dtypes
    p = _prog("experts", build_experts)
    idn = np.eye(128, dtype=np.float32).astype(ml_dtypes.bfloat16)
    ims = []
    for j in range(8):
        es = slice(2 * j, 2 * j + 2)
        wg = np.ascontiguousarray(W["w_gate"][l][es].reshape(2, 32, 128, 8, 128).transpose(0, 3, 2, 1, 4).reshape(2, 8, 128, 4096))
        wu = np.ascontiguousarray(W["w_up"][l][es].reshape(2, 32, 128, 8, 128).transpose(0, 3, 2, 1, 4).reshape(2, 8, 128, 4096))
        wd = np.ascontiguousarray(W["w_down"][l][es].reshape(2, 8, 128, 8, 512).transpose(0, 3, 2, 1, 4).reshape(2, 8, 128, 4096))
        r = route[j]
        ims.append({"idx": r["idx"], "gate": r["gate"], "idxc": r["idxc"], "gatec": r["gatec"],
                    "h2_0": h2[0], "h2_1": h2[1], "h2c_0": h2c[0], "h2c_1": h2c[1], "wg": wg, "wu": wu, "wd": wd, "idn": idn})
    res = run(p, ims).results
    yg = [r["yg"] for r in res]; ygc = [r["ygc"] for r in res]
    del ims
    yall = []; ycall = []
    rank = np.zeros((2, 4096, 16), np.float32); rankc = np.zeros((2, 256, 16), np.float32)
    zr = np.zeros((1, D), np.float32)
    for b in range(2):
        rows = []; rowsc = []
        for e in range(16):
            j = e // 2; q = (e % 2) * 2 + b
            rows.append(yg[j][q]); rowsc.append(ygc[j][q])
            rank[b, :, e] = route[j]["rank"][q].T.reshape(-1)
            rankc[b, :, e] = route[j]["rankc"][q].T.reshape(-1)
        yall.append(np.concatenate(rows, 0)); ycall.append(np.concatenate(rowsc, 0))
    del yg, ygc
    eo = np.stack([np.arange(16) * 512 - 1.0e6, np.arange(16) * 32 - 1.0e6]).astype(np.float32)
    eo = _rep(eo)
    idn_bf = np.eye(128, dtype=np.float32).astype(ml_dtypes.bfloat16)
    p = _prog("combine", build_combine)
    g2 = [_rep(mod_l[m, 5 * D:6 * D]) for m in range(3)]
    ims = []
    for i in range(8):
        b = i // 4; j = i % 4
        rk = np.full((128, 9, 16), 1e6, np.float32)
        rk[:, :8, :] = rank[b, j * 1024:(j + 1) * 1024].reshape(8, 128, 16).transpose(1, 0, 2)
        rk[:64, 8, :] = rankc[b, j * 64:(j + 1) * 64]
        ims.append({"xl": xal[i * 1024:(i + 1) * 1024], "xc": xac[i * 64:(i + 1) * 64], "g_l": g2[b], "g_c": g2[2],
                    "rk": rk, "eo": eo, "yall": yall[b], "ycall": ycall[b], "idn": idn_bf})
    res = run(p, ims).results
    return np.concatenate([r["ol"] for r in res], 0), np.concatenate([r["oc"] for r in res], 0)


def kernel(x, c, ctx, c_ctx, w_ada, b_ada, norm1_w, norm2_w, w_in, w_out, gla_gate_up, gla_gate_b, gla_norm_w,
           gdn_conv_w, gdn_A_log, gdn_dt_bias, gdn_norm_w, sc_conv_w, w_router, w_gate, w_up, w_down, final_norm_w):
    W = dict(norm1_w=norm1_w, norm2_w=norm2_w, w_in=w_in, w_out=w_out, gla_gate_up=gla_gate_up, gla_gate_b=gla_gate_b,
             gla_norm_w=gla_norm_w, gdn_conv_w=gdn_conv_w, gdn_A_log=gdn_A_log, gdn_dt_bias=gdn_dt_bias, gdn_norm_w=gdn_norm_w,
             sc_conv_w=sc_conv_w, w_router=w_router, w_gate=w_gate, w_up=w_up, w_down=w_down)
    W = {k: np.asarray(v, np.float32) for k, v in W.items()}
    x = np.asarray(x, np.float32); ctx = np.asarray(ctx, np.float32)
    w_ada = np.asarray(w_ada, np.float32); b_ada = np.asarray(b_ada, np.float32)
    p = _prog("mod", build_mod)
    cvec = np.concatenate([np.asarray(c, np.float32), np.asarray(c_ctx, np.float32)[None]], 0)
    cT = np.ascontiguousarray(cvec.T.reshape(32, 128, 3).transpose(1, 0, 2))
    ims = []
    for j in range(8):
        wj = np.ascontiguousarray(w_ada[:, :, j * 3072:(j + 1) * 3072].reshape(2, 32, 128, 3072).transpose(0, 2, 1, 3))
        ims.append({"cT": cT, "w": wj, "b": np.ascontiguousarray(b_ada[:, None, j * 3072:(j + 1) * 3072])})
    mod = np.concatenate([r["o"] for r in run(p, ims).results], axis=2)
    del ims
    xl = x.reshape(8192, D); xc = ctx.reshape(512, D)
    for l in range(2):
        xl, xc = _layer(l, xl, xc, mod[l], W)
    ol, _ = _run_norm(xl, xc, np.asarray(final_norm_w, np.float32), None, 0, 0, out_bf16=False, modulate=False)
    return ol.reshape(2, 4096, D).astype(np.float32)
```

```python
import numpy as np
import concourse.bass as bass
import concourse.mybir as mybir
from concourse.bass_utils import run_bass_kernel_spmd

F32 = mybir.dt.float32
BF16 = mybir.dt.bfloat16
I32 = mybir.dt.int32
U32 = mybir.dt.uint32
AF = mybir.ActivationFunctionType
ALU = mybir.AluOpType
AX = mybir.AxisListType

SEM_WRAP = 1 << 30
STORE_Q = "pool"


class P:
    def __init__(self, n_dma_sems=24):
        self.nc = bass.Bass("TRN2", target_bir_lowering=False)
        nc = self.nc
        self.eng = {"pe": nc.tensor, "dve": nc.vector, "act": nc.scalar, "pool": nc.gpsimd, "sp": nc.sync}
        self.sem = {k: nc.alloc_semaphore("s_" + k) for k in self.eng}
        self.cnt = {k: 0 for k in self.eng}
        self.know = {k: {} for k in self.eng}
        self.dsem = [nc.alloc_semaphore("d%d" % i) for i in range(n_dma_sems)]
        self.dval = [0] * n_dma_sems
        self.dnext = 0
        self.state = {}
        self.n_ins = 0
        self.n_wait = 0

    def dram(self, name, shape, dt, kind):
        return self.nc.dram_tensor(name, list(shape), dt, kind=kind).ap()

    def sb(self, name, shape, dt=F32):
        return self.nc.alloc_sbuf_tensor("sb_" + name, list(shape), dt).ap()

    def ps(self, name, shape, dt=F32):
        return self.nc.alloc_psum_tensor("ps_" + name, list(shape), dt).ap()

    def _wait(self, e, tok):
        if tok is None:
            return
        if tok[0] == "e":
            src, pos = tok[1], tok[2]
            if src == e and e == "pe":
                return
            kn = self.know[e].get(src, 0)
            if kn >= pos:
                return
            self.eng[e].wait_ge(self.sem[src], pos)
            self.know[e][src] = pos
            self.n_wait += 1
        else:
            i, val = tok[1], tok[2]
            key = ("d", i)
            if self.know[e].get(key, 0) >= val:
                return
            self.eng[e].wait_ge(self.dsem[i], val)
            self.know[e][key] = val
            self.n_wait += 1

    def _name(self, ref):
        ap = ref[0] if isinstance(ref, tuple) else ref
        return ap if isinstance(ap, str) else ap.tensor.name

    def _st(self, ref):
        if isinstance(ref, tuple):
            ap, key = ref
        else:
            ap, key = ref, None
        name = ap if isinstance(ap, str) else ap.tensor.name
        d = self.state.setdefault(name, {})
        return d, key

    def _deps(self, e, reads, writes):
        need = {}

        def add(tok):
            if tok is None:
                return
            k = (tok[0], tok[1])
            if need.get(k, 0) < tok[2]:
                need[k] = tok[2]
        for r in reads:
            d, key = self._st(r)
            keys = list(d.keys()) if key is None else [k for k in d if k is None or k == key]
            for k in keys:
                add(d[k][0])
        for w in writes:
            d, key = self._st(w)
            keys = list(d.keys()) if key is None else [k for k in d if k is None or k == key]
            for k in keys:
                add(d[k][0])
                for t in d[k][1]:
                    add(t)
        for (ty, src), v in need.items():
            self._wait(e, (ty, src, v))

    @staticmethod
    def _addreader(lst, tok):
        for i, t in enumerate(lst):
            if t[0] == tok[0] and t[1] == tok[1]:
                if t[2] < tok[2]:
                    lst[i] = tok
                return
        lst.append(tok)

    def _commit(self, tok, reads, writes):
        for r in reads:
            d, key = self._st(r)
            if key is None:
                if not d:
                    d[None] = [None, []]
                for k in d:
                    self._addreader(d[k][1], tok)
            else:
                if key not in d:
                    base = d.get(None, [None, []])
                    d[key] = [base[0], list(base[1])]
                self._addreader(d[key][1], tok)
        for w in writes:
            d, key = self._st(w)
            if key is None:
                d.clear()
                d[None] = [tok, []]
            else:
                if None in d and key not in d:
                    pass
                d[key] = [tok, []]

    def op(self, e, fn, reads, writes):
        pr = [r for r in reads if self._name(r).startswith("ps_")]
        if pr:
            reads = [r for r in reads if not self._name(r).startswith("ps_")]
            writes = list(writes) + pr
        self._deps(e, reads, writes)
        ins = fn()
        self.cnt[e] += 1
        ins.then_inc(self.sem[e], 1)
        tok = ("e", e, self.cnt[e])
        self.know[e][e] = self.know[e].get(e, 0)
        self._commit(tok, reads, writes)
        self.n_ins += 1
        return ins

    def dma(self, q, out, in_, reads=None, writes=None, **kw):
        reads = [in_] if reads is None else reads
        writes = [out] if writes is None else writes
        out = self._a(out); in_ = self._a(in_)
        if q == "sp" and STORE_Q and not out.tensor.name.startswith("sb_"):
            q = STORE_Q
        i = self.dnext
        self.dnext = (self.dnext + 1) % len(self.dsem)
        if self.dval[i] > 0:
            self._wait(q, ("d", i, self.dval[i]))
        self._deps(q, reads, writes)
        ins = self.eng[q].dma_start(out=out, in_=in_, **kw)
        self.dval[i] += 16
        ins.then_inc(self.dsem[i], 16)
        tok = ("d", i, self.dval[i])
        self._commit(tok, reads, writes)
        self.n_ins += 1
        return tok

    def finish(self, toks_or_refs):
        for r in toks_or_refs:
            d, key = self._st(r)
            for k in d:
                self._wait("sp", d[k][0])
        for e in self.eng:
            if e != "sp" and self.cnt[e] > 0:
                self._wait("sp", ("e", e, self.cnt[e]))

    @staticmethod
    def _a(x):
        return x[0] if isinstance(x, tuple) else x

    def mm(self, out, lhsT, rhs, start=True, stop=True, okey=None, rkeys=None):
        rd = [lhsT, rhs] if rkeys is None else rkeys
        wr = [out if okey is None else (self._a(out), okey)]
        o_, l_, r_ = self._a(out), self._a(lhsT), self._a(rhs)
        return self.op("pe", lambda: self.nc.tensor.matmul(o_, l_, r_, start=start, stop=stop), rd, wr)

    def tr(self, out, in_, ident):
        o_, i_, d_ = self._a(out), self._a(in_), self._a(ident)
        return self.op("pe", lambda: self.nc.tensor.transpose(o_, i_, d_), [in_, ident], [out])

    def act(self, out, in_, func, bias=None, scale=None, accum_out=None, extra_reads=()):
        kw = {}
        rd = [in_] + list(extra_reads)
        if bias is not None:
            kw["bias"] = self._a(bias)
            if not isinstance(bias, (int, float)):
                rd.append(bias)
        if scale is not None:
            kw["scale"] = self._a(scale)
            if not isinstance(scale, (int, float)):
                rd.append(scale)
        wr = [out]
        if accum_out is not None:
            kw["accum_out"] = self._a(accum_out)
            wr.append(accum_out)
        o_, i_ = self._a(out), self._a(in_)
        return self.op("act", lambda: self.nc.scalar.activation(o_, i_, func, **kw), rd, wr)

    def _veng(self, e):
        return self.nc.vector if e == "dve" else self.nc.gpsimd

    def copy(self, out, in_, e="dve"):
        o_, i_ = self._a(out), self._a(in_)
        if e == "act":
            return self.op("act", lambda: self.nc.scalar.copy(o_, i_), [in_], [out])
        return self.op(e, lambda: self._veng(e).tensor_copy(o_, i_), [in_], [out])

    def tt(self, out, in0, in1, op, e="dve"):
        o_, a_, b_ = self._a(out), self._a(in0), self._a(in1)
        return self.op(e, lambda: self._veng(e).tensor_tensor(o_, a_, b_, op), [in0, in1], [out])

    def ts(self, out, in0, s1, op0, s2=None, op1=None, accum_out=None, e="dve"):
        rd = [in0]
        if not isinstance(s1, (int, float)):
            rd.append(s1)
        if s2 is not None and not isinstance(s2, (int, float)):
            rd.append(s2)
        wr = [out]
        kw = {}
        if op1 is not None:
            kw["op1"] = op1
        if accum_out is not None:
            kw["accum_out"] = self._a(accum_out)
            wr.append(accum_out)
        o_, a_, s1_, s2_ = self._a(out), self._a(in0), self._a(s1), self._a(s2)
        return self.op(e, lambda: self._veng(e).tensor_scalar(o_, a_, s1_, s2_, op0, **kw), rd, wr)

    def stt(self, out, in0, scalar, in1, op0, op1, e="dve"):
        rd = [in0, in1]
        if not isinstance(scalar, (int, float)):
            rd.append(scalar)
        o_, a_, s_, b_ = self._a(out), self._a(in0), self._a(scalar), self._a(in1)
        return self.op(e, lambda: self._veng(e).scalar_tensor_tensor(o_, a_, s_, b_, op0, op1), rd, [out])

    def memset(self, out, val, e="dve"):
        o_ = self._a(out)
        return self.op(e, lambda: self._veng(e).memset(o_, val), [], [out])

    def reduce(self, out, in_, op=ALU.add, axis=AX.X, e="dve"):
        o_, i_ = self._a(out), self._a(in_)
        return self.op(e, lambda: self._veng(e).tensor_reduce(o_, i_, axis, op), [in_], [out])

    def recip(self, out, in_):
        o_, i_ = self._a(out), self._a(in_)
        return self.op("dve", lambda: self.nc.vector.reciprocal(o_, i_), [in_], [out])


def run(p, in_maps, n=8):
    return run_bass_kernel_spmd(p.nc, in_maps, core_ids=list(range(n)))


D = 4096
NCORES = 8


def EPS_AP(p, val=1e-6):
    key = "_eps%g" % val
    if not hasattr(p, key):
        t = p.sb("eps%d" % len([k for k in p.__dict__ if k.startswith("_eps")]), [128, 1])
        p.memset(t, val)
        setattr(p, key, t)
    return getattr(p, key)
_CACHE = {}


def build_norm(out_bf16=True, modulate=True, router=False):
    p = P()
    xl = p.dram("xl", [1024, D], F32, "ExternalInput")
    xc = p.dram("xc", [64, D], F32, "ExternalInput")
    nw = p.dram("nw", [128, D], F32, "ExternalInput")
    odt = BF16 if out_bf16 else F32
    ol = p.dram("ol", [1024, D], odt, "ExternalOutput")
    oc = p.dram("oc", [64, D], odt, "ExternalOutput")
    nw_t = p.sb("nw_t", [128, D])
    p.dma("sp", nw_t, nw)
    wv = {}; sh = {}
    if modulate:
        for ty in ("l", "c"):
            sc_d = p.dram("sc_" + ty, [128, D], F32, "ExternalInput")
            sh_d = p.dram("sh_" + ty, [128, D], F32, "ExternalInput")
            wv[ty] = p.sb("wv_" + ty, [128, D]); sh[ty] = p.sb("sh_" + ty, [128, D])
            p.dma("sp", wv[ty], sc_d); p.dma("sp", sh[ty], sh_d)
            p.stt(wv[ty], wv[ty], 1.0, nw_t, ALU.add, ALU.mult)
    else:
        wv = {"l": nw_t, "c": nw_t}
    if router:
        wr = p.dram("wr", [128, 32, 16], F32, "ExternalInput")
        idn = p.dram("idn", [128, 128], F32, "ExternalInput")
        al = p.dram("al", [1024, 16], F32, "ExternalOutput"); ac = p.dram("ac", [64, 16], F32, "ExternalOutput")
        wrt = p.sb("wrt", [128, 32, 16]); idt = p.sb("idt", [128, 128])
        p.dma("sp", wrt, wr); p.dma("sp", idt, idn)
        hT = p.sb("hT", [128, 32, 128])
        ptr = [p.ps("ptr%d" % i, [128, 512]) for i in range(3)]
        plg = p.ps("plg", [128, 16])
        lg = p.sb("lg", [128, 16]); mx = p.sb("mx", [128, 1]); sm = p.sb("sm", [128, 1]); af = [p.sb("af%d" % i, [128, 16]) for i in range(2)]
    xt = [p.sb("xt%d" % i, [128, D]) for i in range(2)]
    sq = p.sb("sq", [128, D])
    ot = [p.sb("ot%d" % i, [128, D], odt) for i in range(2)]
    ss = [p.sb("ss%d" % i, [128, 1]) for i in range(2)]
    rs = [p.sb("rs%d" % i, [128, 1]) for i in range(2)]
    for t in range(9):
        n = 128 if t < 8 else 64
        ty = "l" if t < 8 else "c"
        src = xl[t * 128:(t + 1) * 128, :] if t < 8 else xc
        dst = ol[t * 128:(t + 1) * 128, :] if t < 8 else oc
        X = xt[t % 2]; O = ot[t % 2]; S = ss[t % 2]; Rr = rs[t % 2]
        p.dma("sp", X[:n], src)
        p.act(sq[:n], X[:n], AF.Square, accum_out=S[:n])
        p.act(Rr[:n], S[:n], AF.Ln, scale=1.0 / D, bias=EPS_AP(p)[:n])
        p.act(Rr[:n], Rr[:n], AF.Exp, scale=-0.5)
        if modulate:
            p.stt(sq[:n], X[:n], Rr[:n], wv[ty][:n], ALU.mult, ALU.mult, e="dve")
            if router:
                p.tt(sq[:n], sq[:n], sh[ty][:n], ALU.add, e="dve")
                p.copy(O[:n], sq[:n], e="act")
            else:
                p.tt(O[:n], sq[:n], sh[ty][:n], ALU.add, e="dve")
        else:
            p.stt(O[:n], X[:n], Rr[:n], wv[ty][:n], ALU.mult, ALU.mult, e="dve")
        p.dma("sp", dst, O[:n])
        if router:
            for g in range(8):
                pt = ptr[g % 3]
                for i in range(4):
                    kc = g * 4 + i
                    p.tr(pt[:, i * 128:i * 128 + n], sq[:n, kc * 128:(kc + 1) * 128], idt[:n, :n])
                src_v = pt.rearrange("p (a t) -> p a t", a=4)[:, :, :n]
                p.copy(hT[:, g * 4:(g + 1) * 4, :n], src_v, e=("dve", "act")[g % 2])
            for kc in range(32):
                p.mm(plg[:n], hT[:, kc, :n], wrt[:, kc, :], start=(kc == 0), stop=(kc == 31))
            A_ = af[t % 2]
            p.copy(lg[:n], plg[:n], e="dve")
            p.reduce(mx[:n], lg[:n], op=ALU.max, axis=AX.X, e="dve")
            p.ts(mx[:n], mx[:n], -1.0, ALU.mult)
            p.act(A_[:n], lg[:n], AF.Exp, bias=mx[:n], accum_out=sm[:n])
            p.recip(sm[:n], sm[:n])
            p.ts(A_[:n], A_[:n], sm[:n], ALU.mult)
            p.dma("sp", al[t * 128:(t + 1) * 128, :] if t < 8 else ac, A_[:n])
    p.finish([ol, oc] + ([al, ac] if router else []))
    return p


NPROJ = 1444
NTOK = 8704


def build_proj():
    p = P()
    NT = NTOK // 128
    hT = p.dram("hT", [NT, 128, 32, 128], BF16, "ExternalInput")
    w = p.dram("w", [128, 32, NPROJ], F32, "ExternalInput")
    o = p.dram("o", [NTOK, NPROJ], F32, "ExternalOutput")
    wb = p.sb("wb", [128, 32, NPROJ], BF16)
    stg = [p.sb("stg%d" % i, [128, 2, NPROJ]) for i in range(2)]
    for c in range(16):
        s = stg[c % 2]
        p.dma("sp", s, w[:, 2 * c:2 * c + 2, :])
        p.copy((wb[:, 2 * c:2 * c + 2, :], c), s, e=("dve", "act")[c % 2])
    ht = [p.sb("ht%d" % i, [128, 32, 128], BF16) for i in range(3)]
    ot = [p.sb("ot%d" % i, [128, NPROJ]) for i in range(2)]
    pst = [p.ps("pp%d" % i, [128, 512]) for i in range(6)]
    ntl = [(0, 512), (512, 512), (1024, NPROJ - 1024)]
    for t in range(NT):
        H = ht[t % 3]; O = ot[t % 2]
        p.dma("sp", H, hT[t])
        for j, (n0, nn) in enumerate(ntl):
            ps = pst[(t % 2) * 3 + j]
            for kc in range(32):
                p.mm(ps[:, :nn], H[:, kc, :], wb[:, kc, n0:n0 + nn], start=(kc == 0), stop=(kc == 31),
                     rkeys=[H, (wb, kc // 2)])
            if j == 1:
                p.copy(O[:, n0:n0 + nn], ps[:, :nn], e="act")
            else:
                p.copy(O[:, n0:n0 + nn], ps[:, :nn], e="dve")
        p.dma("sp", o[t * 128:(t + 1) * 128, :], O)
    p.finish([o])
    return p


def build_mod():
    p = P()
    NCOL = 3072
    cT = p.dram("cT", [128, 32, 3], F32, "ExternalInput")
    w = p.dram("w", [2, 128, 32, NCOL], F32, "ExternalInput")
    b = p.dram("b", [2, 1, NCOL], F32, "ExternalInput")
    o = p.dram("o", [2, 3, NCOL], F32, "ExternalOutput")
    ct = p.sb("ct", [128, 32, 3]); st = p.sb("st", [128, 32, 3])
    ones = p.sb("ones", [1, 3]); bt = p.sb("bt", [1, 2, NCOL])
    p.dma("sp", ct, cT)
    p.dma("sp", bt[:, 0, :], b[0]); p.dma("sp", bt[:, 1, :], b[1])
    p.act(st, ct, AF.Silu)
    p.memset(ones, 1.0)
    wt = [p.sb("wt%d" % i, [128, 8, 512]) for i in range(3)]
    pst = [p.ps("pm%d" % i, [3, 512]) for i in range(2)]
    ot = p.sb("ot", [3, 2, NCOL])
    i = 0
    for l in range(2):
        for nt in range(6):
            ps = pst[(l * 6 + nt) % 2]
            for kg in range(4):
                W = wt[i % 3]; i += 1
                p.dma("sp", W, w[l, :, kg * 8:(kg + 1) * 8, nt * 512:(nt + 1) * 512])
                for k in range(8):
                    p.mm(ps, st[:, kg * 8 + k, :], W[:, k, :], start=(kg == 0 and k == 0), stop=False)
            p.mm(ps, ones, bt[:, l, nt * 512:(nt + 1) * 512], start=False, stop=True)
            p.copy(ot[:, l, nt * 512:(nt + 1) * 512], ps, e="dve")
    p.dma("sp", o[0], ot[:, 0, :]); p.dma("sp", o[1], ot[:, 1, :])
    p.finish([o])
    return p


IN_SIZES = (512, 512, 1024, 1024, 32, 1024, 3072, 1024, 16, 16, 1024, 1024, 1024)
IN_OFF = np.concatenate([[0], np.cumsum(IN_SIZES)]).astype(int)


def head_cols(h):
    o = IN_OFF
    segs = [("gq", o[0] + h * 64, 64), ("gk", o[1] + h * 64, 64), ("gv", o[2] + h * 128, 128), ("gg", o[3] + h * 128, 128),
            ("glr", o[4], 32), ("fin", o[5] + h * 128, 128),
            ("dq", o[6] + h * 128, 128), ("dk", o[6] + 1024 + h * 128, 128), ("dv", o[6] + 2048 + h * 128, 128),
            ("dz", o[7] + h * 128, 128), ("bf", o[8] + h, 1), ("bb", o[8] + 8 + h, 1), ("af", o[9] + h, 1), ("ab", o[9] + 8 + h, 1),
            ("sh", o[10] + h * 128, 128), ("sb", o[11] + h * 128, 128), ("sg", o[12] + h * 128, 128)]
    idx = np.concatenate([np.arange(s, s + n) for _, s, n in segs])
    sl = {}
    pos = 0
    for nm, s, n in segs:
        sl[nm] = slice(pos, pos + n); pos += n
    assert pos == NPROJ
    return idx, sl


def tok_order(lat, ctx):
    return np.concatenate([np.concatenate([ctx[b], lat[b]], 0) for b in range(2)], 0)


TB = 4352
NCH = 68


def build_fnet_sconv():
    p = P()
    finT = p.dram("finT", [2, 128, TB], F32, "ExternalInput")
    shT = p.dram("shT", [2, 128, TB], F32, "ExternalInput")
    sbT = p.dram("sbT", [2, 128, TB], F32, "ExternalInput")
    sgT = p.dram("sgT", [2, 128, TB], F32, "ExternalInput")
    cw = p.dram("cw", [128, 3], F32, "ExternalInput")
    cs_l = p.dram("cs_l", [128, 256], F32, "ExternalInput")
    cs_c = p.dram("cs_c", [128, 256], F32, "ExternalInput")
    tab = p.dram("tab", [8, 4, 128, 2, 8, 512], BF16, "ExternalInput")
    tabc = p.dram("tabc", [128, 2, 2, 256], BF16, "ExternalInput")
    ob = p.dram("ob", [2, 128, TB], BF16, "ExternalOutput")
    od = p.dram("od", [2, 128, TB], BF16, "ExternalOutput")
    A = [p.sb("A%d" % i, [128, TB]) for i in range(4)]
    cwt = p.sb("cwt", [128, 3]); csl = p.sb("csl", [128, 256]); csc = p.sb("csc", [128, 256])
    tct = p.sb("tct", [128, 2, 2, 256], BF16)
    p.dma("sp", cwt, cw); p.dma("sp", csl, cs_l); p.dma("sp", csc, cs_c); p.dma("sp", tct, tabc)
    gcs = [p.sb("gcs%d" % b, [128, 34, 256], BF16) for b in range(2)]
    ps1 = [p.ps("f1_%d" % i, [128, 512]) for i in range(2)]
    ps2 = [p.ps("f2_%d" % i, [128, 512]) for i in range(4)]
    for b in range(2):
        p.dma("sp", A[b], finT[b])
    k = 0
    for b in range(2):
        for g in range(17):
            ps = ps1[k % 2]; k += 1
            for j in range(2):
                tcn = 2 * g + j
                p.mm(ps[:, j * 256:(j + 1) * 256], A[b][:, tcn * 128:(tcn + 1) * 128], csc if tcn < 2 else csl)
            p.copy(gcs[b][:, 2 * g:2 * g + 2, :], ps.rearrange("p (a n) -> p a n", a=2), e=("dve", "act")[k % 2])
    obt = [p.sb("obt%d" % b, [128, TB], BF16) for b in range(2)]
    for b in range(2):
        ps = ps2[b]
        i = 0
        for cs in range(2):
            for tcn in range(2):
                p.mm(ps[:, :256], gcs[b][:, tcn, cs * 128:(cs + 1) * 128], tct[:, cs, tcn, :], start=(i == 0), stop=(i == 3)); i += 1
        p.copy(obt[b][:, 0:256], ps[:, :256], e="dve")
    tb_ = [p.sb("tb%d" % i, [128, 2, 8, 512], BF16) for i in range(4)]
    k = 0
    for j in range(8):
        for tg in range(4):
            T_ = tb_[k % 4]; k += 1
            p.dma("sp", T_, tab[j, tg])
            for b in range(2):
                ps = ps2[(j % 2) * 2 + b]
                for cs in range(2):
                    for t8 in range(8):
                        tcn = 2 + tg * 8 + t8
                        p.mm(ps, gcs[b][:, tcn, cs * 128:(cs + 1) * 128], T_[:, cs, t8, :],
                             start=(tg == 0 and cs == 0 and t8 == 0), stop=(tg == 3 and cs == 1 and t8 == 7))
        for b in range(2):
            p.copy(obt[b][:, 256 + j * 512:256 + (j + 1) * 512], ps2[(j % 2) * 2 + b], e=("dve", "act")[b])
    for b in range(2):
        p.dma("sp", ob[b], obt[b])
    odt = [p.sb("odt%d" % b, [128, TB], BF16) for b in range(2)]
    for b in range(2):
        SH, SB_, SG, ACC = A[0], A[1], A[2], A[3]
        p.dma("sp", SH, shT[b]); p.dma("sp", SB_, sbT[b]); p.dma("sp", SG, sgT[b])
        p.tt(SG, SG, SH, ALU.mult, e="dve")
        p.ts(ACC, SG, cwt[:, 1:2], ALU.mult, e="dve")
        for (lo, n, rows) in ((0, 256, 1), (256, 4096, 64)):
            w_ = n // rows
            u3 = SG[:, lo:lo + n].rearrange("p (r w) -> p r w", r=rows)
            a3 = ACC[:, lo:lo + n].rearrange("p (r w) -> p r w", r=rows)
            p.stt(a3[:, :, 1:], u3[:, :, :w_ - 1], cwt[:, 0:1], a3[:, :, 1:], ALU.mult, ALU.add, e="dve")
            p.stt(a3[:, :, :w_ - 1], u3[:, :, 1:], cwt[:, 2:3], a3[:, :, :w_ - 1], ALU.mult, ALU.add, e="dve")
        p.tt(odt[b], ACC, SB_, ALU.mult, e="dve")
        p.dma("sp", od[b], odt[b])
    p.finish([ob, od])
    return p


def fnet_tables():
    key = "fnet_tables"
    if key in _CACHE:
        return _CACHE[key]
    import ml_dtypes
    bf = ml_dtypes.bfloat16
    out = {}
    c = np.arange(128)
    ang = 2 * np.pi * np.outer(c, c) / 128.0
    for nm, T in (("cs_l", 4096), ("cs_c", 256)):
        s = 1.0 / np.sqrt(T * 128.0)
        out[nm] = np.concatenate([np.cos(ang) * s, np.sin(ang) * s], 1).astype(np.float32)
    t = np.arange(4096)
    m = (np.outer(t, t) % 4096).astype(np.int64)
    base = 2 * np.pi * np.arange(4096) / 4096.0
    cosv = np.cos(base).astype(np.float32); sinv = (-np.sin(base)).astype(np.float32)
    C = cosv[m].astype(bf); S = sinv[m].astype(bf)
    def lay(M):
        return M.reshape(4, 8, 128, 8, 512).transpose(3, 0, 2, 1, 4)
    out["tab"] = np.ascontiguousarray(np.stack([lay(C), lay(S)], axis=3))
    tc = np.arange(256)
    mc = (np.outer(tc, tc) % 256)
    bc = 2 * np.pi * np.arange(256) / 256.0
    Cc = np.cos(bc)[mc].astype(bf); Sc = (-np.sin(bc))[mc].astype(bf)
    def layc(M):
        return M.reshape(2, 128, 256).transpose(1, 0, 2)
    out["tabc"] = np.ascontiguousarray(np.stack([layc(Cc), layc(Sc)], axis=1))
    _CACHE[key] = out
    return out


def const_tile(p, val, parts=128):
    key = "_c_%g" % val
    if not hasattr(p, key):
        t = p.sb("cst%d" % len([k for k in p.__dict__ if k.startswith("_c_")]), [128, 1])
        p.memset(t, val)
        setattr(p, key, t)
    return getattr(p, key)


CH_FWD = list(range(NCH))
CH_BWD = [3, 2, 1, 0] + list(range(NCH - 1, 3, -1))


def hillis(p, A, B, nparts, bwd, C=64):
    src, dst = A, B
    j = 1
    while j < C:
        s3 = src.rearrange("p (n c) -> p n c", c=C); d3 = dst.rearrange("p (n c) -> p n c", c=C)
        if not bwd:
            p.tt(d3[:nparts, :, j:], s3[:nparts, :, j:], s3[:nparts, :, :C - j], ALU.add, e="dve")
            p.copy(d3[:nparts, :, :j], s3[:nparts, :, :j], e="act")
        else:
            p.tt(d3[:nparts, :, :C - j], s3[:nparts, :, :C - j], s3[:nparts, :, j:], ALU.add, e="dve")
            p.copy(d3[:nparts, :, C - j:], s3[:nparts, :, C - j:], e="act")
        src, dst = dst, src
        j *= 2
    return src


def build_gla():
    p = P()
    qT = p.dram("qT", [2, 64, TB], F32, "ExternalInput")
    kT = p.dram("kT", [2, 64, TB], F32, "ExternalInput")
    vtok = p.dram("vtok", [2, 64, NCH, 128], F32, "ExternalInput")
    ggtok = p.dram("ggtok", [2, 64, NCH * 128], F32, "ExternalInput")
    lrT = p.dram("lrT", [2, 2, 16, TB], F32, "ExternalInput")
    up = p.dram("up", [2, 16, 64], F32, "ExternalInput")
    gb = p.dram("gb", [64, 2], F32, "ExternalInput")
    nwr = p.dram("nwr", [64, 128], F32, "ExternalInput")
    msk = p.dram("msk", [2, 64, 64], F32, "ExternalInput")
    idn = p.dram("idn", [64, 64], F32, "ExternalInput")
    oa = p.dram("oa", [2, 64, NCH * 128], BF16, "ExternalOutput")

    WAB = p.sb("WAB", [64, 2 * TB]); LA = WAB[:, :TB]; LB = WAB[:, TB:]
    Q_ = p.sb("Q_", [64, TB]); K_ = p.sb("K_", [64, TB]); KT = p.sb("KT", [64, TB]); AT = p.sb("AT", [64, TB])
    V = p.sb("V", [64, NCH, 128]); O = p.sb("O", [64, NCH, 128])
    upt = p.sb("upt", [16, 2, 64]); gbt = p.sb("gbt", [64, 2]); nwt = p.sb("nwt", [64, 128])
    mk = p.sb("mk", [64, 2, 64]); idt = p.sb("idt", [64, 64])
    tot = p.sb("tot", [64, NCH]); dT = p.sb("dT", [64, NCH])
    S = [p.sb("S%d" % i, [64, 128]) for i in range(3)]
    ss = p.sb("ssq", [64, NCH]); rst = p.sb("rst", [64, NCH])
    one = const_tile(p, 1.0); eps = const_tile(p, 1e-6)
    for d in range(2):
        p.dma("sp", upt[:, d, :], up[d]); p.dma("sp", mk[:, d, :], msk[d])
    p.dma("sp", gbt, gb); p.dma("sp", nwt, nwr); p.dma("sp", idt, idn)
    p.ts(gbt, gbt, -1.0, ALU.mult)
    psA = [p.ps("ga%d" % i, [64, 512]) for i in range(2)]
    psT = [p.ps("gt%d" % i, [64, 512]) for i in range(2)]
    psO = [p.ps("go%d" % i, [64, 512]) for i in range(2)]
    psI = [p.ps("gi%d" % i, [64, 128]) for i in range(2)]
    LR = AT[:16, :]
    tiles = [(i * 512, 512) for i in range(8)] + [(4096, 256)]
    for b in range(2):
        p.dma("sp", V, vtok[b])
        for d in range(2):
            bwd = d == 1
            p.dma("sp", LR, lrT[b, d])
            for i, (t0, tn) in enumerate(tiles):
                ps = psA[i % 2]
                p.mm(ps[:, :tn], upt[:, d, :], LR[:, t0:t0 + tn])
                p.act(LA[:, t0:t0 + tn], ps[:, :tn], AF.Exp, scale=-1.0, bias=gbt[:, d:d + 1])
            p.act(LA, LA, AF.Ln, bias=one[:64])
            cum = hillis(p, LA, LB, 64, bwd)
            assert cum is LA
            c3 = LA.rearrange("p (n c) -> p n c", c=64)
            p.copy(tot, c3[:, :, 0] if bwd else c3[:, :, 63], e="dve")
            p.act(dT, tot, AF.Exp, scale=-1.0 / 16)
            p.act(AT, LA, AF.Exp, scale=-1.0 / 16)
            p.dma("sp", Q_, qT[b])
            p.stt(Q_, Q_, 0.125, AT, ALU.mult, ALU.mult, e="dve")
            p.act(AT, LA, AF.Exp, scale=1.0 / 16)
            p.dma("sp", K_, kT[b])
            p.tt(K_, K_, AT, ALU.mult, e="dve")
            b3 = LB.rearrange("p (n c) -> p n c", c=64)
            p.tt(b3, c3, tot.unsqueeze(2).to_broadcast([64, NCH, 64]), ALU.subtract, e="dve")
            p.act(LB, LB, AF.Exp, scale=1.0 / 16)
            p.dma("sp", AT, kT[b])
            p.tt(LB, LB, AT, ALU.mult, e="dve")
            kt3 = KT.rearrange("p (n c) -> p n c", c=64); at3 = AT.rearrange("p (n c) -> p n c", c=64)
            for g in range(9):
                ng = min(8, NCH - g * 8)
                pt = psT[g % 2]; pa = psA[g % 2]
                for i in range(ng):
                    n = g * 8 + i
                    p.tr(pt[:, i * 64:(i + 1) * 64], LB[:, n * 64:(n + 1) * 64], idt)
                p.copy(kt3[:, g * 8:g * 8 + ng, :], pt[:, :ng * 64].rearrange("p (n c) -> p n c", c=64), e="act")
            for g in range(9):
                ng = min(8, NCH - g * 8)
                pa = psA[g % 2]
                for i in range(ng):
                    n = g * 8 + i
                    p.mm(pa[:, i * 64:(i + 1) * 64], K_[:, n * 64:(n + 1) * 64], Q_[:, n * 64:(n + 1) * 64])
                p.tt(at3[:, g * 8:g * 8 + ng, :], pa[:, :ng * 64].rearrange("p (n c) -> p n c", c=64),
                     mk[:, d:d + 1, :].to_broadcast([64, ng, 64]), ALU.mult, e="dve")
            order = CH_BWD if bwd else CH_FWD
            p.memset(S[0], 0.0)
            si = 0
            for step, n in enumerate(order):
                grp = n // 4
                po = psO[grp % 2]
                sl = po[:, (n % 4) * 128:(n % 4 + 1) * 128]
                p.mm(sl, at3[:, n, :], V[:, n, :], start=True, stop=False)
                p.mm(sl, Q_[:, n * 64:(n + 1) * 64], S[si], start=False, stop=True)
                pi = psI[step % 2]
                p.mm(pi, kt3[:, n, :], V[:, n, :])
                sn = (si + 1) % 3
                p.stt(S[sn], S[si], dT[:, n:n + 1], pi, ALU.mult, ALU.add, e="dve")
                si = sn
                last_in_grp = (n % 4 == 0) if bwd else (n % 4 == 3)
                if last_in_grp:
                    og = O[:, grp * 4:grp * 4 + 4, :]
                    pv = po.rearrange("p (n v) -> p n v", v=128)
                    if not bwd:
                        p.copy(og, pv, e="act")
                    else:
                        p.tt(og, og, pv, ALU.add, e="pool" if False else "dve")
        GG = WAB.rearrange("p (n v) -> p n v", v=128)
        p.dma("sp", WAB, ggtok[b])
        sq = Q_.rearrange("p (n c) -> p n c", c=64)
        SQ = p_view_sq = None
        h = NCH // 2
        for half, buf in ((0, Q_), (1, K_)):
            o_h = O[:, half * h:(half + 1) * h, :]
            b_h = buf.rearrange("p (n v) -> p n v", v=128)
            p.tt(b_h, o_h, o_h, ALU.mult, e="dve")
            p.reduce(ss[:, half * h:(half + 1) * h], b_h, op=ALU.add, axis=AX.X, e="dve")
        p.act(rst, ss, AF.Ln, scale=1.0 / 128, bias=eps[:64])
        p.act(rst, rst, AF.Exp, scale=-0.5)
        p.act(WAB, WAB, AF.Silu)
        p.tt(O, O, rst.unsqueeze(2).to_broadcast([64, NCH, 128]), ALU.mult, e="dve")
        p.tt(O, O, nwt.unsqueeze(1).to_broadcast([64, NCH, 128]), ALU.mult, e="dve")
        OUT = KT.bitcast(BF16).rearrange("p (n v) -> p n v", v=128)
        p.tt(OUT, O, GG, ALU.mult, e="dve")
        p.dma("sp", oa[b], KT.bitcast(BF16))
    p.finish([oa])
    return p


def build_gdn_pre():
    p = P()
    raw = p.dram("raw", [2, 3, 64, NCH, 128], F32, "ExternalInput")
    rawc = p.dram("rawc", [2, 3, 256, 128], F32, "ExternalInput")
    cwr = p.dram("cwr", [3, 64, 3, 128], F32, "ExternalInput")
    o = p.dram("o", [2, 3, 64, NCH * 128], F32, "ExternalOutput")
    RAW = [p.sb("RAW%d" % i, [64, NCH, 128]) for i in range(2)]
    ACC = [p.sb("ACC%d" % i, [64, NCH, 128]) for i in range(2)]
    TMP = p.sb("TMP", [64, NCH, 128])
    M1 = p.sb("M1", [64, 4, 128]); P1 = p.sb("P1", [64, 4, 128]); TC = p.sb("TC", [64, 4, 128])
    cw = p.sb("cw", [64, 3, 3, 128])
    ss = p.sb("ss", [64, NCH]); rn = p.sb("rn", [64, NCH])
    eps = const_tile(p, 1e-6)
    for c in range(3):
        p.dma("sp", cw[:, c], cwr[c])
    k = 0
    for b in range(2):
        for c in range(3):
            R_ = RAW[k % 2]; A_ = ACC[k % 2]; k += 1
            p.dma("sp", R_, raw[b, c])
            p.memset(M1, 0.0, e="pool"); p.memset(P1, 0.0, e="pool")
            src = rawc[b, c]
            p.dma("sp", M1[1:64, 0, :], src[0:63, :])
            p.dma("sp", M1[:, 1:4, :], src[63:255, :].rearrange("(n s) c -> s n c", s=64))
            p.dma("sp", P1[:, 0:3, :], src[1:193, :].rearrange("(n s) c -> s n c", s=64))
            p.dma("sp", P1[0:63, 3, :], src[193:256, :])
            w0 = cw[:, c, 0:1, :]; w1 = cw[:, c, 1:2, :]; w2 = cw[:, c, 2:3, :]
            p.tt(A_, R_, w1.to_broadcast([64, NCH, 128]), ALU.mult, e="dve")
            p.tt(TMP[:, 5:68], R_[:, 4:67], w0.to_broadcast([64, 63, 128]), ALU.mult, e="dve")
            p.tt(A_[:, 5:68], A_[:, 5:68], TMP[:, 5:68], ALU.add, e="dve")
            p.tt(TMP[:, 4:67], R_[:, 5:68], w2.to_broadcast([64, 63, 128]), ALU.mult, e="dve")
            p.tt(A_[:, 4:67], A_[:, 4:67], TMP[:, 4:67], ALU.add, e="dve")
            p.tt(TC, M1, w0.to_broadcast([64, 4, 128]), ALU.mult, e="dve")
            p.tt(A_[:, 0:4], A_[:, 0:4], TC, ALU.add, e="dve")
            p.tt(TC, P1, w2.to_broadcast([64, 4, 128]), ALU.mult, e="dve")
            p.tt(A_[:, 0:4], A_[:, 0:4], TC, ALU.add, e="dve")
            p.act(A_, A_, AF.Silu)
            if c < 2:
                p.tt(TMP, A_, A_, ALU.mult, e="dve")
                p.reduce(ss, TMP, op=ALU.add, axis=AX.X, e="dve")
                p.act(rn, ss, AF.Ln, bias=eps[:64])
                p.act(rn, rn, AF.Exp, scale=-0.5)
                if c == 0:
                    p.ts(rn, rn, float(128 ** -0.5), ALU.mult)
                p.tt(A_, A_, rn.unsqueeze(2).to_broadcast([64, NCH, 128]), ALU.mult, e="dve")
            p.dma("sp", o[b, c], A_.rearrange("p n c -> p (n c)"))
    p.finish([o])
    return p


GROUPS = [(0, 4)] + [(4 + 8 * i, 8) for i in range(8)]


def build_gdn_main():
    p = P()
    qT = p.dram("qT", [2, 128, TB], F32, "ExternalInput")
    kT = p.dram("kT", [2, 128, TB], F32, "ExternalInput")
    ktok = p.dram("ktok", [2, 64, NCH, 128], F32, "ExternalInput")
    vtok = p.dram("vtok", [2, 64, NCH, 128], F32, "ExternalInput")
    atok = p.dram("atok", [2, 2, 64, NCH], F32, "ExternalInput")
    btok = p.dram("btok", [2, 2, 64, NCH], F32, "ExternalInput")
    ztok = p.dram("ztok", [2, 64, NCH * 128], F32, "ExternalInput")
    prm = p.dram("prm", [128, 4], F32, "ExternalInput")
    nwr = p.dram("nwr", [64, 128], F32, "ExternalInput")
    msk = p.dram("msk", [4, 64, 64], F32, "ExternalInput")
    idn = p.dram("idn", [64, 64], F32, "ExternalInput")
    oc = p.dram("oc", [2, 64, NCH * 128], BF16, "ExternalOutput")

    QT = p.sb("QT", [128, TB]); KTs = p.sb("KTs", [128, TB])
    O = p.sb("O", [64, NCH, 128]); Z = p.sb("Z", [64, NCH * 128]); SQ = p.sb("SQ", [64, TB])
    prt = p.sb("prt", [128, 4]); nwt = p.sb("nwt", [64, 128]); mk = p.sb("mk", [64, 4, 64]); idt = p.sb("idt", [64, 64])
    ones = p.sb("ones", [64, 128])
    one = const_tile(p, 1.0); eps = const_tile(p, 1e-6)
    p.dma("sp", prt, prm); p.dma("sp", nwt, nwr); p.dma("sp", idt, idn)
    for i in range(4):
        p.dma("sp", mk[:, i, :], msk[i])
    p.memset(ones, 1.0)
    expA = p.sb("expA", [128, 2])
    p.act(expA, prt[:, 2:4], AF.Exp)
    At = p.sb("At", [64, NCH]); Bt = p.sb("Bt", [64, NCH]); L = p.sb("L", [64, NCH])
    beta = p.sb("beta", [64, NCH]); nbeta = p.sb("nbeta", [64, NCH]); eg = p.sb("eg", [64, NCH])
    tot = p.sb("tot", [64, NCH]); kes = p.sb("kes", [64, NCH]); bw = p.sb("bw", [64, NCH]); dS = p.sb("dS", [128, NCH])
    ss = p.sb("ssq", [64, NCH]); rst = p.sb("rst", [64, NCH])
    def g64(nm):
        return p.sb(nm, [64, 8, 64])
    laU = g64("laU"); laV = g64("laV"); LM = g64("LM"); LMT = g64("LMT"); TMPg = g64("TMPg")
    Xa = g64("Xa"); XTa = g64("XTa"); Xb = g64("Xb"); XTb = g64("XTb"); QKM = g64("QKM")
    R = p.sb("R", [64, 8, 256]); R0 = p.sb("R0", [64, 8, 256]); TT = g64("TT")
    KG = p.sb("KG", [64, 8, 128]); VG = p.sb("VG", [64, 8, 128]); KE = p.sb("KE", [64, 8, 128]); VN = p.sb("VN", [64, 8, 128])
    WT = p.sb("WT", [128, 8, 64]); QG = p.sb("QG", [128, 512]); EGR = p.sb("EGR", [128, 512])
    S = [p.sb("S%d" % i, [128, 128]) for i in range(3)]
    PA = [p.ps("PA%d" % i, [64, 1024]) for i in range(2)]; PB = [p.ps("PB%d" % i, [128, 512]) for i in range(2)]
    PC = p.ps("PC", [128, 512]); PD = p.ps("PD", [64, 512])
    PCv = PC[:64, 0:128]; PCs = PC[:, 128:256]

    def fl(t, ng):
        return t[:, :ng, :].rearrange("p n c -> p (n c)")

    for b in range(2):
        p.dma("sp", QT, qT[b]); p.dma("sp", KTs, kT[b])
        for d in range(2):
            bwd = d == 1
            Um = mk[:, 2 * d, :]; Vm = mk[:, 2 * d + 1, :]
            p.dma("sp", At, atok[b, d]); p.dma("sp", Bt, btok[b, d])
            p.act(L, At, AF.Exp, bias=prt[:64, d:d + 1])
            p.act(L, L, AF.Ln, bias=one[:64])
            p.ts(L, L, expA[:64, d:d + 1], ALU.mult)
            p.act(beta, Bt, AF.Exp, scale=-1.0)
            p.ts(beta, beta, 1.0, ALU.add)
            p.recip(beta, beta)
            p.ts(nbeta, beta, -1.0, ALU.mult)
            p.mm(PB[0][:64, :NCH], Um, L)
            p.mm(PB[1][:, :NCH], ones, L)
            p.act(eg, PB[0][:64, :NCH], AF.Exp, scale=-1.0)
            p.act(dS, PB[1][:, :NCH], AF.Exp, scale=-1.0)
            p.copy(tot, PB[1][:64, :NCH], e="dve")
            p.tt(kes, tot, PB[0][:64, :NCH], ALU.subtract)
            p.act(kes, kes, AF.Exp, scale=-1.0)
            p.tt(bw, beta, eg, ALU.mult)
            p.memset(S[0], 0.0)
            si = 0
            gorder = ([GROUPS[0]] + GROUPS[:0:-1]) if bwd else GROUPS
            for (n0, ng) in gorder:
                t0 = n0 * 64; tn = ng * 64
                p.dma("sp", KG[:, :ng], ktok[b, :, n0:n0 + ng, :]); p.dma("sp", VG[:, :ng], vtok[b, :, n0:n0 + ng, :])
                Lg = L[:, n0:n0 + ng].unsqueeze(2).to_broadcast([64, ng, 64])
                p.tt(laU[:, :ng], Um.unsqueeze(1).to_broadcast([64, ng, 64]), Lg, ALU.mult, e="dve")
                p.tt(laV[:, :ng], Vm.unsqueeze(1).to_broadcast([64, ng, 64]), Lg, ALU.mult, e="dve")
                p.mm(PB[0][:64, :tn], Um, fl(laV, ng))
                p.act(fl(LM, ng), PB[0][:64, :tn], AF.Exp, scale=-1.0)
                p.tt(LM[:, :ng], LM[:, :ng], Vm.unsqueeze(1).to_broadcast([64, ng, 64]), ALU.mult, e="dve")
                p.mm(PB[1][:64, :tn], Vm, fl(laU, ng))
                p.act(fl(LMT, ng), PB[1][:64, :tn], AF.Exp, scale=-1.0)
                p.tt(LMT[:, :ng], LMT[:, :ng], Um.unsqueeze(1).to_broadcast([64, ng, 64]), ALU.mult, e="dve")
                for i in range(ng):
                    ks = KTs[:, t0 + i * 64:t0 + (i + 1) * 64]
                    p.mm(PB[0][:64, i * 64:(i + 1) * 64], ks, ks)
                p.tt(fl(TMPg, ng), PB[0][:64, :tn], fl(LM, ng), ALU.mult, e="dve")
                p.tt(Xa[:, :ng], TMPg[:, :ng], nbeta[:, n0:n0 + ng].unsqueeze(2).to_broadcast([64, ng, 64]), ALU.mult, e="dve")
                for i in range(ng):
                    p.tr(PB[1][:64, i * 64:(i + 1) * 64], Xa[:, i, :], idt)
                p.copy(fl(XTa, ng), PB[1][:64, :tn], e="act")
                p.tt(R0[:, :ng, 0:128], VG[:, :ng], beta[:, n0:n0 + ng].unsqueeze(2).to_broadcast([64, ng, 128]), ALU.mult, e="dve")
                p.tt(R0[:, :ng, 128:256], KG[:, :ng], bw[:, n0:n0 + ng].unsqueeze(2).to_broadcast([64, ng, 128]), ALU.mult, e="dve")
                p.tt(KE[:, :ng], KG[:, :ng], kes[:, n0:n0 + ng].unsqueeze(2).to_broadcast([64, ng, 128]), ALU.mult, e="dve")
                p.tt(TT[:, :ng], XTa[:, :ng], idt.unsqueeze(1).to_broadcast([64, ng, 64]), ALU.add, e="dve")
                X, XT, Xn, XTn = Xa, XTa, Xb, XTb
                for lev in range(1, 6):
                    for i in range(ng):
                        p.mm(PB[0][:64, i * 64:(i + 1) * 64], XT[:, i, :], X[:, i, :])
                    if lev < 5:
                        for i in range(ng):
                            p.mm(PB[1][:64, i * 64:(i + 1) * 64], X[:, i, :], XT[:, i, :])
                    p.copy(fl(Xn, ng), PB[0][:64, :tn], e="act")
                    if lev < 5:
                        p.copy(fl(XTn, ng), PB[1][:64, :tn], e="act")
                    X, XT, Xn, XTn = Xn, XTn, X, XT
                    for i in range(ng):
                        p.mm(PA[0][:, i * 64:(i + 1) * 64], X[:, i, :], TT[:, i, :])
                    p.tt(fl(TT, ng), fl(TT, ng), PA[0][:, :tn], ALU.add, e="dve")
                for hf in range((ng + 3) // 4):
                    for i in range(4):
                        p.mm(PA[hf][:, i * 256:(i + 1) * 256], TT[:, hf * 4 + i, :], R0[:, hf * 4 + i, :])
                    p.copy(R[:, hf * 4:hf * 4 + 4, :].rearrange("p n c -> p (n c)"), PA[hf], e=("dve", "act")[hf])
                for i in range(ng):
                    p.tr(PB[0][:, i * 64:(i + 1) * 64], R[:, i, 128:256], idt)
                p.copy(fl(WT, ng), PB[0][:, :tn], e="act")
                for i in range(ng):
                    sl_ = slice(t0 + i * 64, t0 + (i + 1) * 64)
                    p.mm(PB[1][:64, i * 64:(i + 1) * 64], KTs[:, sl_], QT[:, sl_])
                p.tt(fl(QKM, ng), PB[1][:64, :tn], fl(LMT, ng), ALU.mult, e="dve")
                p.mm(PB[0][:, :tn], ones, fl(laU, ng))
                p.act(EGR[:, :tn], PB[0][:, :tn], AF.Exp, scale=-1.0)
                p.tt(QG[:, :tn], QT[:, t0:t0 + tn], EGR[:, :tn], ALU.mult, e="dve")
                corder = range(ng - 1, -1, -1) if bwd else range(ng)
                for i in corder:
                    n = n0 + i
                    p.mm(PCv, WT[:, i, :], S[si])
                    p.tt(VN[:, i, :], R[:, i, 0:128], PCv, ALU.subtract, e="dve")
                    slot = n % 4
                    osl = PD[:, slot * 128:(slot + 1) * 128]
                    p.mm(osl, QG[:, i * 64:(i + 1) * 64], S[si], start=True, stop=False)
                    p.mm(osl, QKM[:, i, :], VN[:, i, :], start=False, stop=True)
                    p.mm(PCs, KE[:, i, :], VN[:, i, :])
                    sn = (si + 1) % 3
                    p.stt(S[sn], S[si], dS[:, n:n + 1], PCs, ALU.mult, ALU.add, e="dve")
                    si = sn
                    if (slot == 0) if bwd else (slot == 3):
                        grp = n // 4
                        og = O[:, grp * 4:grp * 4 + 4, :]
                        pv = PD.rearrange("p (n v) -> p n v", v=128)
                        if not bwd:
                            p.copy(og, pv, e="act")
                        else:
                            p.tt(og, og, pv, ALU.add, e="pool" if False else "dve")
        p.dma("sp", Z, ztok[b])
        h = NCH // 2
        for half in range(2):
            o_h = O[:, half * h:(half + 1) * h, :]
            b_h = SQ.rearrange("p (n v) -> p n v", v=128)
            p.tt(b_h, o_h, o_h, ALU.mult, e="dve")
            p.reduce(ss[:, half * h:(half + 1) * h], b_h, op=ALU.add, axis=AX.X, e="dve")
        p.act(rst, ss, AF.Ln, scale=1.0 / 128, bias=eps[:64])
        p.act(rst, rst, AF.Exp, scale=-0.5)
        p.act(Z, Z, AF.Silu)
        p.tt(O, O, rst.unsqueeze(2).to_broadcast([64, NCH, 128]), ALU.mult, e="dve")
        p.tt(O, O, nwt.unsqueeze(1).to_broadcast([64, NCH, 128]), ALU.mult, e="dve")
        OUT = SQ.bitcast(BF16).rearrange("p (n v) -> p n v", v=128)
        p.tt(OUT, O, Z.rearrange("p (n v) -> p n v", v=128), ALU.mult, e="dve")
        p.dma("sp", oc[b], SQ.bitcast(BF16))
    p.finish([oc])
    return p


def build_wout():
    p = P()
    NTK = 1088
    mixT = p.dram("mixT", [128, 32, NTK], BF16, "ExternalInput")
    w = p.dram("w", [128, 32, D], F32, "ExternalInput")
    xl = p.dram("xl", [1024, D], F32, "ExternalInput"); xc = p.dram("xc", [64, D], F32, "ExternalInput")
    g_l = p.dram("g_l", [128, D], F32, "ExternalInput"); g_c = p.dram("g_c", [128, D], F32, "ExternalInput")
    ol = p.dram("ol", [1024, D], F32, "ExternalOutput"); oc = p.dram("oc", [64, D], F32, "ExternalOutput")
    MT = p.sb("MT", [128, 32, NTK], BF16)
    for q in range(4):
        p.dma("sp", (MT[:, q * 8:(q + 1) * 8, :], q), mixT[:, q * 8:(q + 1) * 8, :])
    WB = [p.sb("WB%d" % i, [128, 32, 512], BF16) for i in range(2)]
    stg = [p.sb("stg%d" % i, [128, 8, 512]) for i in range(2)]
    gl = [p.sb("gl%d" % i, [128, 512]) for i in range(2)]; gc = [p.sb("gc%d" % i, [128, 512]) for i in range(2)]
    xt = [p.sb("xt%d" % i, [128, 512]) for i in range(3)]; tm = [p.sb("tm%d" % i, [128, 512]) for i in range(2)]
    ot = [p.sb("ot%d" % i, [128, 512]) for i in range(3)]
    pst = [p.ps("pw%d" % i, [128, 512]) for i in range(4)]
    k = 0; u = 0
    for j in range(8):
        cs = slice(j * 512, (j + 1) * 512)
        Wb = WB[j % 2]
        for q in range(4):
            s_ = stg[k % 2]; k += 1
            p.dma("sp", s_, w[:, q * 8:(q + 1) * 8, cs])
            e = ("dve", "act")[q % 2]
            p.copy((Wb[:, q * 8:(q + 1) * 8, :], q), s_, e=e)
        GL = gl[j % 2]; GC = gc[j % 2]
        p.dma("sp", GL, g_l[:, cs]); p.dma("sp", GC, g_c[:, cs])
        for t in range(9):
            n = 128 if t < 8 else 64
            X = xt[u % 3]; O_ = ot[u % 3]; T_ = tm[u % 2]; ps = pst[u % 4]; u += 1
            src = xl[t * 128:(t + 1) * 128, cs] if t < 8 else xc[:, cs]
            dst = ol[t * 128:(t + 1) * 128, cs] if t < 8 else oc[:, cs]
            p.dma("sp", X[:n], src)
            for kc in range(32):
                p.mm(ps[:n], (MT[:, kc, t * 128:t * 128 + n], kc // 8), (Wb[:, kc, :], kc // 8), start=(kc == 0), stop=(kc == 31))
            p.tt(T_[:n], ps[:n], (GL if t < 8 else GC)[:n], ALU.mult, e="dve")
            p.tt(O_[:n], T_[:n], X[:n], ALU.add, e="dve")
            p.dma("sp", dst, O_[:n])
    p.finish([ol, oc])
    return p


def _idma(p, out, in_dram, idx_ap, bounds=None):
    q = "pool"
    i = p.dnext
    p.dnext = (p.dnext + 1) % len(p.dsem)
    if p.dval[i] > 0:
        p._wait(q, ("d", i, p.dval[i]))
    reads = [in_dram, idx_ap]; writes = [out]
    p._deps(q, reads, writes)
    kw = {}
    if bounds is not None:
        rk_ = "_breg_%d" % int(bounds)
        if not hasattr(p, rk_):
            setattr(p, rk_, p.nc.gpsimd.to_reg(int(bounds)))
        kw = {"bounds_check": getattr(p, rk_), "oob_is_err": False}
    ins = p.nc.gpsimd.indirect_dma_start(out=p._a(out), out_offset=None, in_=p._a(in_dram),
                                         in_offset=bass.IndirectOffsetOnAxis(ap=p._a(idx_ap), axis=0), **kw)
    p.dval[i] += 16
    ins.then_inc(p.dsem[i], 16)
    tok = ("d", i, p.dval[i])
    p._commit(tok, reads, writes)
    p.n_ins += 1
    return tok


def build_route():
    p = P()
    arow = p.dram("arow", [4, 128, 4096], F32, "ExternalInput"); acol = p.dram("acol", [4, 128, 32], F32, "ExternalInput")
    arowc = p.dram("arowc", [4, 128, 256], F32, "ExternalInput"); acolc = p.dram("acolc", [4, 128, 2], F32, "ExternalInput")
    iota = p.dram("iota", [128, 512], F32, "ExternalInput"); tv = p.dram("tv", [128, 32], F32, "ExternalInput")
    idx = p.dram("idxr", [4, 1, 512], I32, "ExternalOutput"); gate = p.dram("gater", [4, 1, 512], F32, "ExternalOutput")
    rank = p.dram("rank", [4, 128, 32], F32, "ExternalOutput")
    idxc = p.dram("idxcr", [4, 1, 32], I32, "ExternalOutput"); gatec = p.dram("gatecr", [4, 1, 32], F32, "ExternalOutput")
    rankc = p.dram("rankc", [4, 128, 2], F32, "ExternalOutput")
    io = p.sb("io", [128, 512]); tvt = p.sb("tvt", [128, 32])
    p.dma("sp", io, iota); p.dma("sp", tvt, tv)
    AR = [p.sb("AR%d" % i, [128, 4096]) for i in range(2)]; junk = p.sb("junk", [128, 4096]); junk2 = p.sb("junk2", [128, 4096])
    NAC = p.sb("NAC", [128, 32])
    AC = [p.sb("AC%d" % i, [128, 32]) for i in range(2)]; RK = [p.sb("RK%d" % i, [128, 32]) for i in range(2)]
    TV2 = [p.sb("TV2_%d" % i, [128, 32, 2]) for i in range(2)]
    OH = p.sb("OH", [128, 32, 512])
    PS = [p.ps("pr%d" % i, [2, 512]) for i in range(2)]
    IG = [p.sb("IG%d" % i, [2, 512]) for i in range(2)]; II = [p.sb("II%d" % i, [2, 512], I32) for i in range(2)]
    ARc = [p.sb("ARc%d" % i, [128, 256]) for i in range(2)]; ACc = [p.sb("ACc%d" % i, [128, 2]) for i in range(2)]
    RKc = [p.sb("RKc%d" % i, [128, 2]) for i in range(2)]; TV2c = [p.sb("TV2c%d" % i, [128, 2, 2]) for i in range(2)]
    OHc = p.sb("OHc", [128, 2, 32]); IGc = [p.sb("IGc%d" % i, [2, 32]) for i in range(2)]
    IIc = [p.sb("IIc%d" % i, [2, 32], I32) for i in range(2)]
    for q in range(4):
        A = AR[q % 2]; C = AC[q % 2]; R_ = RK[q % 2]; T2 = TV2[q % 2]; ps = PS[q % 2]
        p.dma("sp", A, arow[q]); p.dma("sp", C, acol[q])
        p.memset(R_, 0.0, e="pool")
        p.ts(NAC, C, -1.0, ALU.mult, e="dve")
        for tc in range(32):
            if tc % 2 == 0:
                p.ts(junk, A, C[:, tc:tc + 1], ALU.is_gt, 0.0, ALU.add, accum_out=R_[:, tc:tc + 1], e="dve")
            else:
                p.act(junk2, A, AF.Sign, bias=NAC[:, tc:tc + 1], accum_out=R_[:, tc:tc + 1])
        rodd = R_.rearrange("p (a two) -> p a two", two=2)[:, :, 1]
        p.ts(rodd, rodd, 0.5, ALU.mult, 2047.5, ALU.add, e="dve")
        p.copy(T2[:, :, 0], tvt, e="dve"); p.copy(T2[:, :, 1], C, e="dve")
        for tc in range(32):
            p.ts(OH[:, tc, :], io, R_[:, tc:tc + 1], ALU.is_equal, e="dve")
        for tc in range(32):
            p.mm(ps, T2[:, tc, :], OH[:, tc, :], start=(tc == 0), stop=(tc == 31))
        p.copy(IG[q % 2], ps, e="dve")
        p.copy(II[q % 2], IG[q % 2], e="dve")
        p.dma("sp", idx[q], II[q % 2][0:1, :]); p.dma("sp", gate[q], IG[q % 2][1:2, :]); p.dma("sp", rank[q], R_)
        A = ARc[q % 2]; C = ACc[q % 2]; R_ = RKc[q % 2]; T2 = TV2c[q % 2]
        p.dma("sp", A, arowc[q]); p.dma("sp", C, acolc[q])
        p.memset(R_, 0.0, e="pool")
        for tc in range(2):
            p.ts(junk[:, :256], A, C[:, tc:tc + 1], ALU.is_gt, 0.0, ALU.add, accum_out=R_[:, tc:tc + 1], e="dve")
        p.copy(T2[:, :, 0], tvt[:, 0:2], e="dve"); p.copy(T2[:, :, 1], C, e="dve")
        for tc in range(2):
            p.ts(OHc[:, tc, :], io[:, :32], R_[:, tc:tc + 1], ALU.is_equal, e="dve")
        for tc in range(2):
            p.mm(ps[:, :32], T2[:, tc, :], OHc[:, tc, :], start=(tc == 0), stop=(tc == 1))
        p.copy(IGc[q % 2], ps[:, :32], e="dve")
        p.copy(IIc[q % 2], IGc[q % 2], e="dve")
        p.dma("sp", idxc[q], IIc[q % 2][0:1, :]); p.dma("sp", gatec[q], IGc[q % 2][1:2, :]); p.dma("sp", rankc[q], R_)
    p.finish([idx, gate, rank, idxc, gatec, rankc])
    return p


DFF = 1024


def build_experts():
    p = P()
    idx = p.dram("idx", [4, 128, 4], I32, "ExternalInput"); gate = p.dram("gate", [4, 128, 4], F32, "ExternalInput")
    idxc = p.dram("idxc", [4, 32, 1], I32, "ExternalInput"); gatec = p.dram("gatec", [4, 32, 1], F32, "ExternalInput")
    h2 = [p.dram("h2_%d" % b, [4096, D], BF16, "ExternalInput") for b in range(2)]
    h2c = [p.dram("h2c_%d" % b, [256, D], BF16, "ExternalInput") for b in range(2)]
    wg = p.dram("wg", [2, 8, 128, 32 * 128], F32, "ExternalInput"); wu = p.dram("wu", [2, 8, 128, 32 * 128], F32, "ExternalInput")
    wd = p.dram("wd", [2, 8, 128, 8 * 512], F32, "ExternalInput")
    idn = p.dram("idn", [128, 128], BF16, "ExternalInput")
    yg = p.dram("yg", [4, 512, D], BF16, "ExternalOutput"); ygc = p.dram("ygc", [4, 32, D], BF16, "ExternalOutput")
    idt = p.sb("idt", [128, 128], BF16); p.dma("sp", idt, idn)
    IDX = p.sb("IDX", [128, 4, 4], I32); G = p.sb("G", [128, 4, 4]); IDXc = p.sb("IDXc", [32, 4], I32); Gc = p.sb("Gc", [32, 4])
    for q in range(4):
        p.dma("sp", IDX[:, q, :], idx[q]); p.dma("sp", G[:, q, :], gate[q])
        p.dma("sp", IDXc[:, q:q + 1], idxc[q]); p.dma("sp", Gc[:, q:q + 1], gatec[q])
    XG = p.sb("XG", [128, 4, D], BF16); XGc = p.sb("XGc", [32, D], BF16)
    XT = [p.sb("XT%d" % b, [128, 32, 512], BF16) for b in range(2)]; XTc = p.sb("XTc", [128, 32, 64], BF16)
    HT = [p.sb("HT%d" % b, [128, 8, 512], BF16) for b in range(2)]; HTc = p.sb("HTc", [128, 8, 64], BF16)
    stg = [p.sb("stg%d" % i, [128, 4096]) for i in range(2)]
    wb = [p.sb("wb%d" % i, [128, 4096], BF16) for i in range(4)]
    HS = [p.sb("HS%d" % i, [128, 512]) for i in range(2)]
    Y = [p.sb("Y%d" % i, [128, 512], BF16) for i in range(3)]
    PT = [p.ps("pt%d" % i, [128, 1024], BF16) for i in range(2)]
    PG = [p.ps("pg%d" % i, [128, 512]) for i in range(2)]; PU = [p.ps("pu%d" % i, [128, 512]) for i in range(2)]
    PY = [p.ps("py%d" % i, [128, 512]) for i in range(2)]
    sk = [0]; wk = [0]

    def load_w(src):
        s_ = stg[sk[0] % 2]; sk[0] += 1
        W = wb[wk[0] % 4]; wk[0] += 1
        p.dma("sp", s_, src)
        p.copy(W[:, :2048], s_[:, :2048], e="dve"); p.copy(W[:, 2048:], s_[:, 2048:], e="pool" if False else "act")
        return W

    for e in range(2):
        for b in range(2):
            q = e * 2 + b
            for rs in range(4):
                _idma(p, XG[:, rs, :], h2[b], IDX[:, q, rs:rs + 1])
            k = 0
            for rs in range(4):
                for g8 in range(4):
                    pt = PT[k % 2]; k += 1
                    for i in range(8):
                        kc = g8 * 8 + i
                        p.tr(pt[:, i * 128:(i + 1) * 128], XG[:, rs, kc * 128:(kc + 1) * 128], idt)
                    p.copy(XT[b][:, g8 * 8:(g8 + 1) * 8, rs * 128:(rs + 1) * 128], pt.rearrange("p (a r) -> p a r", a=8),
                           e=("dve", "act")[k % 2])
            _idma(p, XGc, h2c[b], IDXc[:, q:q + 1])
            for g8 in range(4):
                pt = PT[k % 2]; k += 1
                for i in range(8):
                    kc = g8 * 8 + i
                    p.tr(pt[:, i * 32:(i + 1) * 32], XGc[:, kc * 128:(kc + 1) * 128], idt[:32, :32])
                p.copy(XTc[:, g8 * 8:(g8 + 1) * 8, b * 32:(b + 1) * 32], pt[:, :256].rearrange("p (a r) -> p a r", a=8), e="dve")
        u = 0
        for ft in range(8):
            WG = load_w(wg[e, ft]).rearrange("p (k f) -> p k f", f=128)
            WU = load_w(wu[e, ft]).rearrange("p (k f) -> p k f", f=128)
            for b in range(3):
                rhs = XT[b] if b < 2 else XTc
                n = 512 if b < 2 else 64
                pg = PG[u % 2]; pu = PU[u % 2]; hs = HS[u % 2]; u += 1
                for kc in range(32):
                    p.mm(pg[:, :n], WG[:, kc, :], rhs[:, kc, :], start=(kc == 0), stop=(kc == 31))
                for kc in range(32):
                    p.mm(pu[:, :n], WU[:, kc, :], rhs[:, kc, :], start=(kc == 0), stop=(kc == 31))
                p.act(hs[:, :n], pg[:, :n], AF.Silu)
                dst = HT[b][:, ft, :] if b < 2 else HTc[:, ft, :]
                p.tt(dst, hs[:, :n], pu[:, :n], ALU.mult, e="dve")
        v = 0
        for dt in range(8):
            WD = load_w(wd[e, dt]).rearrange("p (k d) -> p k d", d=512)
            for b in range(2):
                q = e * 2 + b
                for rs in range(4):
                    py = PY[v % 2]; y = Y[v % 3]; v += 1
                    for fc in range(8):
                        p.mm(py, HT[b][:, fc, rs * 128:(rs + 1) * 128], WD[:, fc, :], start=(fc == 0), stop=(fc == 7))
                    p.ts(y, py, G[:, q, rs:rs + 1], ALU.mult, e=("dve", "act")[0])
                    p.dma("sp", yg[q, rs * 128:(rs + 1) * 128, dt * 512:(dt + 1) * 512], y)
                py = PY[v % 2]; y = Y[v % 3]; v += 1
                for fc in range(8):
                    p.mm(py[:32], HTc[:, fc, b * 32:(b + 1) * 32], WD[:, fc, :], start=(fc == 0), stop=(fc == 7))
                p.ts(y[:32], py[:32], Gc[:, q:q + 1], ALU.mult, e="dve")
                p.dma("sp", ygc[q, :, dt * 512:(dt + 1) * 512], y[:32])
    p.finish([yg, ygc])
    return p


def build_combine():
    p = P()
    xl = p.dram("xl", [1024, D], F32, "ExternalInput"); xc = p.dram("xc", [64, D], F32, "ExternalInput")
    g_l = p.dram("g_l", [128, D], F32, "ExternalInput"); g_c = p.dram("g_c", [128, D], F32, "ExternalInput")
    rk = p.dram("rk", [128, 9, 16], F32, "ExternalInput")
    eo = p.dram("eo", [128, 2, 16], F32, "ExternalInput")
    yall = p.dram("yall", [16 * 512, D], BF16, "ExternalInput")
    ycall = p.dram("ycall", [16 * 32, D], BF16, "ExternalInput")
    idn = p.dram("idn", [128, 128], BF16, "ExternalInput")
    ol = p.dram("ol", [1024, D], F32, "ExternalOutput"); oc = p.dram("oc", [64, D], F32, "ExternalOutput")
    BIG = 1.0e6
    GL = p.sb("GL", [128, D]); GC = p.sb("GC", [128, D]); idt = p.sb("idt", [128, 128], BF16)
    p.dma("sp", GL, g_l); p.dma("sp", GC, g_c); p.dma("sp", idt, idn)
    RK = p.sb("RK", [128, 9, 16]); EO = p.sb("EO", [128, 2, 16]); SEL = p.sb("SEL", [128, 9, 16]); IDF = p.sb("IDF", [128, 9, 16])
    IDI = p.sb("IDI", [128, 9, 16], I32)
    p.dma("sp", RK, rk); p.dma("sp", EO, eo)
    for (t0, t1, cap, ty) in ((0, 8, 512, 0), (8, 9, 32, 1)):
        r = RK[:, t0:t1, :]
        p.ts(SEL[:, t0:t1, :], r, float(cap), ALU.is_lt)
        p.tt(IDF[:, t0:t1, :], r, EO[:, ty:ty + 1, :].to_broadcast([128, t1 - t0, 16]), ALU.add)
        p.tt(IDF[:, t0:t1, :], IDF[:, t0:t1, :], SEL[:, t0:t1, :], ALU.mult)
        p.ts(IDF[:, t0:t1, :], IDF[:, t0:t1, :], BIG, ALU.add)
    p.copy(IDI, IDF)
    NG = 6
    GB = [p.sb("GB%d" % i, [128, D], BF16) for i in range(NG)]
    X = [p.sb("X%d" % i, [128, D]) for i in range(2)]
    TM = [p.sb("TM%d" % i, [128, 512]) for i in range(2)]
    PS = [p.ps("pc%d" % i, [128, 512]) for i in range(8)]
    k = 0; u = 0
    for t in range(9):
        n = 128 if t < 8 else 64
        src = xl[t * 128:(t + 1) * 128, :] if t < 8 else xc
        dst = ol[t * 128:(t + 1) * 128, :] if t < 8 else oc
        ysrc = yall if t < 8 else ycall
        nrows = 16 * 512 if t < 8 else 16 * 32
        Xt = X[t % 2]
        p.dma("sp", Xt[:n], src)
        for e in range(16):
            gb = GB[k % NG]; k += 1
            p.memset(gb[:n], 0.0, e="dve")
            _idma(p, gb[:n], ysrc, IDI[:n, t, e:e + 1], bounds=nrows - 1)
            for ct in range(8):
                p.mm(PS[ct][:n], idt[:n, :n], gb[:n, ct * 512:(ct + 1) * 512], start=(e == 0), stop=(e == 15))
        G_ = GL if t < 8 else GC
        for ct in range(8):
            cs = slice(ct * 512, (ct + 1) * 512)
            tm = TM[u % 2]; u += 1
            p.tt(tm[:n], PS[ct][:n], G_[:n, cs], ALU.mult, e="dve")
            p.tt(Xt[:n, cs], Xt[:n, cs], tm[:n], ALU.add, e="dve")
        p.dma("sp", dst, Xt[:n])
    p.finish([ol, oc])
    return p


def _prog(name, fn):
    if name not in _CACHE:
        _CACHE[name] = fn()
    return _CACHE[name]


def _rep(v, n=128):
    return np.ascontiguousarray(np.broadcast_to(np.asarray(v)[None], (n,) + tuple(np.asarray(v).shape)))


def _fm(a):
    return np.ascontiguousarray(a.reshape(2, TB, -1).transpose(0, 2, 1))


def _tokc(a):
    return np.ascontiguousarray(a.reshape(2, NCH, 64, -1).transpose(0, 2, 1, 3))


_U64 = np.triu(np.ones((64, 64), np.float32))
_V64 = np.tril(np.ones((64, 64), np.float32), -1)


def _run_norm(xl, xc, nw, mod_l, sc_i, sh_i, out_bf16=True, modulate=True, router_w=None):
    name = "norm_%d%d%d" % (out_bf16, modulate, router_w is not None)
    p = _prog(name, lambda: build_norm(out_bf16, modulate, router_w is not None))
    nwr = _rep(nw)
    ims = []
    extra = {}
    if router_w is not None:
        extra = {"wr": np.ascontiguousarray(router_w.reshape(32, 128, 16).transpose(1, 0, 2)), "idn": np.eye(128, dtype=np.float32)}
    reps = {}
    if modulate:
        for m in range(3):
            reps[m] = (_rep(mod_l[m, sc_i * D:(sc_i + 1) * D]), _rep(mod_l[m, sh_i * D:(sh_i + 1) * D]))
    for i in range(8):
        b = i // 4
        d = {"xl": xl[i * 1024:(i + 1) * 1024], "xc": xc[i * 64:(i + 1) * 64], "nw": nwr}
        if modulate:
            d.update({"sc_l": reps[b][0], "sh_l": reps[b][1], "sc_c": reps[2][0], "sh_c": reps[2][1]})
        d.update(extra)
        ims.append(d)
    res = run(p, ims).results
    ol = np.concatenate([r["ol"] for r in res], 0); oc = np.concatenate([r["oc"] for r in res], 0)
    if router_w is not None:
        return ol, oc, np.concatenate([r["al"] for r in res], 0), np.concatenate([r["ac"] for r in res], 0)
    return ol, oc


def _layer(l, xl, xc, mod_l, W):
    h1l, h1c = _run_norm(xl, xc, W["norm1_w"][l], mod_l, 1, 0)
    h = tok_order(h1l.reshape(2, 4096, D), h1c.reshape(2, 256, D))
    hT = np.ascontiguousarray(h.reshape(68, 128, 32, 128).transpose(0, 3, 2, 1))
    del h
    p = _prog("proj", build_proj)
    w_in = W["w_in"][l]
    ims = []
    sls = []
    for c in range(8):
        idx, sl = head_cols(c); sls.append(sl)
        ims.append({"hT": hT, "w": np.ascontiguousarray(w_in[:, idx].reshape(32, 128, NPROJ).transpose(1, 0, 2))})
    proj = [r["o"] for r in run(p, ims).results]
    del ims, hT
    tabs = fnet_tables()
    p = _prog("fnet", build_fnet_sconv)
    ims = []
    for c in range(8):
        pc = proj[c]; sl = sls[c]
        d = {"finT": _fm(pc[:, sl["fin"]]), "shT": _fm(pc[:, sl["sh"]]), "sbT": _fm(pc[:, sl["sb"]]), "sgT": _fm(pc[:, sl["sg"]]),
             "cw": np.ascontiguousarray(W["sc_conv_w"][l][:, c * 128:(c + 1) * 128].T)}
        d.update(tabs)
        ims.append(d)
    res = run(p, ims).results
    ob = np.stack([r["ob"] for r in res]); od = np.stack([r["od"] for r in res])
    p = _prog("gla", build_gla)
    ims = []
    for c in range(8):
        pc = proj[c]; sl = sls[c]
        lr = pc[:, sl["glr"]]
        ims.append({"qT": _fm(pc[:, sl["gq"]]), "kT": _fm(pc[:, sl["gk"]]), "vtok": _tokc(pc[:, sl["gv"]]),
                    "ggtok": _tokc(pc[:, sl["gg"]]).reshape(2, 64, -1),
                    "lrT": np.ascontiguousarray(np.stack([_fm(lr[:, :16]), _fm(lr[:, 16:])], 1)),
                    "up": np.ascontiguousarray(W["gla_gate_up"][l][:, :, c * 64:(c + 1) * 64]),
                    "gb": np.ascontiguousarray(W["gla_gate_b"][l][:, c * 64:(c + 1) * 64].T),
                    "nwr": _rep(W["gla_norm_w"][l], 64),
                    "msk": np.stack([_U64, _U64.T.copy()]), "idn": np.eye(64, dtype=np.float32)})
    oa = np.stack([r["oa"] for r in run(p, ims).results])
    p = _prog("gdn_pre", build_gdn_pre)
    ims = []
    gw = W["gdn_conv_w"][l]
    for c in range(8):
        pc = proj[c]; sl = sls[c]
        comps = [pc[:, sl[n]] for n in ("dq", "dk", "dv")]
        raw = np.ascontiguousarray(np.stack([_tokc(a) for a in comps], 1))
        rawc = np.ascontiguousarray(np.stack([a.reshape(2, TB, 128)[:, :256] for a in comps], 1))
        cwr = np.stack([np.broadcast_to(gw[:, j * 1024 + c * 128: j * 1024 + (c + 1) * 128][None], (64, 3, 128)) for j in range(3)])
        ims.append({"raw": raw, "rawc": rawc, "cwr": np.ascontiguousarray(cwr)})
    qkvn = [r["o"] for r in run(p, ims).results]
    p = _prog("gdn_main", build_gdn_main)
    ims = []
    msk = np.stack([_U64, _V64, _U64.T.copy(), _V64.T.copy()])
    for c in range(8):
        pc = proj[c]; sl = sls[c]
        q5 = qkvn[c].reshape(2, 3, 64, NCH, 128)

        def featmajor(x_):
            return np.ascontiguousarray(x_.transpose(0, 3, 2, 1).reshape(2, 128, TB))
        prm = np.array([W["gdn_dt_bias"][l][0, c], W["gdn_dt_bias"][l][1, c], W["gdn_A_log"][l][0, c], W["gdn_A_log"][l][1, c]], np.float32)
        ims.append({"qT": featmajor(q5[:, 0]), "kT": featmajor(q5[:, 1]), "ktok": np.ascontiguousarray(q5[:, 1]),
                    "vtok": np.ascontiguousarray(q5[:, 2]),
                    "atok": np.ascontiguousarray(np.stack([_tokc(pc[:, sl["af"]])[..., 0], _tokc(pc[:, sl["ab"]])[..., 0]], 1)),
                    "btok": np.ascontiguousarray(np.stack([_tokc(pc[:, sl["bf"]])[..., 0], _tokc(pc[:, sl["bb"]])[..., 0]], 1)),
                    "ztok": _tokc(pc[:, sl["dz"]]).reshape(2, 64, -1), "prm": _rep(prm),
                    "nwr": _rep(W["gdn_norm_w"][l], 64), "msk": msk, "idn": np.eye(64, dtype=np.float32)})
    oc_ = np.stack([r["oc"] for r in run(p, ims).results])
    del ims, proj, qkvn

    def tokmaj_to_fm(o):
        o = o.reshape(8, 2, 64, NCH, 128).transpose(0, 4, 1, 3, 2)
        return o.reshape(8 * 128, 2, TB)

    def fmh(o):
        return o.transpose(0, 2, 1, 3).reshape(8 * 128, 2, TB)
    mixT = np.concatenate([tokmaj_to_fm(oa), fmh(ob), tokmaj_to_fm(oc_), fmh(od)], 0)
    p = _prog("wout", build_wout)
    w = np.ascontiguousarray(W["w_out"][l].reshape(32, 128, D).transpose(1, 0, 2))
    g1 = [_rep(mod_l[m, 2 * D:3 * D]) for m in range(3)]
    ims = []
    for i in range(8):
        b = i // 4; j = i % 4
        lat = mixT[:, b, 256 + j * 1024: 256 + (j + 1) * 1024]; cx = mixT[:, b, j * 64:j * 64 + 64]
        mt = np.concatenate([lat, cx], 1).reshape(32, 128, 1088).transpose(1, 0, 2)
        ims.append({"mixT": np.ascontiguousarray(mt), "w": w, "xl": xl[i * 1024:(i + 1) * 1024], "xc": xc[i * 64:(i + 1) * 64],
                    "g_l": g1[b], "g_c": g1[2]})
    res = run(p, ims).results
    xal = np.concatenate([r["ol"] for r in res], 0); xac = np.concatenate([r["oc"] for r in res], 0)
    del ims, mixT
    h2, h2c, al, ac = _run_norm(xal, xac, W["norm2_w"][l], mod_l, 4, 3, router_w=W["w_router"][l])
    al = al.reshape(2, 4096, 16); ac = ac.reshape(2, 256, 16)
    h2 = h2.reshape(2, 4096, D); h2c = h2c.reshape(2, 256, D)
    p = _prog("route", build_route)
    iota = _rep(np.arange(512, dtype=np.float32))
    tv = (np.arange(32)[None, :] * 128 + np.arange(128)[:, None]).astype(np.float32)
    ims = []
    for j in range(8):
        arow = []; acol = []; arowc = []; acolc = []
        for e in (2 * j, 2 * j + 1):
            for b in range(2):
                a = al[b, :, e]; c_ = ac[b, :, e]
                arow.append(np.broadcast_to(a[None], (128, 4096))); acol.append(a.reshape(32, 128).T)
                arowc.append(np.broadcast_to(c_[None], (128, 256))); acolc.append(c_.reshape(2, 128).T)
        ims.append({"arow": np.ascontiguousarray(np.stack(arow)), "acol": np.ascontiguousarray(np.stack(acol)),
                    "arowc": np.ascontiguousarray(np.stack(arowc)), "acolc": np.ascontiguousarray(np.stack(acolc)), "iota": iota, "tv": tv})
    route = []
    for r in run(p, ims).results:
        route.append({"idx": np.ascontiguousarray(r["idxr"].reshape(4, 4, 128).transpose(0, 2, 1)),
                      "gate": np.ascontiguousarray(r["gater"].reshape(4, 4, 128).transpose(0, 2, 1)),
                      "idxc": np.ascontiguousarray(r["idxcr"].reshape(4, 32, 1)), "gatec": np.ascontiguousarray(r["gatecr"].reshape(4, 32, 1)),
                      "rank": r["rank"], "rankc": r["rankc"]})
    import ml_dtypes
    p = _prog("experts", build_experts)
    idn = np.eye(128, dtype=np.float32).astype(ml_dtypes.bfloat16)
    ims = []
    for j in range(8):
        es = slice(2 * j, 2 * j + 2)
        wg = np.ascontiguousarray(W["w_gate"][l][es].reshape(2, 32, 128, 8, 128).transpose(0, 3, 2, 1, 4).reshape(2, 8, 128, 4096))
        wu = np.ascontiguousarray(W["w_up"][l][es].reshape(2, 32, 128, 8, 128).transpose(0, 3, 2, 1, 4).reshape(2, 8, 128, 4096))
        wd = np.ascontiguousarray(W["w_down"][l][es].reshape(2, 8, 128, 8, 512).transpose(0, 3, 2, 1, 4).reshape(2, 8, 128, 4096))
        r = route[j]
        ims.append({"idx": r["idx"], "gate": r["gate"], "idxc": r["idxc"], "gatec": r["gatec"],
                    "h2_0": h2[0], "h2_1": h2[1], "h2c_0": h2c[0], "h2c_1": h2c[1], "wg": wg, "wu": wu, "wd": wd, "idn": idn})
    res = run(p, ims).results
    yg = [r["yg"] for r in res]; ygc = [r["ygc"] for r in res]
    del ims
    yall = []; ycall = []
    rank = np.zeros((2, 4096, 16), np.float32); rankc = np.zeros((2, 256, 16), np.float32)
    zr = np.zeros((1, D), np.float32)
    for b in range(2):
        rows = []; rowsc = []
        for e in range(16):
            j = e // 2; q = (e % 2) * 2 + b
            rows.append(yg[j][q]); rowsc.append(ygc[j][q])
            rank[b, :, e] = route[j]["rank"][q].T.reshape(-1)
            rankc[b, :, e] = route[j]["rankc"][q].T.reshape(-1)
        yall.append(np.concatenate(rows, 0)); ycall.append(np.concatenate(rowsc, 0))
    del yg, ygc
    eo = np.stack([np.arange(16) * 512 - 1.0e6, np.arange(16) * 32 - 1.0e6]).astype(np.float32)
    eo = _rep(eo)
    idn_bf = np.eye(128, dtype=np.float32).astype(ml_dtypes.bfloat16)
    p = _prog("combine", build_combine)
    g2 = [_rep(mod_l[m, 5 * D:6 * D]) for m in range(3)]
    ims = []
    for i in range(8):
        b = i // 4; j = i % 4
        rk = np.full((128, 9, 16), 1e6, np.float32)
        rk[:, :8, :] = rank[b, j * 1024:(j + 1) * 1024].reshape(8, 128, 16).transpose(1, 0, 2)
        rk[:64, 8, :] = rankc[b, j * 64:(j + 1) * 64]
        ims.append({"xl": xal[i * 1024:(i + 1) * 1024], "xc": xac[i * 64:(i + 1) * 64], "g_l": g2[b], "g_c": g2[2],
                    "rk": rk, "eo": eo, "yall": yall[b], "ycall": ycall[b], "idn": idn_bf})
    res = run(p, ims).results
    return np.concatenate([r["ol"] for r in res], 0), np.concatenate([r["oc"] for r in res], 0)


def kernel(x, c, ctx, c_ctx, w_ada, b_ada, norm1_w, norm2_w, w_in, w_out, gla_gate_up, gla_gate_b, gla_norm_w,
           gdn_conv_w, gdn_A_log, gdn_dt_bias, gdn_norm_w, sc_conv_w, w_router, w_gate, w_up, w_down, final_norm_w):
    W = dict(norm1_w=norm1_w, norm2_w=norm2_w, w_in=w_in, w_out=w_out, gla_gate_up=gla_gate_up, gla_gate_b=gla_gate_b,
             gla_norm_w=gla_norm_w, gdn_conv_w=gdn_conv_w, gdn_A_log=gdn_A_log, gdn_dt_bias=gdn_dt_bias, gdn_norm_w=gdn_norm_w,
             sc_conv_w=sc_conv_w, w_router=w_router, w_gate=w_gate, w_up=w_up, w_down=w_down)
    W = {k: np.asarray(v, np.float32) for k, v in W.items()}
    x = np.asarray(x, np.float32); ctx = np.asarray(ctx, np.float32)
    w_ada = np.asarray(w_ada, np.float32); b_ada = np.asarray(b_ada, np.float32)
    p = _prog("mod", build_mod)
    cvec = np.concatenate([np.asarray(c, np.float32), np.asarray(c_ctx, np.float32)[None]], 0)
    cT = np.ascontiguousarray(cvec.T.reshape(32, 128, 3).transpose(1, 0, 2))
    ims = []
    for j in range(8):
        wj = np.ascontiguousarray(w_ada[:, :, j * 3072:(j + 1) * 3072].reshape(2, 32, 128, 3072).transpose(0, 2, 1, 3))
        ims.append({"cT": cT, "w": wj, "b": np.ascontiguousarray(b_ada[:, None, j * 3072:(j + 1) * 3072])})
    mod = np.concatenate([r["o"] for r in run(p, ims).results], axis=2)
    del ims
    xl = x.reshape(8192, D); xc = ctx.reshape(512, D)
    for l in range(2):
        xl, xc = _layer(l, xl, xc, mod[l], W)
    ol, _ = _run_norm(xl, xc, np.asarray(final_norm_w, np.float32), None, 0, 0, out_bf16=False, modulate=False)
    return ol.reshape(2, 4096, D).astype(np.float32)
```
